# Optimizing a Trainium2 kernel written in Bass

```python
import jax
import jax.numpy as jnp
from jax import lax
import numpy as np

D_MODEL = 2048
BATCH = 4
SEQ = 2048
DEPTH = 1
DEC_BATCH = 128
DEC_SEQ = 4
PAST_LEN = 16384
PAGE_SIZE = 128

GLA_HEADS = 4
GLA_DK = D_MODEL // 4
GLA_DV = D_MODEL // 2
GLA_HK = GLA_DK // GLA_HEADS
GLA_HV = GLA_DV // GLA_HEADS
GLA_LOW_RANK = 16
GLA_TAU = 16.0
GLA_CHUNK = 64
CONV_CH = D_MODEL // 2
CONV_K = 3
N_EXPERTS = 32
TOP_K = 4
D_FF = D_MODEL
SWIGLU_LIMIT = 7.0
SWIGLU_ALPHA = 1.702
MOE_BLOCK = 128
N_MOD = 6
EPS = 1e-6
IN_SPLITS = (GLA_DK, GLA_DK, GLA_DV, GLA_LOW_RANK, GLA_DV, CONV_CH, CONV_CH, CONV_CH, D_MODEL, D_MODEL)
D_IN = sum(IN_SPLITS)
SPLIT_POINTS = tuple(int(s) for s in np.cumsum(IN_SPLITS)[:-1])

kernel_name = 'gla_shortconv_moe_adaln_decode_step'


def rmsnorm(x, w):
    xf = x.astype(jnp.float32)
    y = xf * lax.rsqrt(jnp.mean(xf * xf, axis=-1, keepdims=True) + EPS)
    return (y * w.astype(jnp.float32)).astype(x.dtype)


def adaln(c, w, b):
    m = (jax.nn.silu(c) @ w + b).reshape(c.shape[0], N_MOD, D_MODEL)
    return [m[:, i, None, :] for i in range(N_MOD)]


def gla_chunked(q, k, v, log_a, s0):
    bsz, seq_len = q.shape[:2]
    c = min(GLA_CHUNK, seq_len)
    pad = (-seq_len) % c
    f32 = jnp.float32

    def blocks(t):
        t = jnp.pad(t.astype(f32), ((0, 0), (0, pad), (0, 0), (0, 0)))
        return t.reshape(bsz, -1, c, GLA_HEADS, t.shape[-1]).transpose(0, 3, 1, 2, 4)

    q, k, v, log_a = blocks(q), blocks(k), blocks(v), blocks(log_a)
    b = jnp.cumsum(log_a, axis=3)
    b_mid = b[:, :, :, c // 2:c // 2 + 1]
    b_last = b[:, :, :, -1:]
    mask = jnp.tril(jnp.ones((c, c), dtype=bool))
    scores = jnp.einsum('bhnid,bhnjd->bhnij', q * jnp.exp(b - b_mid), k * jnp.exp(b_mid - b))
    o_intra = jnp.einsum('bhnij,bhnje->bhnie', jnp.where(mask, scores, 0.0), v)
    ds = jnp.einsum('bhncd,bhnce->bhnde', k * jnp.exp(b_last - b), v)
    decay = jnp.exp(b_last[:, :, :, 0])

    def step(s, inp):
        dec, d = inp
        return dec[..., None] * s + d, s

    s_final, s_prev = lax.scan(step, s0.astype(f32),
                               (jnp.moveaxis(decay, 2, 0), jnp.moveaxis(ds, 2, 0)))
    o_inter = jnp.einsum('bhncd,nbhde->bhnce', q * jnp.exp(b), s_prev)
    o = (o_intra + o_inter).transpose(0, 2, 3, 1, 4).reshape(bsz, -1, GLA_HEADS, v.shape[-1])
    return o[:, :seq_len], s_final


def token_mixer(xn, gla_s0, conv_buf, w_in, w_gk_up, b_gk, gla_norm_w, w_conv, w_out):
    bsz, seq_len, _ = xn.shape
    q, k, v, a_low, g, cb, cc, ch, ga, gb = jnp.split(xn @ w_in, SPLIT_POINTS, axis=-1)

    def heads(t, d):
        return t.reshape(bsz, seq_len, GLA_HEADS, d)

    log_a = jax.nn.log_sigmoid((a_low @ w_gk_up + b_gk).astype(jnp.float32)) / GLA_TAU
    o, s_new = gla_chunked(heads(q, GLA_HK) * (GLA_HK ** -0.5), heads(k, GLA_HK),
                           heads(v, GLA_HV), heads(log_a, GLA_HK), gla_s0)
    o = o * lax.rsqrt(jnp.mean(o * o, axis=-1, keepdims=True) + EPS) * gla_norm_w.astype(jnp.float32)
    o = o * jax.nn.silu(heads(g, GLA_HV).astype(jnp.float32))
    o_a = o.reshape(bsz, seq_len, GLA_DV).astype(xn.dtype)

    u = cc * ch
    u_ext = jnp.concatenate([conv_buf.astype(u.dtype), u], axis=1)
    conv = sum(w_conv[j] * u_ext[:, j:j + seq_len] for j in range(CONV_K))
    o_b = cb * conv

    y = (jax.nn.sigmoid(ga) * (o_a @ w_out[:GLA_DV])
         + jax.nn.sigmoid(gb) * (o_b @ w_out[GLA_DV:]))
    return y, s_new, u_ext[:, seq_len:]


def moe(x2, w_router, b_router, w_up, b_up, w_down, b_down):
    n_tok = x2.shape[0]
    logits = (x2 @ w_router + b_router).astype(jnp.float32)
    top_v, top_e = lax.top_k(logits, TOP_K)
    gate_w = jax.nn.softmax(top_v, axis=-1).reshape(-1)
    n_pairs = n_tok * TOP_K
    e_flat = top_e.reshape(-1)
    order = jnp.argsort(e_flat)
    e_sorted = e_flat[order]
    tok_sorted = order // TOP_K
    counts = jnp.bincount(e_flat, length=N_EXPERTS)
    padded = (counts + MOE_BLOCK - 1) // MOE_BLOCK * MOE_BLOCK
    start_raw = jnp.cumsum(counts) - counts
    end_pad = jnp.cumsum(padded)
    start_pad = end_pad - padded
    dest = start_pad[e_sorted] + (jnp.arange(n_pairs) - start_raw[e_sorted])
    n_blocks = -(-n_pairs // MOE_BLOCK) + N_EXPERTS
    rows = jnp.zeros((n_blocks * MOE_BLOCK, D_MODEL), x2.dtype).at[dest].set(x2[tok_sorted])
    block_e = jnp.minimum(jnp.searchsorted(end_pad, jnp.arange(n_blocks) * MOE_BLOCK, side='right'),
                          N_EXPERTS - 1)

    def expert_block(args):
        xb, e = args
        h = xb @ w_up[e] + b_up[e]
        h_gate, h_lin = h[:, :D_FF], h[:, D_FF:]
        h_gate = jnp.minimum(h_gate, SWIGLU_LIMIT)
        h_lin = jnp.clip(h_lin, -SWIGLU_LIMIT, SWIGLU_LIMIT)
        act = (h_lin + 1) * h_gate * jax.nn.sigmoid(SWIGLU_ALPHA * h_gate)
        return act @ w_down[e] + b_down[e]

    out_rows = lax.map(expert_block, (rows.reshape(n_blocks, MOE_BLOCK, D_MODEL), block_e))
    out_rows = out_rows.reshape(-1, D_MODEL)
    contrib = (out_rows[dest].astype(jnp.float32) * gate_w[order][:, None]).astype(x2.dtype)
    return jnp.zeros_like(x2).at[tok_sorted].add(contrib)


def trunk(x, c, gla_state, conv_state, params):
    (w_ada, b_ada, norm1_w, w_in, w_gk_up, b_gk, gla_norm_w, w_conv, w_out,
     norm2_w, w_router, b_router, w_up, b_up, w_down, b_down, final_norm_w) = params
    h = x
    new_gla, new_conv = [], []
    for l in range(DEPTH):
        sh1, sc1, g1, sh2, sc2, g2 = adaln(c, w_ada[l], b_ada[l])
        xn = rmsnorm(h, norm1_w[l]) * (1 + sc1) + sh1
        mix, s_new, buf_new = token_mixer(xn, gla_state[l], conv_state[l], w_in[l], w_gk_up[l],
                                          b_gk[l], gla_norm_w[l], w_conv[l], w_out[l])
        h = h + g1 * mix
        hn = rmsnorm(h, norm2_w[l]) * (1 + sc2) + sh2
        ff = moe(hn.reshape(-1, D_MODEL), w_router[l], b_router[l], w_up[l], b_up[l],
                 w_down[l], b_down[l]).reshape(h.shape)
        h = h + g2 * ff
        new_gla.append(s_new.astype(gla_state.dtype))
        new_conv.append(buf_new.astype(conv_state.dtype))
    return rmsnorm(h, final_norm_w), jnp.stack(new_gla), jnp.stack(new_conv)


def setup_inputs(seed: int = 0) -> dict:
    key = jax.random.key(seed)
    ks = jax.random.split(key, 24)
    nrm = jax.random.normal
    f32 = jnp.float32
    return {
        'x_prompt': nrm(ks[0], (BATCH, SEQ, D_MODEL), f32),
        'x_sample': nrm(ks[1], (DEC_BATCH, DEC_SEQ, D_MODEL), f32),
        'state_gla': nrm(ks[2], (DEPTH, DEC_BATCH, GLA_HEADS, GLA_HK, GLA_HV), f32),
        'state_conv': nrm(ks[3], (DEPTH, DEC_BATCH, CONV_K - 1, CONV_CH), f32),
        'c_prompt': nrm(ks[4], (BATCH, D_MODEL), f32),
        'c_sample': nrm(ks[5], (DEC_BATCH, D_MODEL), f32),
        'w_ada': nrm(ks[6], (DEPTH, D_MODEL, N_MOD * D_MODEL), f32) * (0.5 * D_MODEL ** -0.5),
        'b_ada': nrm(ks[7], (DEPTH, N_MOD * D_MODEL), f32) * 0.02,
        'norm1_w': 1.0 + 0.02 * nrm(ks[8], (DEPTH, D_MODEL), f32),
        'w_in': nrm(ks[9], (DEPTH, D_MODEL, D_IN), f32) * D_MODEL ** -0.5,
        'w_gk_up': nrm(ks[10], (DEPTH, GLA_LOW_RANK, GLA_DK), f32) * GLA_LOW_RANK ** -0.5,
        'b_gk': nrm(ks[11], (DEPTH, GLA_DK), f32) * 0.1,
        'gla_norm_w': 1.0 + 0.02 * nrm(ks[12], (DEPTH, GLA_HV), f32),
        'w_conv': nrm(ks[13], (DEPTH, CONV_K, CONV_CH), f32) * CONV_K ** -0.5,
        'w_out': nrm(ks[14], (DEPTH, GLA_DV + CONV_CH, D_MODEL), f32) * (GLA_DV + CONV_CH) ** -0.5,
        'norm2_w': 1.0 + 0.02 * nrm(ks[15], (DEPTH, D_MODEL), f32),
        'w_router': nrm(ks[16], (DEPTH, D_MODEL, N_EXPERTS), f32) * D_MODEL ** -0.5,
        'b_router': nrm(ks[17], (DEPTH, N_EXPERTS), f32) * 0.01,
        'w_up': nrm(ks[18], (DEPTH, N_EXPERTS, D_MODEL, 2 * D_FF), f32) * D_MODEL ** -0.5,
        'b_up': nrm(ks[19], (DEPTH, N_EXPERTS, 2 * D_FF), f32) * 0.02,
        'w_down': nrm(ks[20], (DEPTH, N_EXPERTS, D_FF, D_MODEL), f32) * D_FF ** -0.5,
        'b_down': nrm(ks[21], (DEPTH, N_EXPERTS, D_MODEL), f32) * 0.02,
        'final_norm_w': 1.0 + 0.02 * nrm(ks[22], (D_MODEL,), f32),
    }


def reference(x_prompt, x_sample, state_gla, state_conv, c_prompt, c_sample, w_ada, b_ada, norm1_w,
              w_in, w_gk_up, b_gk, gla_norm_w, w_conv, w_out, norm2_w, w_router, b_router, w_up, b_up,
              w_down, b_down, final_norm_w):
    params = (w_ada, b_ada, norm1_w, w_in, w_gk_up, b_gk, gla_norm_w, w_conv, w_out,
              norm2_w, w_router, b_router, w_up, b_up, w_down, b_down, final_norm_w)
    n_prompt = x_prompt.shape[0]
    gla0 = jnp.zeros((DEPTH, n_prompt, GLA_HEADS, GLA_HK, GLA_HV), state_gla.dtype)
    conv0 = jnp.zeros((DEPTH, n_prompt, CONV_K - 1, CONV_CH), state_conv.dtype)
    y_prompt, gla_prompt, conv_prompt = trunk(x_prompt, c_prompt, gla0, conv0, params)
    y_sample, gla_sample, conv_sample = trunk(x_sample, c_sample, state_gla, state_conv, params)
    return (y_prompt, y_sample, gla_prompt, conv_prompt, gla_sample, conv_sample)
```

```python
import contextlib
import numpy as np
import concourse.bass as bass
import concourse.mybir as mybir
from concourse.bass_utils import run_bass_kernel_spmd

F32 = mybir.dt.float32
BF16 = mybir.dt.bfloat16
AF = mybir.ActivationFunctionType
ALU = mybir.AluOpType

D = 2048
DC = 16
HEADS = 4
HK = 128
HV = 256
DV = 1024
CH = 1024
DIN = 10256
C_Q, C_K, C_V, C_AL, C_G, C_CB, C_CC, C_CHH, C_GA, C_GB = 0, 512, 1024, 2048, 2064, 3088, 4112, 5136, 6160, 8208
EPS = 1e-6
LIMIT = 7.0
ALPHA = 1.702

FULL = dict(NP=1024, NPRE=1024, GT=256, NSEQ=16, E=32, FF=2048, TOPK=4, CAP=448)


class Buf:
    def __init__(self, name):
        self.name = name
        self.w = None
        self.r = []


class Tile:
    def __init__(self, name, t):
        self.name = name
        self.t = t
        self.buf = Buf(name)
        self.dsem = None


class Op:
    __slots__ = ("eng", "fn", "deps", "idx", "signal", "ordinal", "is_dma", "sem", "val", "n")


COMPUTE = ("pe", "act", "dve", "pool")


class Sched:
    def __init__(self, nc, es):
        self.nc = nc
        self.es = es
        self.ops = {k: [] for k in ("pe", "act", "dve", "pool", "sp")}
        self.seen = {k: {} for k in self.ops}
        self.esem = {k: es.enter_context(nc.semaphore("sem_" + k)) for k in COMPUTE}
        self.dsems = []
        self.psum = []
        self.psi = 0
        self.cur = es
        self.pending = {k: [] for k in self.ops}

    def sb(self, name, shape, dt):
        t = self.cur.enter_context(self.nc.sbuf_tensor("s_" + name, list(shape), dt))
        return Tile(name, t)

    def barrier(self):
        deps = []
        for k in COMPUTE:
            if self.ops[k]:
                deps.append(self.ops[k][-1])
        for d in self.dsems:
            if len(d) > 2 and d[2] is not None:
                deps.append(d[2])
        for k in self.ops:
            self.pending[k] = list(deps)

    def init_psum(self, n=8):
        for i in range(n):
            t = self.es.enter_context(self.nc.psum_tensor("ps%d" % i, [128, 512], F32))
            self.psum.append(Tile("ps%d" % i, t))

    def ps(self):
        p = self.psum[self.psi % len(self.psum)]
        self.psi += 1
        return p

    def new_dsem(self, name):
        s = self.es.enter_context(self.nc.semaphore("dsem_" + name))
        d = [s, 0, None]
        self.dsems.append(d)
        return d

    def _bufs(self, lst):
        out = []
        for x in lst:
            out.append(x.buf if isinstance(x, Tile) else x)
        return out

    def _add(self, eng, fn, r, w, is_dma=False, dsem=None, n=1):
        op = Op()
        op.eng, op.fn, op.is_dma, op.n = eng, fn, is_dma, n
        op.signal = False
        op.ordinal = None
        op.idx = len(self.ops[eng])
        rb, wb = self._bufs(r), self._bufs(w)
        deps = []
        for b in rb:
            if b.w is not None:
                deps.append(b.w)
        for b in wb:
            if b.w is not None:
                deps.append(b.w)
            deps.extend(b.r)
        if self.pending[eng]:
            deps.extend(self.pending[eng])
            self.pending[eng] = []
        keep = []
        seen = self.seen[eng]
        for d in deps:
            if d is op:
                continue
            if d.is_dma:
                key = ("d", id(d.sem))
                if seen.get(key, 0) >= d.val:
                    continue
                seen[key] = d.val
                keep.append(d)
            else:
                if d.eng == eng and eng == "pe":
                    continue
                if d.is_dma:
                    continue
                key = ("c", d.eng)
                if seen.get(key, -1) >= d.idx:
                    continue
                seen[key] = d.idx
                d.signal = True
                keep.append(d)
        op.deps = keep
        if is_dma:
            dsem[1] += 16 * n
            op.sem, op.val = dsem, dsem[1]
            dsem[2] = op
        for b in wb:
            b.w = op
            b.r = []
        for b in rb:
            if b not in wb:
                b.r.append(op)
        self.ops[eng].append(op)
        return op

    def op(self, eng, fn, r=(), w=()):
        return self._add(eng, fn, list(r), list(w))

    def dma(self, q, fn, r=(), w=(), sem=None, n=1):
        if sem is None:
            tl = [x for x in list(w) + list(r) if isinstance(x, Tile)][0]
            if tl.dsem is None:
                tl.dsem = self.new_dsem(tl.name)
            sem = tl.dsem
        return self._add(q, fn, list(r), list(w), is_dma=True, dsem=sem, n=n)

    def emit(self, eng, e):
        ordn = 0
        for op in self.ops[eng]:
            if op.signal:
                ordn += 1
                op.ordinal = ordn

    def assign(self):
        for eng in COMPUTE:
            ordn = 0
            for op in self.ops[eng]:
                if op.signal:
                    ordn += 1
                    op.ordinal = ordn

    def run(self, eng, e):
        for op in self.ops[eng]:
            waits = {}
            for d in op.deps:
                if d.is_dma:
                    k = id(d.sem)
                    if k not in waits or waits[k][1] < d.val:
                        waits[k] = (d.sem[0], d.val)
                else:
                    k = d.eng
                    if k not in waits or waits[k][1] < d.ordinal:
                        waits[k] = (self.esem[d.eng], d.ordinal)
            for sem, val in waits.values():
                e.wait_ge(sem, val)
            insts = op.fn(e)
            if not isinstance(insts, (list, tuple)):
                insts = [insts]
            if op.is_dma:
                assert len(insts) == op.n, (len(insts), op.n)
                for i in insts:
                    i.then_inc(op.sem[0], 16)
            elif op.signal:
                insts[-1].then_inc(self.esem[eng], 1)


def build(cfg, debug=()):
    NP, NPRE, GT, NSEQ, E, FF, TOPK, CAP = (cfg[k] for k in ("NP", "NPRE", "GT", "NSEQ", "E", "FF", "TOPK", "CAP"))
    NS = NSEQ * 4
    NT = NP + NS
    FC = FF // 128
    NR = NSEQ + 1
    nc = bass.Bass("TRN2", target_bir_lowering=False)

    def din(name, shape):
        return nc.dram_tensor(name, list(shape), F32, kind="ExternalInput").ap()

    def dout(name, shape):
        return nc.dram_tensor(name, list(shape), F32, kind="ExternalOutput").ap()

    xp = din("xp", [NP, D])
    xpre = din("xpre", [NPRE, D])
    xs = din("xs", [NS, D])
    cvec = din("cvec", [NR, D])
    pflag = din("pflag", [128, 1])
    sgla = din("sgla", [NSEQ, HEADS, HK, HV])
    sconv = din("sconv", [NSEQ * 2, CH])
    cst = din("cst", [128, 1664])
    w_ada = din("w_ada", [D, 6 * D])
    b_ada = din("b_ada", [96, 128])
    norm1_w = din("norm1_w", [16, 128])
    w_in = din("w_in", [D, DIN])
    w_gk_up = din("w_gk_up", [16, 512])
    b_gk = din("b_gk", [1, 512])
    gla_norm_w = din("gla_norm_w", [2, 128])
    w_conv = din("w_conv", [24, 128])
    w_out = din("w_out", [2048, D])
    norm2_w = din("norm2_w", [16, 128])
    w_router = din("w_router", [D, E])
    b_router = din("b_router", [1, E])
    w_up = din("w_up", [E, D, 2 * FF])
    b_up = din("b_up", [E * 2 * FC, 128])
    w_down = din("w_down", [E, FF, D])
    b_down = din("b_down", [E * DC, 128])
    final_norm_w = din("final_norm_w", [16, 128])

    y_p = dout("y_p", [NP, D])
    y_s = dout("y_s", [NS, D])
    gla_p = dout("gla_p", [HEADS, HK, HV])
    conv_p = dout("conv_p", [2, CH])
    gla_s = dout("gla_s", [NSEQ, HEADS, HK, HV])
    conv_s = dout("conv_s", [NSEQ * 2, CH])
    hscr = nc.dram_tensor("hscr", [DC, 128, NT], F32, kind="Internal").ap()
    dbg = {k: dout("dbg_" + k, shp) for k, shp in debug}

    es = contextlib.ExitStack()
    with es:
        S = Sched(nc, es)
        S.init_psum(8)
        sb = S.sb

        cs = sb("cst", [128, 1664], F32)
        S.dma("sp", lambda e: e.dma_start(out=cs.t[:], in_=cst), w=[cs])
        ident_f = cs.t[:, 0:128]
        eps_c = cs.t[:, 640:641]
        one_c = cs.t[:, 641:642]
        cb = sb("cstb", [128, 640], BF16)
        S.op("dve", lambda e: e.tensor_copy(out=cb.t[:], in_=cs.t[:, 0:640]), r=[cs], w=[cb])
        ident_b = cb.t[:, 0:128]
        maskT_b = cb.t[:, 128:256]
        ones_b = cb.t[:, 512:640]
        maskT_f = cs.t[:, 128:256]
        mrev_f = cs.t[:, 256:384]
        smask_f = cs.t[:, 384:448]
        smrev_f = cs.t[:, 448:512]
        ones_f = cs.t[:, 512:640]
        CST = [cs, cb]

        def load_vecT(name, src, n):
            stg = sb(name + "_stg", [128, 128], F32)
            dst = sb(name, [128, n], F32)
            S.dma("sp", lambda e: e.dma_start(out=stg.t[0:n, :], in_=src), w=[stg])
            p = S.ps()
            S.op("pe", lambda e: e.transpose(p.t[:, 0:n], stg.t[0:n, :], ident_f[0:n, 0:n]), r=[stg] + CST, w=[p])
            S.op("dve", lambda e: e.tensor_copy(out=dst.t[:, :], in_=p.t[:, 0:n]), r=[p], w=[dst])
            return dst

        badaT = load_vecT("badaT", b_ada, 96)
        n1T = load_vecT("n1T", norm1_w, 16)
        n2T = load_vecT("n2T", norm2_w, 16)
        nfT = load_vecT("nfT", final_norm_w, 16)
        gnT = load_vecT("gnT", gla_norm_w, 2)
        wcT = load_vecT("wcT", w_conv, 24)
        pfl = sb("pfl", [128, 1], F32)
        S.dma("sp", lambda e: e.dma_start(out=pfl.t[:], in_=pflag), w=[pfl])

        wgk_f = sb("wgk_f", [16, 512], F32)
        S.dma("sp", lambda e: e.dma_start(out=wgk_f.t[:], in_=w_gk_up), w=[wgk_f])
        wgk = sb("wgk", [16, 512], BF16)
        S.op("dve", lambda e: e.tensor_copy(out=wgk.t[:], in_=wgk_f.t[:]), r=[wgk_f], w=[wgk])
        bgk_f = sb("bgk_f", [1, 512], F32)
        S.dma("sp", lambda e: e.dma_start(out=bgk_f.t[:], in_=b_gk), w=[bgk_f])
        bgk = sb("bgk", [1, 512], BF16)
        S.op("dve", lambda e: e.tensor_copy(out=bgk.t[:], in_=bgk_f.t[:]), r=[bgk_f], w=[bgk])

        NW = 3
        wpool = []
        wstate = [0]
        nw_cur = [NW]

        def alloc_wpool(tag, n=NW):
            wpool[:] = [sb("wt%s%d" % (tag, i), [128, 16, 512], BF16) for i in range(n)]
            nw_cur[0] = n

        def wtile():
            t = wpool[wstate[0] % nw_cur[0]]
            wstate[0] += 1
            return t

        def load_w(src2d, c0, ncols, kc=16, t=None, col_off=0):
            if t is None:
                t = wtile()
            v = src2d.rearrange("(kc p) n -> p kc n", p=128)
            S.dma("pool", lambda e: e.dma_start(out=t.t[:, 0:kc, col_off:col_off + ncols], in_=v[:, :, c0:c0 + ncols]), w=[t])
            return t

        siluT = sb("siluT", [128, 16, NR], BF16)
        modT = sb("modT", [128, 96, NR], F32)
        A1 = sb("A1", [128, 16, NR], F32)
        A2 = sb("A2", [128, 16, NR], F32)
        es_ada = contextlib.ExitStack()
        S.cur = es_ada
        alloc_wpool("A")
        cv = sb("cv", [NR, D], F32)
        S.dma("sp", lambda e: e.dma_start(out=cv.t[:], in_=cvec), w=[cv])
        cvs = sb("cvs", [NR, D], F32)
        S.op("act", lambda e: e.activation(out=cvs.t[:], in_=cv.t[:], func=AF.Silu), r=[cv], w=[cvs])
        p = S.ps()
        for kc in range(16):
            S.op("pe", lambda e, kc=kc, p=p: e.transpose(p.t[:, kc * NR:(kc + 1) * NR], cvs.t[0:NR, kc * 128:(kc + 1) * 128],
                                                       ident_f[0:NR, 0:NR]), r=[cvs] + CST, w=[p])
        S.op("dve", lambda e, p=p: e.tensor_copy(out=siluT.t[:].rearrange("p a b -> p (a b)"), in_=p.t[:, 0:16 * NR]), r=[p], w=[siluT])
        for ct in range(24):
            W = load_w(w_ada, ct * 512, 512)
            for sbk in range(4):
                nb = ct * 4 + sbk
                p = S.ps()
                for kc in range(16):
                    S.op("pe", lambda e, kc=kc, p=p, W=W, sbk=sbk: e.matmul(p.t[:, 0:NR], W.t[:, kc, sbk * 128:(sbk + 1) * 128],
                                                                            siluT.t[:, kc, :], start=(kc == 0), stop=(kc == 15)),
                         r=[W, siluT], w=[p])
                S.op("act", lambda e, p=p, nb=nb: e.activation(out=modT.t[:, nb, :], in_=p.t[:, 0:NR], func=AF.Identity,
                                                               bias=badaT.t[:, nb:nb + 1], scale=1.0), r=[p, badaT], w=[modT])
        for dc in range(16):
            S.op("dve", lambda e, dc=dc: e.tensor_scalar(out=A1.t[:, dc, :], in0=modT.t[:, 16 + dc, :], scalar1=1.0,
                                                         scalar2=n1T.t[:, dc:dc + 1], op0=ALU.add, op1=ALU.mult), r=[modT, n1T], w=[A1])
            S.op("dve", lambda e, dc=dc: e.tensor_scalar(out=A2.t[:, dc, :], in0=modT.t[:, 64 + dc, :], scalar1=1.0,
                                                         scalar2=n2T.t[:, dc:dc + 1], op0=ALU.add, op1=ALU.mult), r=[modT, n2T], w=[A2])
        M_SH1, M_G1, M_SH2, M_G2 = 0, 32, 48, 80
        S.barrier()
        es_ada.close()
        S.cur = es

        xstage = []
        xst = [0]

        def load_xT(src, ntok, dst, toff):
            for tc0 in range(0, ntok, 128):
                n = min(128, ntok - tc0)
                stg = xstage[0]
                xst[0] += 1
                S.dma("sp", lambda e, stg=stg, tc0=tc0, n=n: e.dma_start(out=stg.t[0:n, :], in_=src[tc0:tc0 + n, :]), w=[stg])
                for q4 in range(4):
                    p = S.ps()
                    for i in range(4):
                        dc = q4 * 4 + i
                        S.op("pe", lambda e, p=p, i=i, dc=dc, stg=stg, n=n: e.transpose(p.t[:, i * 128:i * 128 + n], stg.t[0:n, dc * 128:(dc + 1) * 128],
                                                                                          ident_f[0:n, 0:n]), r=[stg] + CST, w=[p])
                    S.op("act" if q4 % 2 else "dve",
                         (lambda e, p=p, q4=q4, tc0=tc0, n=n: e.activation(out=dst.t[:, q4 * 4:(q4 + 1) * 4, toff + tc0:toff + tc0 + n],
                                                                           in_=p.t[:, :].rearrange("p (a b) -> p a b", a=4)[:, :, 0:n], func=AF.Copy))
                         if q4 % 2 else
                         (lambda e, p=p, q4=q4, tc0=tc0, n=n: e.tensor_copy(out=dst.t[:, q4 * 4:(q4 + 1) * 4, toff + tc0:toff + tc0 + n],
                                                                            in_=p.t[:, :].rearrange("p (a b) -> p a b", a=4)[:, :, 0:n])),
                         r=[p], w=[dst])

        sqt = []
        rstd_l = []
        ntmp = []

        def alloc_tmps(tag, with_x=False):
            sqt[:] = [sb("sq%s%d" % (tag, i), [128, 512], BF16) for i in range(2)]
            rstd_l[:] = [sb("rstd%s" % tag, [128, 512], F32)]
            ntmp[:] = [sb("ntmp%s%d" % (tag, i), [128, 512], F32) for i in range(2)]
            if with_x:
                xstage[:] = [sb("xstg%s" % tag, [128, D], F32)]

        def rmsnorm_fm(src, soff, ntok, dst, doff, A, B_mod, sample, post=None):
            rstd = rstd_l[0]
            p = S.ps()
            for dc in range(16):
                sq = sqt[dc % 2]
                S.op("act", lambda e, dc=dc, sq=sq: e.activation(out=sq.t[:, 0:ntok], in_=src.t[:, dc, soff:soff + ntok], func=AF.Square),
                     r=[src], w=[sq])
                S.op("pe", lambda e, dc=dc, sq=sq, p=p: e.matmul(p.t[:, 0:ntok], ones_b, sq.t[:, 0:ntok], start=(dc == 0), stop=(dc == 15)),
                     r=[sq] + CST, w=[p])
            S.op("act", lambda e, p=p: e.activation(out=rstd.t[:, 0:ntok], in_=p.t[:, 0:ntok], func=AF.Sqrt, bias=eps_c, scale=1.0 / D),
                 r=[p] + CST, w=[rstd])
            S.op("dve", lambda e: e.reciprocal(out=rstd.t[:, 0:ntok], in_=rstd.t[:, 0:ntok]), r=[rstd], w=[rstd])
            for dc_ in range(16):
                tm = ntmp[dc_ % 2]
                S.op("dve", lambda e, dc=dc_, tm=tm: e.tensor_tensor(out=tm.t[:, 0:ntok], in0=src.t[:, dc, soff:soff + ntok], in1=rstd.t[:, 0:ntok],
                                                                    op=ALU.mult), r=[src, rstd], w=[tm])
                dc = dc_ if post is None else dc_ % 2
                if not sample:
                    if B_mod is None:
                        S.op("act", lambda e, dc=dc, dcf=dc_, tm=tm: e.activation(out=dst.t[:, dc, doff:doff + ntok], in_=tm.t[:, 0:ntok], func=AF.Copy,
                                                                         scale=A.t[:, dcf:dcf + 1]), r=[tm, A], w=[dst])
                    else:
                        S.op("act", lambda e, dc=dc, dcf=dc_, tm=tm: e.activation(out=dst.t[:, dc, doff:doff + ntok], in_=tm.t[:, 0:ntok], func=AF.Identity,
                                                                         scale=A.t[:, dcf, NSEQ:NSEQ + 1], bias=modT.t[:, B_mod + dcf, NSEQ:NSEQ + 1]),
                             r=[tm, A, modT], w=[dst])
                else:
                    tv = tm.t[:, 0:ntok].rearrange("p (s t) -> p s t", t=4)
                    S.op("dve", lambda e, dc=dc, dcf=dc_, tv=tv: e.tensor_tensor(out=tv, in0=tv, in1=A.t[:, dcf, 0:NSEQ].unsqueeze(2).to_broadcast([128, NSEQ, 4]),
                                                                        op=ALU.mult), r=[tm, A], w=[tm])
                    S.op("dve", lambda e, dc=dc, dcf=dc_, tv=tv: e.tensor_tensor(out=dst.t[:, dc, doff:doff + ntok].rearrange("p (s t) -> p s t", t=4), in0=tv,
                                                                        in1=modT.t[:, B_mod + dcf, 0:NSEQ].unsqueeze(2).to_broadcast([128, NSEQ, 4]),
                                                                        op=ALU.add), r=[tm, modT], w=[dst])
                if post is not None:
                    post(dc_, dc)

        es_mix = contextlib.ExitStack()
        S.cur = es_mix
        alloc_wpool("B")
        alloc_tmps("B", with_x=True)
        xg = sb("xg", [128, 16, GT], F32)
        xn = sb("xn", [128, 16, GT], BF16)
        NTC = GT // 128
        qT = sb("qT", [128, 4, GT], F32)
        kT = sb("kT", [128, 4, GT], F32)
        ktm = sb("ktm", [128, NTC, 512], F32)
        vtm = sb("vtm", [128, NTC, 1024], BF16)
        Ltm = sb("Ltm", [128, NTC, 512], F32)
        alT = sb("alT", [16, GT], BF16)
        sgT = sb("sgT", [128, 8, GT], BF16)
        oT = sb("oT", [128, 8, GT], F32)
        oaT = sb("oaT", [128, 8, GT], BF16)
        obT = sb("obT", [128, 8, GT], BF16)
        Sst = [sb("Sst%d" % h, [128, 256], F32) for h in range(4)]
        Sbf = [sb("Sbf%d" % h, [128, 256], BF16) for h in range(4)]
        halo = sb("halo", [128, 8, 2], F32)
        for h in range(4):
            S.op("dve", lambda e, h=h: e.memset(Sst[h].t[:], 0.0), w=[Sst[h]])
            S.op("dve", lambda e, h=h: e.memset(Sbf[h].t[:], 0.0), w=[Sbf[h]])
        S.op("dve", lambda e: e.memset(halo.t[:], 0.0), w=[halo])
        expb = sb("expb", [128, 128], F32)
        expnb = sb("expnb", [128, 128], F32)
        qp = sb("qp", [128, 128], BF16)
        kp = sb("kp", [128, 128], BF16)
        kk = sb("kk", [128, 512], BF16)
        erev = sb("erev", [128, 512], F32)
        PTm = sb("PTm", [128, 128], BF16)
        gtmp = [sb("gtmp%d" % i, [128, 512], F32) for i in range(4)]
        cct = sb("cct", [128, 4, GT], F32)
        uext = sb("uext", [128, 4, GT + 2 * max(1, NSEQ)], F32)
        cnv = sb("cnv", [128, GT], F32)
        s0f = [sb("s0f%d" % i, [128, 256], F32) for i in range(2)]
        s0b = [sb("s0b%d" % i, [128, 256], BF16) for i in range(2)]
        snew = [sb("snew%d" % i, [128, 256], F32) for i in range(2)]
        ohs = sb("ohs", [128, NSEQ], F32)
        S.op("dve", lambda e: e.tensor_copy(out=ohs.t[:, :], in_=cs.t[:, 642:642 + NSEQ]), r=[cs], w=[ohs])
        scv = sb("scv", [128, 8, NSEQ * 2], F32)
        ctm = sb("ctm", [NSEQ * 2, CH], F32)
        S.dma("sp", lambda e: e.dma_start(out=ctm.t[:], in_=sconv), w=[ctm])
        p = S.ps()
        for blk in range(8):
            S.op("pe", lambda e, blk=blk, p=p: e.transpose(p.t[:, blk * NSEQ * 2:(blk + 1) * NSEQ * 2], ctm.t[0:NSEQ * 2, blk * 128:(blk + 1) * 128],
                                                         ident_f[0:NSEQ * 2, 0:NSEQ * 2]), r=[ctm] + CST, w=[p])
        S.op("dve", lambda e, p=p: e.tensor_copy(out=scv.t[:].rearrange("p a b -> p (a b)"), in_=p.t[:, 0:8 * NSEQ * 2]), r=[p], w=[scv])

        def proj_fm(W, wc0, ntok, t0, M=128):
            p = S.ps()
            for kc in range(16):
                S.op("pe", lambda e, kc=kc, p=p: e.matmul(p.t[0:M, 0:ntok], W.t[:, kc, wc0:wc0 + M], xn.t[:, kc, t0:t0 + ntok],
                                                        start=(kc == 0), stop=(kc == 15)), r=[W, xn], w=[p])
            return p

        def proj_tm(W, tc, n, ncols=512):
            p = S.ps()
            for kc in range(16):
                S.op("pe", lambda e, kc=kc, p=p: e.matmul(p.t[0:n, 0:ncols], xn.t[:, kc, tc * 128:tc * 128 + n], W.t[:, kc, 0:ncols],
                                                        start=(kc == 0), stop=(kc == 15)), r=[W, xn], w=[p])
            return p

        def mixer_group(kind, src, ntok, hoff, last_prefix=False):
            sample = kind == "samp"
            full = kind != "pre"
            ntc = (ntok + 127) // 128
            load_xT(src, ntok, xg, 0)
            rmsnorm_fm(xg, 0, ntok, xn, 0, A1, M_SH1, sample)
            if full:
                W = load_w(w_in, C_Q, 512)
                for h in range(4):
                    p = proj_fm(W, h * 128, ntok, 0)
                    S.op("act", lambda e, p=p, h=h: e.activation(out=qT.t[:, h, 0:ntok], in_=p.t[:, 0:ntok], func=AF.Copy, scale=HK ** -0.5),
                         r=[p], w=[qT])
            W = load_w(w_in, C_K, 512)
            if full:
                for h in range(4):
                    p = proj_fm(W, h * 128, ntok, 0)
                    S.op("dve", lambda e, p=p, h=h: e.tensor_copy(out=kT.t[:, h, 0:ntok], in_=p.t[:, 0:ntok]), r=[p], w=[kT])
            for tc in range(ntc):
                n = min(128, ntok - tc * 128)
                p = proj_tm(W, tc, n)
                S.op("act", lambda e, p=p, tc=tc, n=n: e.activation(out=ktm.t[0:n, tc, :], in_=p.t[0:n, :], func=AF.Copy), r=[p], w=[ktm])
            for vt in range(2):
                W = load_w(w_in, C_V + vt * 512, 512)
                for tc in range(ntc):
                    n = min(128, ntok - tc * 128)
                    p = proj_tm(W, tc, n)
                    S.op("dve" if vt else "act",
                         (lambda e, p=p, tc=tc, n=n, vt=vt: e.tensor_copy(out=vtm.t[0:n, tc, vt * 512:(vt + 1) * 512], in_=p.t[0:n, :])) if vt else
                         (lambda e, p=p, tc=tc, n=n, vt=vt: e.activation(out=vtm.t[0:n, tc, vt * 512:(vt + 1) * 512], in_=p.t[0:n, :], func=AF.Copy)),
                         r=[p], w=[vtm])
            W = load_w(w_in, C_AL, 16)
            p = proj_fm(W, 0, ntok, 0, M=16)
            S.op("dve", lambda e, p=p: e.tensor_copy(out=alT.t[:, 0:ntok], in_=p.t[0:16, 0:ntok]), r=[p], w=[alT])
            for tc in range(ntc):
                n = min(128, ntok - tc * 128)
                p = S.ps()
                S.op("pe", lambda e, p=p, tc=tc, n=n: e.matmul(p.t[0:n, :], alT.t[:, tc * 128:tc * 128 + n], wgk.t[:, :], start=True, stop=False),
                     r=[alT, wgk], w=[p])
                S.op("pe", lambda e, p=p, n=n: e.matmul(p.t[0:n, :], ones_b[0:1, 0:n], bgk.t[:, :], start=False, stop=True), r=[bgk] + CST, w=[p])
                S.op("act", lambda e, p=p, tc=tc, n=n: e.activation(out=Ltm.t[0:n, tc, :], in_=p.t[0:n, :], func=AF.Exp, scale=-1.0), r=[p], w=[Ltm])
                S.op("act", lambda e, tc=tc, n=n: e.activation(out=Ltm.t[0:n, tc, :], in_=Ltm.t[0:n, tc, :], func=AF.Ln, bias=one_c[0:n, :], scale=1.0),
                     r=[Ltm] + CST, w=[Ltm])
            if full:
                for gt in range(2):
                    W = load_w(w_in, C_G + gt * 512, 512)
                    for b4 in range(4):
                        p = proj_fm(W, b4 * 128, ntok, 0)
                        S.op("act", lambda e, p=p, gt=gt, b4=b4: e.activation(out=sgT.t[:, gt * 4 + b4, 0:ntok], in_=p.t[:, 0:ntok], func=AF.Silu),
                             r=[p], w=[sgT])
            for tc in range(ntc):
                n = min(128, ntok - tc * 128)
                mk_f = smask_f[0:n, 0:n] if sample else maskT_f[0:n, 0:n]
                mr_f = smrev_f[0:n, 0:n] if sample else mrev_f[0:n, 0:n]
                p = S.ps()
                S.op("pe", lambda e, p=p, tc=tc, n=n, mr_f=mr_f: e.matmul(p.t[0:n, :], mr_f, Ltm.t[0:n, tc, :], start=True, stop=True),
                     r=[Ltm] + CST, w=[p])
                S.op("act", lambda e, p=p, n=n: e.activation(out=erev.t[0:n, :], in_=p.t[0:n, :], func=AF.Exp, scale=-1.0 / 16), r=[p], w=[erev])
                S.op("dve", lambda e, tc=tc, n=n: e.tensor_tensor(out=kk.t[0:n, :], in0=ktm.t[0:n, tc, :], in1=erev.t[0:n, :], op=ALU.mult),
                     r=[ktm, erev], w=[kk])
                for h in range(4):
                    pb = S.ps()
                    S.op("pe", lambda e, pb=pb, tc=tc, n=n, h=h, mk_f=mk_f: e.matmul(pb.t[:, 0:n], Ltm.t[0:n, tc, h * 128:(h + 1) * 128], mk_f,
                                                                                    start=True, stop=True), r=[Ltm] + CST, w=[pb])
                    S.op("act", lambda e, pb=pb, n=n: e.activation(out=expb.t[:, 0:n], in_=pb.t[:, 0:n], func=AF.Exp, scale=-1.0 / 16), r=[pb], w=[expb])
                    if full:
                        S.op("act", lambda e, pb=pb, n=n: e.activation(out=expnb.t[:, 0:n], in_=pb.t[:, 0:n], func=AF.Exp, scale=1.0 / 16), r=[pb], w=[expnb])
                        S.op("dve", lambda e, h=h, tc=tc, n=n: e.tensor_tensor(out=qp.t[:, 0:n], in0=qT.t[:, h, tc * 128:tc * 128 + n], in1=expb.t[:, 0:n],
                                                                               op=ALU.mult), r=[qT, expb], w=[qp])
                        S.op("dve", lambda e, h=h, tc=tc, n=n: e.tensor_tensor(out=kp.t[:, 0:n], in0=kT.t[:, h, tc * 128:tc * 128 + n], in1=expnb.t[:, 0:n],
                                                                               op=ALU.mult), r=[kT, expnb], w=[kp])
                        pp = S.ps()
                        S.op("pe", lambda e, pp=pp, n=n: e.matmul(pp.t[0:n, 0:n], kp.t[:, 0:n], qp.t[:, 0:n], start=True, stop=True), r=[kp, qp], w=[pp])
                        S.op("dve", lambda e, pp=pp, n=n, mk_f=mk_f: e.tensor_tensor(out=PTm.t[0:n, 0:n], in0=pp.t[0:n, 0:n], in1=mk_f, op=ALU.mult),
                             r=[pp] + CST, w=[PTm])
                        if not sample:
                            po = S.ps()
                            for eb in range(2):
                                S.op("pe", lambda e, po=po, eb=eb, n=n, tc=tc, h=h: e.matmul(po.t[:, eb * 128:eb * 128 + n],
                                                                                            vtm.t[0:n, tc, h * 256 + eb * 128:h * 256 + (eb + 1) * 128],
                                                                                            PTm.t[0:n, 0:n], start=True, stop=False), r=[vtm, PTm], w=[po])
                                S.op("pe", lambda e, po=po, eb=eb, n=n, h=h: e.matmul(po.t[:, eb * 128:eb * 128 + n], Sbf[h].t[:, eb * 128:(eb + 1) * 128],
                                                                                     qp.t[:, 0:n], start=False, stop=True), r=[Sbf[h], qp], w=[po])
                            S.op("act", lambda e, po=po, h=h, tc=tc, n=n: e.activation(out=oT.t[:, 2 * h:2 * h + 2, tc * 128:tc * 128 + n],
                                                                                       in_=po.t[:, 0:256].rearrange("p (a b) -> p a b", a=2)[:, :, 0:n],
                                                                                       func=AF.Copy), r=[po], w=[oT])
                    if not sample:
                        pd = S.ps()
                        S.op("pe", lambda e, pd=pd, n=n, tc=tc, h=h: e.matmul(pd.t[:, 0:256], kk.t[0:n, h * 128:(h + 1) * 128], vtm.t[0:n, tc, h * 256:(h + 1) * 256],
                                                                             start=True, stop=True), r=[kk, vtm], w=[pd])
                        S.op("dve", lambda e, pd=pd, h=h, n=n: e.scalar_tensor_tensor(out=Sst[h].t[:, :], in0=Sst[h].t[:, :], scalar=expb.t[:, n - 1:n], in1=pd.t[:, 0:256],
                                                                                      op0=ALU.mult, op1=ALU.add), r=[Sst[h], expb, pd], w=[Sst[h]])
                        S.op("act", lambda e, h=h: e.activation(out=Sbf[h].t[:, :], in_=Sst[h].t[:, :], func=AF.Copy), r=[Sst[h]], w=[Sbf[h]])
                    else:
                        for s in range(NSEQ):
                            i2 = (s * 4 + h) % 2
                            S.dma("sp", lambda e, s=s, h=h, i2=i2: e.dma_start(out=s0f[i2].t[:, :], in_=sgla[s, h]), w=[s0f[i2]])
                            S.op("act", lambda e, i2=i2: e.activation(out=s0b[i2].t[:, :], in_=s0f[i2].t[:, :], func=AF.Copy), r=[s0f[i2]], w=[s0b[i2]])
                            po = S.ps()
                            for eb in range(2):
                                S.op("pe", lambda e, po=po, eb=eb, n=n, h=h, s=s: e.matmul(po.t[:, eb * 4:eb * 4 + 4],
                                                                                          vtm.t[0:n, 0, h * 256 + eb * 128:h * 256 + (eb + 1) * 128],
                                                                                          PTm.t[0:n, s * 4:s * 4 + 4], start=True, stop=False), r=[vtm, PTm], w=[po])
                                S.op("pe", lambda e, po=po, eb=eb, i2=i2, s=s: e.matmul(po.t[:, eb * 4:eb * 4 + 4], s0b[i2].t[:, eb * 128:(eb + 1) * 128],
                                                                                       qp.t[:, s * 4:s * 4 + 4], start=False, stop=True), r=[s0b[i2], qp], w=[po])
                            S.op("act", lambda e, po=po, h=h, s=s: e.activation(out=oT.t[:, 2 * h:2 * h + 2, s * 4:s * 4 + 4],
                                                                                in_=po.t[:, 0:8].rearrange("p (a b) -> p a b", a=2), func=AF.Copy), r=[po], w=[oT])
                            S.op("dve", lambda e, s=s, h=h, n=n: e.tensor_scalar(out=kp.t[0:n, :], in0=kk.t[0:n, h * 128:(h + 1) * 128], scalar1=ohs.t[0:n, s:s + 1],
                                                                               scalar2=None, op0=ALU.mult), r=[kk, ohs], w=[kp])
                            pd = S.ps()
                            S.op("pe", lambda e, pd=pd, n=n, h=h: e.matmul(pd.t[:, 0:256], kp.t[0:n, :], vtm.t[0:n, 0, h * 256:(h + 1) * 256], start=True, stop=True),
                                 r=[kp, vtm], w=[pd])
                            S.op("dve", lambda e, pd=pd, i2=i2, s=s: e.scalar_tensor_tensor(out=snew[i2].t[:, :], in0=s0f[i2].t[:, :], scalar=expb.t[:, s * 4 + 3:s * 4 + 4],
                                                                                           in1=pd.t[:, 0:256], op0=ALU.mult, op1=ALU.add),
                                 r=[s0f[i2], expb, pd], w=[snew[i2]])
                            S.dma("sp", lambda e, s=s, h=h, i2=i2: e.dma_start(out=gla_s[s, h], in_=snew[i2].t[:, :]), r=[snew[i2]])
            if last_prefix:
                for h in range(4):
                    S.op("dve", lambda e, h=h: e.tensor_scalar(out=Sst[h].t[:, :], in0=Sst[h].t[:, :], scalar1=pfl.t[:, 0:1], scalar2=None, op0=ALU.mult),
                         r=[Sst[h], pfl], w=[Sst[h]])
                    S.op("act", lambda e, h=h: e.activation(out=Sbf[h].t[:, :], in_=Sst[h].t[:, :], func=AF.Copy), r=[Sst[h]], w=[Sbf[h]])
            rstd = rstd_l[0]
            if full:
                for h in range(4):
                    p = S.ps()
                    for eb in range(2):
                        sq = ntmp[eb]
                        S.op("act", lambda e, sq=sq, h=h, eb=eb: e.activation(out=sq.t[:, 0:ntok], in_=oT.t[:, 2 * h + eb, 0:ntok], func=AF.Square), r=[oT], w=[sq])
                        S.op("pe", lambda e, sq=sq, p=p, eb=eb: e.matmul(p.t[:, 0:ntok], ones_f, sq.t[:, 0:ntok], start=(eb == 0), stop=(eb == 1)),
                             r=[sq] + CST, w=[p])
                    S.op("act", lambda e, p=p: e.activation(out=rstd.t[:, 0:ntok], in_=p.t[:, 0:ntok], func=AF.Sqrt, bias=eps_c, scale=1.0 / HV),
                         r=[p] + CST, w=[rstd])
                    S.op("dve", lambda e: e.reciprocal(out=rstd.t[:, 0:ntok], in_=rstd.t[:, 0:ntok]), r=[rstd], w=[rstd])
                    for eb in range(2):
                        tm = gtmp[eb]
                        S.op("dve", lambda e, tm=tm, h=h, eb=eb: e.tensor_tensor(out=tm.t[:, 0:ntok], in0=oT.t[:, 2 * h + eb, 0:ntok], in1=rstd.t[:, 0:ntok], op=ALU.mult),
                             r=[oT, rstd], w=[tm])
                        S.op("dve", lambda e, tm=tm, h=h, eb=eb: e.scalar_tensor_tensor(out=oaT.t[:, 2 * h + eb, 0:ntok], in0=tm.t[:, 0:ntok], scalar=gnT.t[:, eb:eb + 1],
                                                                                       in1=sgT.t[:, 2 * h + eb, 0:ntok], op0=ALU.mult, op1=ALU.mult),
                             r=[tm, gnT, sgT], w=[oaT])
            if full or last_prefix:
                for half in range(2):
                    if sample:
                        ue = uext.t[:, :, 0:NSEQ * 6].rearrange("p b (s t) -> p b s t", t=6)
                        for b4 in range(4):
                            S.op("dve", lambda e, b4=b4, half=half, ue=ue: e.tensor_copy(out=ue[:, b4, :, 0:2],
                                                                                        in_=scv.t[:, half * 4 + b4, :].rearrange("p (s t) -> p s t", t=2)),
                                 r=[scv], w=[uext])
                    elif full:
                        S.op("dve", lambda e, half=half: e.tensor_copy(out=uext.t[:, :, 0:2], in_=halo.t[:, half * 4:half * 4 + 4, :]), r=[halo], w=[uext])
                    t0 = 0 if full else ntok - 2
                    nn = ntok - t0
                    W = load_w(w_in, C_CC + half * 512, 512)
                    for b4 in range(4):
                        p = proj_fm(W, b4 * 128, nn, t0)
                        S.op("act", lambda e, p=p, b4=b4, nn=nn: e.activation(out=cct.t[:, b4, 0:nn], in_=p.t[:, 0:nn], func=AF.Copy), r=[p], w=[cct])
                    W = load_w(w_in, C_CHH + half * 512, 512)
                    for b4 in range(4):
                        p = proj_fm(W, b4 * 128, nn, t0)
                        if sample:
                            ue = uext.t[:, :, 0:NSEQ * 6].rearrange("p b (s t) -> p b s t", t=6)
                            S.op("dve", lambda e, p=p, b4=b4, ue=ue: e.tensor_tensor(out=ue[:, b4, :, 2:6], in0=p.t[:, 0:ntok].rearrange("p (s t) -> p s t", t=4),
                                                                                    in1=cct.t[:, b4, 0:ntok].rearrange("p (s t) -> p s t", t=4), op=ALU.mult),
                                 r=[p, cct], w=[uext])
                        elif full:
                            S.op("dve", lambda e, p=p, b4=b4: e.tensor_tensor(out=uext.t[:, b4, 2:2 + ntok], in0=p.t[:, 0:ntok], in1=cct.t[:, b4, 0:ntok], op=ALU.mult),
                                 r=[p, cct], w=[uext])
                        else:
                            S.op("dve", lambda e, p=p, b4=b4, half=half: e.scalar_tensor_tensor(out=halo.t[:, half * 4 + b4, :], in0=p.t[:, 0:2], scalar=pfl.t[:, 0:1],
                                                                                               in1=cct.t[:, b4, 0:2], op0=ALU.mult, op1=ALU.mult),
                                 r=[p, pfl, cct], w=[halo])
                    if not full:
                        continue
                    W = load_w(w_in, C_CB + half * 512, 512)
                    for b4 in range(4):
                        blk = half * 4 + b4
                        p = proj_fm(W, b4 * 128, ntok, 0)
                        if sample:
                            ue = uext.t[:, :, 0:NSEQ * 6].rearrange("p b (s t) -> p b s t", t=6)
                            cv3 = cnv.t[:, 0:ntok].rearrange("p (s t) -> p s t", t=4)
                            u0, u1, u2 = ue[:, b4, :, 0:4], ue[:, b4, :, 1:5], ue[:, b4, :, 2:6]
                        else:
                            cv3 = cnv.t[:, 0:ntok]
                            u0, u1, u2 = uext.t[:, b4, 0:ntok], uext.t[:, b4, 1:1 + ntok], uext.t[:, b4, 2:2 + ntok]
                        S.op("dve", lambda e, cv3=cv3, u0=u0, blk=blk: e.tensor_scalar(out=cv3, in0=u0, scalar1=wcT.t[:, blk:blk + 1], scalar2=None, op0=ALU.mult),
                             r=[uext, wcT], w=[cnv])
                        S.op("dve", lambda e, cv3=cv3, u1=u1, blk=blk: e.scalar_tensor_tensor(out=cv3, in0=u1, scalar=wcT.t[:, 8 + blk:9 + blk], in1=cv3,
                                                                                             op0=ALU.mult, op1=ALU.add), r=[uext, wcT, cnv], w=[cnv])
                        S.op("dve", lambda e, cv3=cv3, u2=u2, blk=blk: e.scalar_tensor_tensor(out=cv3, in0=u2, scalar=wcT.t[:, 16 + blk:17 + blk], in1=cv3,
                                                                                             op0=ALU.mult, op1=ALU.add), r=[uext, wcT, cnv], w=[cnv])
                        S.op("dve", lambda e, p=p, blk=blk: e.tensor_tensor(out=obT.t[:, blk, 0:ntok], in0=p.t[:, 0:ntok], in1=cnv.t[:, 0:ntok], op=ALU.mult),
                             r=[p, cnv], w=[obT])
                    if sample:
                        ue = uext.t[:, :, 0:NSEQ * 6].rearrange("p b (s t) -> p b s t", t=6)
                        for b4 in range(4):
                            S.op("dve", lambda e, b4=b4, half=half, ue=ue: e.tensor_copy(out=scv.t[:, half * 4 + b4, :].rearrange("p (s t) -> p s t", t=2),
                                                                                        in_=ue[:, b4, :, 4:6]), r=[uext], w=[scv])
                    else:
                        S.op("dve", lambda e, half=half: e.tensor_copy(out=halo.t[:, half * 4:half * 4 + 4, :], in_=uext.t[:, :, ntok:ntok + 2]), r=[uext], w=[halo])
            if not full:
                return
            for t4 in range(4):
                Wo = load_w(w_out, t4 * 512, 512)
                Wa = load_w(w_in, C_GA + t4 * 512, 512)
                Wb = load_w(w_in, C_GB + t4 * 512, 512)
                for sbk in range(4):
                    j = t4 * 4 + sbk
                    pA = S.ps()
                    for kc in range(8):
                        S.op("pe", lambda e, kc=kc, pA=pA, Wo=Wo, sbk=sbk: e.matmul(pA.t[:, 0:ntok], Wo.t[:, kc, sbk * 128:(sbk + 1) * 128], oaT.t[:, kc, 0:ntok],
                                                                                   start=(kc == 0), stop=(kc == 7)), r=[Wo, oaT], w=[pA])
                    pB = S.ps()
                    for kc in range(8):
                        S.op("pe", lambda e, kc=kc, pB=pB, Wo=Wo, sbk=sbk: e.matmul(pB.t[:, 0:ntok], Wo.t[:, 8 + kc, sbk * 128:(sbk + 1) * 128], obT.t[:, kc, 0:ntok],
                                                                                   start=(kc == 0), stop=(kc == 7)), r=[Wo, obT], w=[pB])
                    pGa = proj_fm(Wa, sbk * 128, ntok, 0)
                    pGb = proj_fm(Wb, sbk * 128, ntok, 0)
                    S.op("act", lambda e, pGa=pGa: e.activation(out=gtmp[0].t[:, 0:ntok], in_=pGa.t[:, 0:ntok], func=AF.Sigmoid), r=[pGa], w=[gtmp[0]])
                    S.op("act", lambda e, pGb=pGb: e.activation(out=gtmp[1].t[:, 0:ntok], in_=pGb.t[:, 0:ntok], func=AF.Sigmoid), r=[pGb], w=[gtmp[1]])
                    S.op("dve", lambda e, pA=pA: e.tensor_tensor(out=gtmp[0].t[:, 0:ntok], in0=pA.t[:, 0:ntok], in1=gtmp[0].t[:, 0:ntok], op=ALU.mult),
                         r=[pA, gtmp[0]], w=[gtmp[0]])
                    S.op("dve", lambda e, pB=pB: e.tensor_tensor(out=gtmp[1].t[:, 0:ntok], in0=pB.t[:, 0:ntok], in1=gtmp[1].t[:, 0:ntok], op=ALU.mult),
                         r=[pB, gtmp[1]], w=[gtmp[1]])
                    S.op("dve", lambda e: e.tensor_tensor(out=gtmp[0].t[:, 0:ntok], in0=gtmp[0].t[:, 0:ntok], in1=gtmp[1].t[:, 0:ntok], op=ALU.add),
                         r=[gtmp[0], gtmp[1]], w=[gtmp[0]])
                    if not sample:
                        S.op("dve", lambda e, j=j: e.scalar_tensor_tensor(out=xg.t[:, j, 0:ntok], in0=gtmp[0].t[:, 0:ntok], scalar=modT.t[:, M_G1 + j, NSEQ:NSEQ + 1],
                                                                         in1=xg.t[:, j, 0:ntok], op0=ALU.mult, op1=ALU.add), r=[gtmp[0], modT, xg], w=[xg])
                    else:
                        g3 = gtmp[0].t[:, 0:ntok].rearrange("p (s t) -> p s t", t=4)
                        S.op("dve", lambda e, j=j, g3=g3: e.tensor_tensor(out=g3, in0=g3, in1=modT.t[:, M_G1 + j, 0:NSEQ].unsqueeze(2).to_broadcast([128, NSEQ, 4]),
                                                                         op=ALU.mult), r=[gtmp[0], modT], w=[gtmp[0]])
                        S.op("dve", lambda e, j=j: e.tensor_tensor(out=xg.t[:, j, 0:ntok], in0=gtmp[0].t[:, 0:ntok], in1=xg.t[:, j, 0:ntok], op=ALU.add),
                             r=[gtmp[0], xg], w=[xg])
            S.dma("sp", lambda e: e.dma_start(out=hscr[:, :, hoff:hoff + ntok].rearrange("c p t -> p c t"), in_=xg.t[:, :, 0:ntok]), r=[xg], w=[hbuf], sem=hsem)

        hsem = S.new_dsem("hscr")
        hbuf = Buf("hscr")

        npg = NPRE // GT
        for g in range(npg):
            mixer_group("pre", xpre[g * GT:(g + 1) * GT, :], GT, 0, last_prefix=(g == npg - 1))
        for g in range(NP // GT):
            mixer_group("main", xp[g * GT:(g + 1) * GT, :], GT, g * GT)
        for h in range(4):
            S.dma("sp", lambda e, h=h: e.dma_start(out=gla_p[h], in_=Sst[h].t[:, :]), r=[Sst[h]])
        S.dma("sp", lambda e: [e.dma_start(out=conv_p[:, b * 128:(b + 1) * 128].rearrange("t p -> p t"), in_=halo.t[:, b, :], allow_slow_non_contiguous=True)
                               for b in range(8)], r=[halo], n=8)
        mixer_group("samp", xs, NS, NP)
        cso = sb("cso", [NSEQ * 2, CH], F32)
        for hb in range(2):
            p = S.ps()
            for b4 in range(4):
                S.op("pe", lambda e, p=p, b4=b4, hb=hb: e.transpose(p.t[0:NSEQ * 2, b4 * 128:(b4 + 1) * 128], scv.t[:, hb * 4 + b4, :], ident_f), r=[scv] + CST, w=[p])
            S.op("dve", lambda e, p=p, hb=hb: e.tensor_copy(out=cso.t[:, hb * 512:(hb + 1) * 512], in_=p.t[0:NSEQ * 2, :]), r=[p], w=[cso])
        S.dma("sp", lambda e: e.dma_start(out=conv_s, in_=cso.t[:, :]), r=[cso])

        S.barrier()
        es_mix.close()
        S.cur = es
        NCH = (NT + 127) // 128
        NBLK = (CAP + 127) // 128
        BLKS = [(b * 128, min(128, CAP - b * 128)) for b in range(NBLK)]
        oscr = nc.dram_tensor("oscr", [E, 128, NBLK, D], BF16, kind="Internal").ap()
        obuf = Buf("oscr")
        iota_f = cs.t[:, 1152:1152 + CAP]
        mstrict_f = cs.t[:, 1024:1152]
        osc_sem = S.new_dsem("oscr")
        gwT = sb("gwT", [E, NT], F32)
        rkT = sb("rkT", [E, NT], F32)
        esel = sb("esel", [E, 128], F32)
        es_h = contextlib.ExitStack()
        S.cur = es_h
        alloc_wpool("C", 2)
        hntm = sb("hntm", [128, NCH, D], BF16)
        gwtm = sb("gwtm", [128, NCH, E], F32)
        mktm = sb("mktm", [128, NCH, E], F32)
        rktm = sb("rktm", [128, NCH, E], F32)
        top8 = sb("top8", [128, 8], F32)
        nmx = sb("nmx", [128, 1], F32)
        ssum = sb("ssum", [128, 1], F32)
        wr_f = sb("wr_f", [128, 16, E], F32)
        S.dma("sp", lambda e: e.dma_start(out=wr_f.t[:], in_=w_router.rearrange("(kc p) n -> p kc n", p=128)), w=[wr_f])
        wr = sb("wr", [128, 16, E], BF16)
        S.op("dve", lambda e: e.tensor_copy(out=wr.t[:], in_=wr_f.t[:]), r=[wr_f], w=[wr])
        br_f = sb("br_f", [1, E], F32)
        S.dma("sp", lambda e: e.dma_start(out=br_f.t[:], in_=b_router), w=[br_f])
        br = sb("br", [1, E], BF16)
        S.op("dve", lambda e: e.tensor_copy(out=br.t[:], in_=br_f.t[:]), r=[br_f], w=[br])
        S.op("dve", lambda e: e.memset(mktm.t[:], 0.0), w=[mktm])

        es_m1 = contextlib.ExitStack()
        S.cur = es_m1
        alloc_tmps("M1")
        hTt = sb("hTt", [128, 16, 512], F32)
        hnT = sb("hnT", [128, 16, 512], BF16)
        hnf = sb("hnf", [128, 2, 512], F32)
        lg = sb("lg", [128, E], F32)
        tiles = [(c, min(512, NP - c), False) for c in range(0, NP, 512)] + [(NP, NS, True)]
        for (t0, nn, smp) in tiles:
            S.dma("sp", lambda e, t0=t0, nn=nn: e.dma_start(out=hTt.t[:, :, 0:nn], in_=hscr[:, :, t0:t0 + nn].rearrange("c p t -> p c t")), w=[hTt], r=[hbuf])

            def post(dcf, dc, t0=t0, nn=nn):
                S.op("act", lambda e: e.activation(out=hnT.t[:, dcf, 0:nn], in_=hnf.t[:, dc, 0:nn], func=AF.Copy), r=[hnf], w=[hnT])
                p = S.ps()
                nsub = (nn + 127) // 128
                for c4 in range(nsub):
                    n = min(128, nn - c4 * 128)
                    S.op("pe", lambda e, p=p, c4=c4, n=n: e.transpose(p.t[0:n, c4 * 128:(c4 + 1) * 128], hnf.t[:, dc, c4 * 128:c4 * 128 + n], ident_f),
                         r=[hnf] + CST, w=[p])
                tc0 = t0 // 128
                if nn % 128 == 0:
                    S.op("dve", lambda e, p=p: e.tensor_copy(out=hntm.t[:, tc0:tc0 + nsub, dcf * 128:(dcf + 1) * 128],
                                                             in_=p.t[:, 0:nsub * 128].rearrange("p (c f) -> p c f", f=128)), r=[p], w=[hntm])
                else:
                    assert nsub == 1
                    S.op("dve", lambda e, p=p: e.tensor_copy(out=hntm.t[0:nn, tc0, dcf * 128:(dcf + 1) * 128], in_=p.t[0:nn, 0:128]), r=[p], w=[hntm])
            rmsnorm_fm(hTt, 0, nn, hnf, 0, A2, M_SH2, smp, post=post)
            for c0 in range(0, nn, 128):
                n = min(128, nn - c0)
                ch = (t0 + c0) // 128
                p = S.ps()
                for kc in range(16):
                    S.op("pe", lambda e, p=p, kc=kc, c0=c0, n=n: e.matmul(p.t[0:n, 0:E], hnT.t[:, kc, c0:c0 + n], wr.t[:, kc, :], start=(kc == 0), stop=False),
                         r=[hnT, wr], w=[p])
                S.op("pe", lambda e, p=p, n=n: e.matmul(p.t[0:n, 0:E], ones_b[0:1, 0:n], br.t[:, :], start=False, stop=True), r=[br] + CST, w=[p])
                S.op("dve", lambda e, p=p, n=n: e.tensor_copy(out=lg.t[0:n, :], in_=p.t[0:n, 0:E]), r=[p], w=[lg])
                S.op("dve", lambda e, n=n: e.max(out=top8.t[0:n, :], in_=lg.t[0:n, :]), r=[lg], w=[top8])
                S.op("dve", lambda e, n=n, ch=ch: e.tensor_scalar(out=mktm.t[0:n, ch, :], in0=lg.t[0:n, :], scalar1=top8.t[0:n, TOPK - 1:TOPK], scalar2=None, op0=ALU.is_ge),
                     r=[lg, top8], w=[mktm])
                S.op("dve", lambda e, n=n: e.tensor_scalar(out=nmx.t[0:n, :], in0=top8.t[0:n, 0:1], scalar1=-1.0, scalar2=None, op0=ALU.mult), r=[top8], w=[nmx])
                S.op("act", lambda e, n=n: e.activation(out=lg.t[0:n, :], in_=lg.t[0:n, :], func=AF.Exp, bias=nmx.t[0:n, :], scale=1.0), r=[lg, nmx], w=[lg])
                S.op("dve", lambda e, n=n, ch=ch: e.tensor_tensor(out=lg.t[0:n, :], in0=lg.t[0:n, :], in1=mktm.t[0:n, ch, :], op=ALU.mult), r=[lg, mktm], w=[lg])
                S.op("dve", lambda e, n=n: e.reduce_sum(out=ssum.t[0:n, :], in_=lg.t[0:n, :], axis=mybir.AxisListType.X), r=[lg], w=[ssum])
                S.op("dve", lambda e, n=n: e.reciprocal(out=ssum.t[0:n, :], in_=ssum.t[0:n, :]), r=[ssum], w=[ssum])
                S.op("dve", lambda e, n=n, ch=ch: e.tensor_scalar(out=gwtm.t[0:n, ch, :], in0=lg.t[0:n, :], scalar1=ssum.t[0:n, 0:1], scalar2=None, op0=ALU.mult),
                     r=[lg, ssum], w=[gwtm])
                p2 = S.ps()
                S.op("pe", lambda e, p2=p2, n=n, ch=ch: e.transpose(p2.t[0:E, 0:n], gwtm.t[0:n, ch, :], ident_f[0:n, 0:n]), r=[gwtm] + CST, w=[p2])
                S.op("dve", lambda e, p2=p2, n=n, ch=ch: e.tensor_copy(out=gwT.t[:, ch * 128:ch * 128 + n], in_=p2.t[0:E, 0:n]), r=[p2], w=[gwT])
        SCH = NCH - 1
        for ch in range(NCH):
            n = min(128, NT - ch * 128)
            p = S.ps()
            prev = [] if ch == SCH else [SCH] + list(range(ch))
            S.op("pe", lambda e, p=p, n=n, ch=ch, prev=prev: e.matmul(p.t[0:n, 0:E], mstrict_f[0:n, 0:n], mktm.t[0:n, ch, :], start=True, stop=(len(prev) == 0)),
                 r=[mktm] + CST, w=[p])
            for i2, c2 in enumerate(prev):
                k2 = min(128, NT - c2 * 128)
                S.op("pe", lambda e, p=p, n=n, c2=c2, k2=k2, i2=i2, prev=prev: e.matmul(p.t[0:n, 0:E], ones_f[0:k2, 0:n], mktm.t[0:k2, c2, :], start=False,
                                                                                      stop=(i2 == len(prev) - 1)), r=[mktm] + CST, w=[p])
            S.op("dve", lambda e, p=p, n=n, ch=ch: e.scalar_tensor_tensor(out=rktm.t[0:n, ch, :], in0=p.t[0:n, 0:E], scalar=1.0, in1=mktm.t[0:n, ch, :],
                                                                         op0=ALU.add, op1=ALU.mult), r=[p, mktm], w=[rktm])
            S.op("dve", lambda e, n=n, ch=ch: e.tensor_scalar(out=rktm.t[0:n, ch, :], in0=rktm.t[0:n, ch, :], scalar1=-1.0, scalar2=None, op0=ALU.add),
                 r=[rktm], w=[rktm])
            p2 = S.ps()
            S.op("pe", lambda e, p2=p2, n=n, ch=ch: e.transpose(p2.t[0:E, 0:n], rktm.t[0:n, ch, :], ident_f[0:n, 0:n]), r=[rktm] + CST, w=[p2])
            S.op("dve", lambda e, p2=p2, n=n, ch=ch: e.tensor_copy(out=rkT.t[:, ch * 128:ch * 128 + n], in_=p2.t[0:E, 0:n]), r=[p2], w=[rkT])
        S.barrier()
        es_m1.close()

        es_m2 = contextlib.ExitStack()
        S.cur = es_m2
        sel = sb("sel", [128, NCH, CAP], BF16)
        xbT = sb("xbT", [128, 16, CAP], BF16)
        actT = sb("actT", [128, FC, CAP], BF16)
        oute = [sb("oute%d" % i, [128, NBLK, D], BF16) for i in range(1)]
        bupT = sb("bupT", [128, 2 * FC], F32)
        bstg = sb("bstg", [128, 128], F32)
        mt = [sb("mt%d" % i, [128, CAP], F32) for i in range(3)]
        S.op("dve", lambda e: e.memset(oute[0].t[:], 0.0), w=[oute[0]])
        for ex in range(E):
            S.dma("sp", lambda e, ex=ex: e.dma_start(out=bstg.t[0:2 * FC, :], in_=b_up[ex * 2 * FC:(ex + 1) * 2 * FC, :]), w=[bstg])
            p = S.ps()
            S.op("pe", lambda e, p=p: e.transpose(p.t[:, 0:2 * FC], bstg.t[0:2 * FC, :], ident_f[0:2 * FC, 0:2 * FC]), r=[bstg] + CST, w=[p])
            S.op("dve", lambda e, p=p: e.tensor_copy(out=bupT.t[:, :], in_=p.t[:, 0:2 * FC]), r=[p], w=[bupT])
            for ch in range(NCH):
                n = min(128, NT - ch * 128)
                S.op("dve", lambda e, n=n, ch=ch, ex=ex: e.tensor_scalar(out=sel.t[0:n, ch, :], in0=iota_f[0:n, :], scalar1=rktm.t[0:n, ch, ex:ex + 1], scalar2=None,
                                                                        op0=ALU.is_equal), r=[rktm] + CST, w=[sel])
            for f in range(16):
                p = S.ps()
                for ch in range(NCH):
                    n = min(128, NT - ch * 128)
                    S.op("pe", lambda e, p=p, f=f, ch=ch, n=n: e.matmul(p.t[:, 0:CAP], hntm.t[0:n, ch, f * 128:(f + 1) * 128], sel.t[0:n, ch, :],
                                                                       start=(ch == 0), stop=(ch == NCH - 1)), r=[hntm, sel], w=[p])
                S.op("act" if f % 2 else "dve",
                     (lambda e, p=p, f=f: e.activation(out=xbT.t[:, f, :], in_=p.t[:, 0:CAP], func=AF.Copy)) if f % 2 else
                     (lambda e, p=p, f=f: e.tensor_copy(out=xbT.t[:, f, :], in_=p.t[:, 0:CAP])), r=[p], w=[xbT])
            for f2 in range(0, FC, 2):
                nb = min(2, FC - f2)
                W = wtile()
                vsrc = w_up[ex].rearrange("(kc p) n -> p kc n", p=128)
                S.dma("pool", lambda e, W=W, f2=f2, nb=nb, vsrc=vsrc: [
                    e.dma_start(out=W.t[:, :, 0:nb * 128], in_=vsrc[:, :, f2 * 128:(f2 + nb) * 128]),
                    e.dma_start(out=W.t[:, :, 256:256 + nb * 128], in_=vsrc[:, :, FF + f2 * 128:FF + (f2 + nb) * 128])], w=[W], n=2)
                for fb in range(nb):
                    f = f2 + fb
                    pg = S.ps()
                    for kc in range(16):
                        S.op("pe", lambda e, pg=pg, kc=kc, W=W, fb=fb: e.matmul(pg.t[:, 0:CAP], W.t[:, kc, fb * 128:(fb + 1) * 128], xbT.t[:, kc, :],
                                                                               start=(kc == 0), stop=(kc == 15)), r=[W, xbT], w=[pg])
                    pl = S.ps()
                    for kc in range(16):
                        S.op("pe", lambda e, pl=pl, kc=kc, W=W, fb=fb: e.matmul(pl.t[:, 0:CAP], W.t[:, kc, 256 + fb * 128:256 + (fb + 1) * 128], xbT.t[:, kc, :],
                                                                               start=(kc == 0), stop=(kc == 15)), r=[W, xbT], w=[pl])
                    S.op("dve", lambda e, pg=pg, f=f: e.tensor_scalar(out=mt[0].t[:, :], in0=pg.t[:, 0:CAP], scalar1=bupT.t[:, f:f + 1], scalar2=LIMIT,
                                                                     op0=ALU.add, op1=ALU.min), r=[pg, bupT], w=[mt[0]])
                    S.op("act", lambda e: e.activation(out=mt[1].t[:, :], in_=mt[0].t[:, :], func=AF.Sigmoid, scale=ALPHA), r=[mt[0]], w=[mt[1]])
                    S.op("dve", lambda e, pl=pl, f=f: e.tensor_scalar(out=mt[2].t[:, :], in0=pl.t[:, 0:CAP], scalar1=bupT.t[:, FC + f:FC + f + 1], scalar2=LIMIT,
                                                                     op0=ALU.add, op1=ALU.min), r=[pl, bupT], w=[mt[2]])
                    S.op("dve", lambda e: e.tensor_scalar(out=mt[2].t[:, :], in0=mt[2].t[:, :], scalar1=-LIMIT, scalar2=1.0, op0=ALU.max, op1=ALU.add),
                         r=[mt[2]], w=[mt[2]])
                    S.op("dve", lambda e: e.tensor_tensor(out=mt[0].t[:, :], in0=mt[0].t[:, :], in1=mt[1].t[:, :], op=ALU.mult), r=[mt[0], mt[1]], w=[mt[0]])
                    S.op("dve", lambda e, f=f: e.tensor_tensor(out=actT.t[:, f, :], in0=mt[0].t[:, :], in1=mt[2].t[:, :], op=ALU.mult), r=[mt[0], mt[2]], w=[actT])
            ot = oute[0]
            for t4 in range(4):
                W = wtile()
                vsrc = w_down[ex].rearrange("(kc p) n -> p kc n", p=128)
                S.dma("pool", lambda e, W=W, t4=t4, vsrc=vsrc: e.dma_start(out=W.t[:, 0:FC, :], in_=vsrc[:, :, t4 * 512:(t4 + 1) * 512]), w=[W])
                for blk, (b0, bn) in enumerate(BLKS):
                    py = S.ps()
                    for fc in range(FC):
                        S.op("pe", lambda e, py=py, fc=fc, W=W, b0=b0, bn=bn: e.matmul(py.t[0:bn, :], actT.t[:, fc, b0:b0 + bn], W.t[:, fc, :],
                                                                                     start=(fc == 0), stop=(fc == FC - 1)), r=[W, actT], w=[py])
                    S.op("act" if blk % 2 else "dve",
                         (lambda e, py=py, blk=blk, bn=bn, t4=t4, ot=ot: e.activation(out=ot.t[0:bn, blk, t4 * 512:(t4 + 1) * 512], in_=py.t[0:bn, :], func=AF.Copy)) if blk % 2 else
                         (lambda e, py=py, blk=blk, bn=bn, t4=t4, ot=ot: e.tensor_copy(out=ot.t[0:bn, blk, t4 * 512:(t4 + 1) * 512], in_=py.t[0:bn, :])), r=[py], w=[ot])
            S.dma("sp", lambda e, ex=ex, ot=ot: e.dma_start(out=oscr[ex], in_=ot.t[:, :, :]), r=[ot], w=[obuf], sem=osc_sem)
        S.barrier()
        es_m2.close()
        es_h.close()

        S.cur = es
        hT = sb("hT", [128, 16, NT], F32)
        oin = [sb("oin%d" % i, [128, NBLK, D], BF16) for i in range(2)]
        selT = [sb("selT%d" % i, [128, NBLK, NT], BF16) for i in range(1)]
        gwr = [sb("gwr%d" % i, [128, NT], F32) for i in range(1)]
        ct = [sb("ct%d" % i, [128, NS], F32) for i in range(2)]
        yo = sb("yo", [128, D], F32)
        bdn = sb("bdn", [E, D], F32)
        alloc_tmps("M3")
        S.dma("sp", lambda e: e.dma_start(out=hT.t[:, :, :], in_=hscr.rearrange("c p t -> p c t")), w=[hT], r=[hbuf])
        S.dma("sp", lambda e: e.dma_start(out=bdn.t[:, :], in_=b_down.rearrange("(e a) b -> e (a b)", a=16)), w=[bdn])
        ctiles = [(c, min(512, NP - c)) for c in range(0, NP, 512)] + [(NP, NS)]

        def accum(py, j, c0, nn, k):
            if c0 < NP:
                S.op("dve", lambda e: e.scalar_tensor_tensor(out=hT.t[:, j, c0:c0 + nn], in0=py.t[:, 0:nn], scalar=modT.t[:, M_G2 + j, NSEQ:NSEQ + 1],
                                                             in1=hT.t[:, j, c0:c0 + nn], op0=ALU.mult, op1=ALU.add), r=[py, modT, hT], w=[hT])
            else:
                cc_ = ct[k % 2]
                S.op("dve", lambda e: e.tensor_tensor(out=cc_.t[:, 0:nn].rearrange("p (s t) -> p s t", t=4), in0=py.t[:, 0:nn].rearrange("p (s t) -> p s t", t=4),
                                                      in1=modT.t[:, M_G2 + j, 0:NSEQ].unsqueeze(2).to_broadcast([128, NSEQ, 4]), op=ALU.mult),
                     r=[py, modT], w=[cc_])
                S.op("dve", lambda e: e.tensor_tensor(out=hT.t[:, j, c0:c0 + nn], in0=cc_.t[:, 0:nn], in1=hT.t[:, j, c0:c0 + nn], op=ALU.add),
                     r=[cc_, hT], w=[hT])

        for j in range(16):
            for (c0, nn) in ctiles:
                py = S.ps()
                S.op("pe", lambda e, py=py, j=j, c0=c0, nn=nn: e.matmul(py.t[:, 0:nn], bdn.t[:, j * 128:(j + 1) * 128], gwT.t[:, c0:c0 + nn], start=True, stop=True),
                     r=[bdn, gwT], w=[py])
                accum(py, j, c0, nn, j)
        for ex in range(E):
            oi, sT, gr = oin[ex % 2], selT[0], gwr[0]
            S.dma("sp", lambda e, ex=ex, oi=oi: e.dma_start(out=oi.t[:, :, :], in_=oscr[ex]), w=[oi], r=[obuf])
            S.op("dve", lambda e, ex=ex: e.tensor_copy(out=esel.t[:, :], in_=cs.t[0:E, ex:ex + 1].to_broadcast([E, 128])), r=[cs], w=[esel])
            for (c0, nn) in ctiles:
                p = S.ps()
                S.op("pe", lambda e, p=p, c0=c0, nn=nn: e.matmul(p.t[:, 0:nn], esel.t[:, :], gwT.t[:, c0:c0 + nn], start=True, stop=True), r=[esel, gwT], w=[p])
                S.op("act", lambda e, p=p, c0=c0, nn=nn, gr=gr: e.activation(out=gr.t[:, c0:c0 + nn], in_=p.t[:, 0:nn], func=AF.Copy), r=[p], w=[gr])
                p = S.ps()
                S.op("pe", lambda e, p=p, c0=c0, nn=nn: e.matmul(p.t[:, 0:nn], esel.t[:, :], rkT.t[:, c0:c0 + nn], start=True, stop=True), r=[esel, rkT], w=[p])
                for blk, (b0, bn) in enumerate(BLKS):
                    S.op("dve", lambda e, p=p, c0=c0, nn=nn, blk=blk, bn=bn, sT=sT, gr=gr: e.scalar_tensor_tensor(out=sT.t[0:bn, blk, c0:c0 + nn], in0=p.t[0:bn, 0:nn],
                                                                                                                 scalar=cs.t[0:bn, 960 + blk:961 + blk], in1=gr.t[0:bn, c0:c0 + nn],
                                                                                                                 op0=ALU.is_equal, op1=ALU.mult), r=[p, gr] + CST, w=[sT])
            for j in range(16):
                for (c0, nn) in ctiles:
                    py = S.ps()
                    for blk, (b0, bn) in enumerate(BLKS):
                        S.op("pe", lambda e, py=py, blk=blk, bn=bn, j=j, c0=c0, nn=nn, oi=oi, sT=sT: e.matmul(py.t[:, 0:nn], oi.t[0:bn, blk, j * 128:(j + 1) * 128], sT.t[0:bn, blk, c0:c0 + nn],
                                                                                                             start=(blk == 0), stop=(blk == NBLK - 1)), r=[oi, sT], w=[py])
                    accum(py, j, c0, nn, j)
        osem = S.new_dsem("yout")
        for (c0, nn) in ctiles:
            rmsnorm_fm(hT, c0, nn, hT, c0, nfT, None, False)
        for c0 in range(0, NT, 128):
            n = min(128, NT - c0)
            for q4 in range(4):
                p = S.ps()
                for i in range(4):
                    dc = q4 * 4 + i
                    S.op("pe", lambda e, p=p, i=i, dc=dc, c0=c0, n=n: e.transpose(p.t[0:n, i * 128:(i + 1) * 128], hT.t[:, dc, c0:c0 + n], ident_f), r=[hT] + CST, w=[p])
                S.op("act" if q4 % 2 else "dve",
                     (lambda e, p=p, q4=q4, n=n: e.activation(out=yo.t[0:n, q4 * 512:(q4 + 1) * 512], in_=p.t[0:n, :], func=AF.Copy)) if q4 % 2 else
                     (lambda e, p=p, q4=q4, n=n: e.tensor_copy(out=yo.t[0:n, q4 * 512:(q4 + 1) * 512], in_=p.t[0:n, :])), r=[p], w=[yo])
            if c0 < NP:
                S.dma("sp", lambda e, c0=c0, n=n: e.dma_start(out=y_p[c0:c0 + n, :], in_=yo.t[0:n, :]), r=[yo], sem=osem)
            else:
                S.dma("sp", lambda e, c0=c0, n=n: e.dma_start(out=y_s[c0 - NP:c0 - NP + n, :], in_=yo.t[0:n, :]), r=[yo], sem=osem)

        def fin(e):
            for d in S.dsems:
                if d[1] > 0:
                    e.wait_ge(d[0], d[1])
            return e.nop()
        S.op("sp", fin)

        S.assign()
        with nc.Block() as block:
            @block.tensor
            def _(e):
                S.run("pe", e)

            @block.vector
            def _(e):
                S.run("dve", e)

            @block.scalar
            def _(e):
                S.run("act", e)

            @block.gpsimd
            def _(e):
                S.run("pool", e)

            @block.sync
            def _(e):
                S.run("sp", e)
    return nc


def make_cst(NSEQ):
    c = np.zeros((128, 1664), np.float32)
    i = np.arange(128)
    c[:, 0:128] = np.eye(128, dtype=np.float32)
    c[:, 128:256] = (i[:, None] <= i[None, :]).astype(np.float32)
    c[:, 256:384] = (i[:, None] > i[None, :]).astype(np.float32)
    j = np.arange(64)
    same = (j[:, None] // 4) == (j[None, :] // 4)
    c[0:64, 384:448] = (same & (j[:, None] <= j[None, :])).astype(np.float32)
    c[0:64, 448:512] = (same & (j[:, None] > j[None, :])).astype(np.float32)
    c[:, 512:640] = 1.0
    c[:, 640] = EPS
    c[:, 641] = 1.0
    for s in range(NSEQ):
        c[s * 4:(s + 1) * 4, 642 + s] = 1.0
    c[:, 1152:1664] = np.arange(512, dtype=np.float32)[None, :]
    c[:, 960] = i
    c[:, 961] = i + 128
    c[:, 962] = i + 256
    c[:, 963] = i + 384
    c[:, 1024:1152] = (i[:, None] < i[None, :]).astype(np.float32)
    return c


def make_in_maps(inp, cfg, n_cores, seq_len, n_seq_prompt):
    NP, NPRE, NSEQ, E, FF = cfg["NP"], cfg["NPRE"], cfg["NSEQ"], cfg["E"], cfg["FF"]
    FC = FF // 128
    f = lambda a: np.ascontiguousarray(a, dtype=np.float32)
    shared = dict(
        cst=make_cst(NSEQ),
        w_ada=f(inp["w_ada"][0]), b_ada=f(inp["b_ada"][0].reshape(96, 128)), norm1_w=f(inp["norm1_w"][0].reshape(16, 128)),
        w_in=f(inp["w_in"][0]), w_gk_up=f(inp["w_gk_up"][0]), b_gk=f(inp["b_gk"][0].reshape(1, 512)),
        gla_norm_w=f(inp["gla_norm_w"][0].reshape(2, 128)), w_conv=f(inp["w_conv"][0].reshape(24, 128)), w_out=f(inp["w_out"][0]),
        norm2_w=f(inp["norm2_w"][0].reshape(16, 128)), w_router=f(inp["w_router"][0]), b_router=f(inp["b_router"][0].reshape(1, E)),
        w_up=f(inp["w_up"][0]), b_up=f(inp["b_up"][0].reshape(E * 2 * FC, 128)), w_down=f(inp["w_down"][0]),
        b_down=f(inp["b_down"][0].reshape(E * 16, 128)), final_norm_w=f(inp["final_norm_w"].reshape(16, 128)),
    )
    maps = []
    for c in range(n_cores):
        b, half = c // 2, c % 2
        m = dict(shared)
        m["xp"] = f(inp["x_prompt"][b, half * NP:(half + 1) * NP])
        m["xpre"] = f(inp["x_prompt"][b, 0:NPRE])
        m["pflag"] = np.full((128, 1), float(half), np.float32)
        m["xs"] = f(inp["x_sample"][c * NSEQ:(c + 1) * NSEQ].reshape(NSEQ * 4, D))
        m["cvec"] = f(np.concatenate([inp["c_sample"][c * NSEQ:(c + 1) * NSEQ], inp["c_prompt"][b:b + 1]], axis=0))
        m["sgla"] = f(inp["state_gla"][0, c * NSEQ:(c + 1) * NSEQ])
        m["sconv"] = f(inp["state_conv"][0, c * NSEQ:(c + 1) * NSEQ].reshape(NSEQ * 2, CH))
        maps.append(m)
    return maps


def gather_outputs(res, cfg, n_cores, n_seq_prompt, seq_len):
    NP, NSEQ = cfg["NP"], cfg["NSEQ"]
    y_p = np.zeros((n_seq_prompt, seq_len, D), np.float32)
    y_s = np.zeros((n_cores * NSEQ, 4, D), np.float32)
    gla_p = np.zeros((1, n_seq_prompt, HEADS, HK, HV), np.float32)
    conv_p = np.zeros((1, n_seq_prompt, 2, CH), np.float32)
    gla_s = np.zeros((1, n_cores * NSEQ, HEADS, HK, HV), np.float32)
    conv_s = np.zeros((1, n_cores * NSEQ, 2, CH), np.float32)
    for c in range(n_cores):
        b, half = c // 2, c % 2
        r = res[c]
        y_p[b, half * NP:(half + 1) * NP] = r["y_p"]
        y_s[c * NSEQ:(c + 1) * NSEQ] = r["y_s"].reshape(NSEQ, 4, D)
        if half == 1:
            gla_p[0, b] = r["gla_p"]
            conv_p[0, b] = r["conv_p"]
        gla_s[0, c * NSEQ:(c + 1) * NSEQ] = r["gla_s"]
        conv_s[0, c * NSEQ:(c + 1) * NSEQ] = r["conv_s"].reshape(NSEQ, 2, CH)
    return (y_p, y_s, gla_p, conv_p, gla_s, conv_s)


def kernel(**inputs):
    cfg = FULL
    n_cores = 8
    nc = build(cfg)
    maps = make_in_maps(inputs, cfg, n_cores, 2048, 4)
    res = run_bass_kernel_spmd(nc, maps, core_ids=list(range(n_cores)))
    return gather_outputs(res.results, cfg, n_cores, 4, 2048)
```

```python
import contextlib
import numpy as np
import concourse.bass as bass
import concourse.mybir as mybir
from concourse.bass_utils import run_bass_kernel_spmd

F32 = mybir.dt.float32
BF16 = mybir.dt.bfloat16
AF = mybir.ActivationFunctionType
ALU = mybir.AluOpType

D = 2048
DC = 16
HEADS = 4
HK = 128
HV = 256
DV = 1024
CH = 1024
DIN = 10256
C_Q, C_K, C_V, C_AL, C_G, C_CB, C_CC, C_CHH, C_GA, C_GB = 0, 512, 1024, 2048, 2064, 3088, 4112, 5136, 6160, 8208
EPS = 1e-6
LIMIT = 7.0
ALPHA = 1.702

FULL = dict(NP=1024, NPRE=1024, GT=256, NSEQ=16, E=32, FF=2048, TOPK=4, CAP=448)


class Buf:
    def __init__(self, name):
        self.name = name
        self.w = None
        self.r = []


class Tile:
    def __init__(self, name, t):
        self.name = name
        self.t = t
        self.buf = Buf(name)
        self.dsem = None


class Op:
    __slots__ = ("eng", "fn", "deps", "idx", "signal", "ordinal", "is_dma", "sem", "val", "n")


COMPUTE = ("pe", "act", "dve", "pool")


class Sched:
    def __init__(self, nc, es):
        self.nc = nc
        self.es = es
        self.ops = {k: [] for k in ("pe", "act", "dve", "pool", "sp")}
        self.seen = {k: {} for k in self.ops}
        self.esem = {k: es.enter_context(nc.semaphore("sem_" + k)) for k in COMPUTE}
        self.dsems = []
        self.psum = []
        self.psi = 0
        self.cur = es
        self.pending = {k: [] for k in self.ops}

    def sb(self, name, shape, dt):
        t = self.cur.enter_context(self.nc.sbuf_tensor("s_" + name, list(shape), dt))
        return Tile(name, t)

    def barrier(self):
        deps = []
        for k in COMPUTE:
            if self.ops[k]:
                deps.append(self.ops[k][-1])
        for d in self.dsems:
            if len(d) > 2 and d[2] is not None:
                deps.append(d[2])
        for k in self.ops:
            self.pending[k] = list(deps)

    def init_psum(self, n=8):
        for i in range(n):
            t = self.es.enter_context(self.nc.psum_tensor("ps%d" % i, [128, 512], F32))
            self.psum.append(Tile("ps%d" % i, t))

    def ps(self):
        p = self.psum[self.psi % len(self.psum)]
        self.psi += 1
        return p

    def new_dsem(self, name):
        s = self.es.enter_context(self.nc.semaphore("dsem_" + name))
        d = [s, 0, None]
        self.dsems.append(d)
        return d

    def _bufs(self, lst):
        out = []
        for x in lst:
            out.append(x.buf if isinstance(x, Tile) else x)
        return out

    def _add(self, eng, fn, r, w, is_dma=False, dsem=None, n=1):
        op = Op()
        op.eng, op.fn, op.is_dma, op.n = eng, fn, is_dma, n
        op.signal = False
        op.ordinal = None
        op.idx = len(self.ops[eng])
        rb, wb = self._bufs(r), self._bufs(w)
        deps = []
        for b in rb:
            if b.w is not None:
                deps.append(b.w)
        for b in wb:
            if b.w is not None:
                deps.append(b.w)
            deps.extend(b.r)
        if self.pending[eng]:
            deps.extend(self.pending[eng])
            self.pending[eng] = []
        keep = []
        seen = self.seen[eng]
        for d in deps:
            if d is op:
                continue
            if d.is_dma:
                key = ("d", id(d.sem))
                if seen.get(key, 0) >= d.val:
                    continue
                seen[key] = d.val
                keep.append(d)
            else:
                if d.eng == eng and eng == "pe":
                    continue
                if d.is_dma:
                    continue
                key = ("c", d.eng)
                if seen.get(key, -1) >= d.idx:
                    continue
                seen[key] = d.idx
                d.signal = True
                keep.append(d)
        op.deps = keep
        if is_dma:
            dsem[1] += 16 * n
            op.sem, op.val = dsem, dsem[1]
            dsem[2] = op
        for b in wb:
            b.w = op
            b.r = []
        for b in rb:
            if b not in wb:
                b.r.append(op)
        self.ops[eng].append(op)
        return op

    def op(self, eng, fn, r=(), w=()):
        return self._add(eng, fn, list(r), list(w))

    def dma(self, q, fn, r=(), w=(), sem=None, n=1):
        if sem is None:
            tl = [x for x in list(w) + list(r) if isinstance(x, Tile)][0]
            if tl.dsem is None:
                tl.dsem = self.new_dsem(tl.name)
            sem = tl.dsem
        return self._add(q, fn, list(r), list(w), is_dma=True, dsem=sem, n=n)

    def emit(self, eng, e):
        ordn = 0
        for op in self.ops[eng]:
            if op.signal:
                ordn += 1
                op.ordinal = ordn

    def assign(self):
        for eng in COMPUTE:
            ordn = 0
            for op in self.ops[eng]:
                if op.signal:
                    ordn += 1
                    op.ordinal = ordn

    def run(self, eng, e):
        for op in self.ops[eng]:
            waits = {}
            for d in op.deps:
                if d.is_dma:
                    k = id(d.sem)
                    if k not in waits or waits[k][1] < d.val:
                        waits[k] = (d.sem[0], d.val)
                else:
                    k = d.eng
                    if k not in waits or waits[k][1] < d.ordinal:
                        waits[k] = (self.esem[d.eng], d.ordinal)
            for sem, val in waits.values():
                e.wait_ge(sem, val)
            insts = op.fn(e)
            if not isinstance(insts, (list, tuple)):
                insts = [insts]
            if op.is_dma:
                assert len(insts) == op.n, (len(insts), op.n)
                for i in insts:
                    i.then_inc(op.sem[0], 16)
            elif op.signal:
                insts[-1].then_inc(self.esem[eng], 1)


def build(cfg, debug=()):
    NP, NPRE, GT, NSEQ, E, FF, TOPK, CAP = (cfg[k] for k in ("NP", "NPRE", "GT", "NSEQ", "E", "FF", "TOPK", "CAP"))
    NS = NSEQ * 4
    NT = NP + NS
    FC = FF // 128
    NR = NSEQ + 1
    nc = bass.Bass("TRN2", target_bir_lowering=False)

    def din(name, shape):
        return nc.dram_tensor(name, list(shape), F32, kind="ExternalInput").ap()

    def dout(name, shape):
        return nc.dram_tensor(name, list(shape), F32, kind="ExternalOutput").ap()

    xp = din("xp", [NP, D])
    xpre = din("xpre", [NPRE, D])
    xs = din("xs", [NS, D])
    cvec = din("cvec", [NR, D])
    pflag = din("pflag", [128, 1])
    sgla = din("sgla", [NSEQ, HEADS, HK, HV])
    sconv = din("sconv", [NSEQ * 2, CH])
    cst = din("cst", [128, 1664])
    w_ada = din("w_ada", [D, 6 * D])
    b_ada = din("b_ada", [96, 128])
    norm1_w = din("norm1_w", [16, 128])
    w_in = din("w_in", [D, DIN])
    w_gk_up = din("w_gk_up", [16, 512])
    b_gk = din("b_gk", [1, 512])
    gla_norm_w = din("gla_norm_w", [2, 128])
    w_conv = din("w_conv", [24, 128])
    w_out = din("w_out", [2048, D])
    norm2_w = din("norm2_w", [16, 128])
    w_router = din("w_router", [D, E])
    b_router = din("b_router", [1, E])
    w_up = din("w_up", [E, D, 2 * FF])
    b_up = din("b_up", [E * 2 * FC, 128])
    w_down = din("w_down", [E, FF, D])
    b_down = din("b_down", [E * DC, 128])
    final_norm_w = din("final_norm_w", [16, 128])

    y_p = dout("y_p", [NP, D])
    y_s = dout("y_s", [NS, D])
    gla_p = dout("gla_p", [HEADS, HK, HV])
    conv_p = dout("conv_p", [2, CH])
    gla_s = dout("gla_s", [NSEQ, HEADS, HK, HV])
    conv_s = dout("conv_s", [NSEQ * 2, CH])
    hscr = nc.dram_tensor("hscr", [DC, 128, NT], F32, kind="Internal").ap()
    dbg = {k: dout("dbg_" + k, shp) for k, shp in debug}

    es = contextlib.ExitStack()
    with es:
        S = Sched(nc, es)
        S.init_psum(8)
        sb = S.sb

        cs = sb("cst", [128, 1664], F32)
        S.dma("sp", lambda e: e.dma_start(out=cs.t[:], in_=cst), w=[cs])
        ident_f = cs.t[:, 0:128]
        eps_c = cs.t[:, 640:641]
        one_c = cs.t[:, 641:642]
        cb = sb("cstb", [128, 640], BF16)
        S.op("dve", lambda e: e.tensor_copy(out=cb.t[:], in_=cs.t[:, 0:640]), r=[cs], w=[cb])
        ident_b = cb.t[:, 0:128]
        maskT_b = cb.t[:, 128:256]
        ones_b = cb.t[:, 512:640]
        maskT_f = cs.t[:, 128:256]
        mrev_f = cs.t[:, 256:384]
        smask_f = cs.t[:, 384:448]
        smrev_f = cs.t[:, 448:512]
        ones_f = cs.t[:, 512:640]
        CST = [cs, cb]

        def load_vecT(name, src, n):
            stg = sb(name + "_stg", [128, 128], F32)
            dst = sb(name, [128, n], F32)
            S.dma("sp", lambda e: e.dma_start(out=stg.t[0:n, :], in_=src), w=[stg])
            p = S.ps()
            S.op("pe", lambda e: e.transpose(p.t[:, 0:n], stg.t[0:n, :], ident_f[0:n, 0:n]), r=[stg] + CST, w=[p])
            S.op("dve", lambda e: e.tensor_copy(out=dst.t[:, :], in_=p.t[:, 0:n]), r=[p], w=[dst])
            return dst

        badaT = load_vecT("badaT", b_ada, 96)
        n1T = load_vecT("n1T", norm1_w, 16)
        n2T = load_vecT("n2T", norm2_w, 16)
        nfT = load_vecT("nfT", final_norm_w, 16)
        gnT = load_vecT("gnT", gla_norm_w, 2)
        wcT = load_vecT("wcT", w_conv, 24)
        pfl = sb("pfl", [128, 1], F32)
        S.dma("sp", lambda e: e.dma_start(out=pfl.t[:], in_=pflag), w=[pfl])

        wgk_f = sb("wgk_f", [16, 512], F32)
        S.dma("sp", lambda e: e.dma_start(out=wgk_f.t[:], in_=w_gk_up), w=[wgk_f])
        wgk = sb("wgk", [16, 512], BF16)
        S.op("dve", lambda e: e.tensor_copy(out=wgk.t[:], in_=wgk_f.t[:]), r=[wgk_f], w=[wgk])
        bgk_f = sb("bgk_f", [1, 512], F32)
        S.dma("sp", lambda e: e.dma_start(out=bgk_f.t[:], in_=b_gk), w=[bgk_f])
        bgk = sb("bgk", [1, 512], BF16)
        S.op("dve", lambda e: e.tensor_copy(out=bgk.t[:], in_=bgk_f.t[:]), r=[bgk_f], w=[bgk])

        NW = 3
        wpool = []
        wstate = [0]
        nw_cur = [NW]

        def alloc_wpool(tag, n=NW):
            wpool[:] = [sb("wt%s%d" % (tag, i), [128, 16, 512], BF16) for i in range(n)]
            nw_cur[0] = n

        def wtile():
            t = wpool[wstate[0] % nw_cur[0]]
            wstate[0] += 1
            return t

        def load_w(src2d, c0, ncols, kc=16, t=None, col_off=0):
            if t is None:
                t = wtile()
            v = src2d.rearrange("(kc p) n -> p kc n", p=128)
            S.dma("pool", lambda e: e.dma_start(out=t.t[:, 0:kc, col_off:col_off + ncols], in_=v[:, :, c0:c0 + ncols]), w=[t])
            return t

        siluT = sb("siluT", [128, 16, NR], BF16)
        modT = sb("modT", [128, 96, NR], F32)
        A1 = sb("A1", [128, 16, NR], F32)
        A2 = sb("A2", [128, 16, NR], F32)
        es_ada = contextlib.ExitStack()
        S.cur = es_ada
        alloc_wpool("A")
        cv = sb("cv", [NR, D], F32)
        S.dma("sp", lambda e: e.dma_start(out=cv.t[:], in_=cvec), w=[cv])
        cvs = sb("cvs", [NR, D], F32)
        S.op("act", lambda e: e.activation(out=cvs.t[:], in_=cv.t[:], func=AF.Silu), r=[cv], w=[cvs])
        p = S.ps()
        for kc in range(16):
            S.op("pe", lambda e, kc=kc, p=p: e.transpose(p.t[:, kc * NR:(kc + 1) * NR], cvs.t[0:NR, kc * 128:(kc + 1) * 128],
                                                       ident_f[0:NR, 0:NR]), r=[cvs] + CST, w=[p])
        S.op("dve", lambda e, p=p: e.tensor_copy(out=siluT.t[:].rearrange("p a b -> p (a b)"), in_=p.t[:, 0:16 * NR]), r=[p], w=[siluT])
        for ct in range(24):
            W = load_w(w_ada, ct * 512, 512)
            for sbk in range(4):
                nb = ct * 4 + sbk
                p = S.ps()
                for kc in range(16):
                    S.op("pe", lambda e, kc=kc, p=p, W=W, sbk=sbk: e.matmul(p.t[:, 0:NR], W.t[:, kc, sbk * 128:(sbk + 1) * 128],
                                                                            siluT.t[:, kc, :], start=(kc == 0), stop=(kc == 15)),
                         r=[W, siluT], w=[p])
                S.op("act", lambda e, p=p, nb=nb: e.activation(out=modT.t[:, nb, :], in_=p.t[:, 0:NR], func=AF.Identity,
                                                               bias=badaT.t[:, nb:nb + 1], scale=1.0), r=[p, badaT], w=[modT])
        for dc in range(16):
            S.op("dve", lambda e, dc=dc: e.tensor_scalar(out=A1.t[:, dc, :], in0=modT.t[:, 16 + dc, :], scalar1=1.0,
                                                         scalar2=n1T.t[:, dc:dc + 1], op0=ALU.add, op1=ALU.mult), r=[modT, n1T], w=[A1])
            S.op("dve", lambda e, dc=dc: e.tensor_scalar(out=A2.t[:, dc, :], in0=modT.t[:, 64 + dc, :], scalar1=1.0,
                                                         scalar2=n2T.t[:, dc:dc + 1], op0=ALU.add, op1=ALU.mult), r=[modT, n2T], w=[A2])
        M_SH1, M_G1, M_SH2, M_G2 = 0, 32, 48, 80
        S.barrier()
        es_ada.close()
        S.cur = es

        xstage = []
        xst = [0]

        def load_xT(src, ntok, dst, toff):
            for tc0 in range(0, ntok, 128):
                n = min(128, ntok - tc0)
                stg = xstage[0]
                xst[0] += 1
                S.dma("sp", lambda e, stg=stg, tc0=tc0, n=n: e.dma_start(out=stg.t[0:n, :], in_=src[tc0:tc0 + n, :]), w=[stg])
                for q4 in range(4):
                    p = S.ps()
                    for i in range(4):
                        dc = q4 * 4 + i
                        S.op("pe", lambda e, p=p, i=i, dc=dc, stg=stg, n=n: e.transpose(p.t[:, i * 128:i * 128 + n], stg.t[0:n, dc * 128:(dc + 1) * 128],
                                                                                          ident_f[0:n, 0:n]), r=[stg] + CST, w=[p])
                    S.op("act" if q4 % 2 else "dve",
                         (lambda e, p=p, q4=q4, tc0=tc0, n=n: e.activation(out=dst.t[:, q4 * 4:(q4 + 1) * 4, toff + tc0:toff + tc0 + n],
                                                                           in_=p.t[:, :].rearrange("p (a b) -> p a b", a=4)[:, :, 0:n], func=AF.Copy))
                         if q4 % 2 else
                         (lambda e, p=p, q4=q4, tc0=tc0, n=n: e.tensor_copy(out=dst.t[:, q4 * 4:(q4 + 1) * 4, toff + tc0:toff + tc0 + n],
                                                                            in_=p.t[:, :].rearrange("p (a b) -> p a b", a=4)[:, :, 0:n])),
                         r=[p], w=[dst])

        sqt = []
        rstd_l = []
        ntmp = []

        def alloc_tmps(tag, with_x=False):
            sqt[:] = [sb("sq%s%d" % (tag, i), [128, 512], BF16) for i in range(2)]
            rstd_l[:] = [sb("rstd%s" % tag, [128, 512], F32)]
            ntmp[:] = [sb("ntmp%s%d" % (tag, i), [128, 512], F32) for i in range(2)]
            if with_x:
                xstage[:] = [sb("xstg%s" % tag, [128, D], F32)]

        def rmsnorm_fm(src, soff, ntok, dst, doff, A, B_mod, sample, post=None):
            rstd = rstd_l[0]
            p = S.ps()
            for dc in range(16):
                sq = sqt[dc % 2]
                S.op("act", lambda e, dc=dc, sq=sq: e.activation(out=sq.t[:, 0:ntok], in_=src.t[:, dc, soff:soff + ntok], func=AF.Square),
                     r=[src], w=[sq])
                S.op("pe", lambda e, dc=dc, sq=sq, p=p: e.matmul(p.t[:, 0:ntok], ones_b, sq.t[:, 0:ntok], start=(dc == 0), stop=(dc == 15)),
                     r=[sq] + CST, w=[p])
            S.op("act", lambda e, p=p: e.activation(out=rstd.t[:, 0:ntok], in_=p.t[:, 0:ntok], func=AF.Sqrt, bias=eps_c, scale=1.0 / D),
                 r=[p] + CST, w=[rstd])
            S.op("dve", lambda e: e.reciprocal(out=rstd.t[:, 0:ntok], in_=rstd.t[:, 0:ntok]), r=[rstd], w=[rstd])
            for dc_ in range(16):
                tm = ntmp[dc_ % 2]
                S.op("dve", lambda e, dc=dc_, tm=tm: e.tensor_tensor(out=tm.t[:, 0:ntok], in0=src.t[:, dc, soff:soff + ntok], in1=rstd.t[:, 0:ntok],
                                                                    op=ALU.mult), r=[src, rstd], w=[tm])
                dc = dc_ if post is None else dc_ % 2
                if not sample:
                    if B_mod is None:
                        S.op("act", lambda e, dc=dc, dcf=dc_, tm=tm: e.activation(out=dst.t[:, dc, doff:doff + ntok], in_=tm.t[:, 0:ntok], func=AF.Copy,
                                                                         scale=A.t[:, dcf:dcf + 1]), r=[tm, A], w=[dst])
                    else:
                        S.op("act", lambda e, dc=dc, dcf=dc_, tm=tm: e.activation(out=dst.t[:, dc, doff:doff + ntok], in_=tm.t[:, 0:ntok], func=AF.Identity,
                                                                         scale=A.t[:, dcf, NSEQ:NSEQ + 1], bias=modT.t[:, B_mod + dcf, NSEQ:NSEQ + 1]),
                             r=[tm, A, modT], w=[dst])
                else:
                    tv = tm.t[:, 0:ntok].rearrange("p (s t) -> p s t", t=4)
                    S.op("dve", lambda e, dc=dc, dcf=dc_, tv=tv: e.tensor_tensor(out=tv, in0=tv, in1=A.t[:, dcf, 0:NSEQ].unsqueeze(2).to_broadcast([128, NSEQ, 4]),
                                                                        op=ALU.mult), r=[tm, A], w=[tm])
                    S.op("dve", lambda e, dc=dc, dcf=dc_, tv=tv: e.tensor_tensor(out=dst.t[:, dc, doff:doff + ntok].rearrange("p (s t) -> p s t", t=4), in0=tv,
                                                                        in1=modT.t[:, B_mod + dcf, 0:NSEQ].unsqueeze(2).to_broadcast([128, NSEQ, 4]),
                                                                        op=ALU.add), r=[tm, modT], w=[dst])
                if post is not None:
                    post(dc_, dc)

        es_mix = contextlib.ExitStack()
        S.cur = es_mix
        alloc_wpool("B")
        alloc_tmps("B", with_x=True)
        xg = sb("xg", [128, 16, GT], F32)
        xn = sb("xn", [128, 16, GT], BF16)
        NTC = GT // 128
        qT = sb("qT", [128, 4, GT], F32)
        kT = sb("kT", [128, 4, GT], F32)
        ktm = sb("ktm", [128, NTC, 512], F32)
        vtm = sb("vtm", [128, NTC, 1024], BF16)
        Ltm = sb("Ltm", [128, NTC, 512], F32)
        alT = sb("alT", [16, GT], BF16)
        sgT = sb("sgT", [128, 8, GT], BF16)
        oT = sb("oT", [128, 8, GT], F32)
        oaT = sb("oaT", [128, 8, GT], BF16)
        obT = sb("obT", [128, 8, GT], BF16)
        Sst = [sb("Sst%d" % h, [128, 256], F32) for h in range(4)]
        Sbf = [sb("Sbf%d" % h, [128, 256], BF16) for h in range(4)]
        halo = sb("halo", [128, 8, 2], F32)
        for h in range(4):
            S.op("dve", lambda e, h=h: e.memset(Sst[h].t[:], 0.0), w=[Sst[h]])
            S.op("dve", lambda e, h=h: e.memset(Sbf[h].t[:], 0.0), w=[Sbf[h]])
        S.op("dve", lambda e: e.memset(halo.t[:], 0.0), w=[halo])
        expb = sb("expb", [128, 128], F32)
        expnb = sb("expnb", [128, 128], F32)
        qp = sb("qp", [128, 128], BF16)
        kp = sb("kp", [128, 128], BF16)
        kk = sb("kk", [128, 512], BF16)
        erev = sb("erev", [128, 512], F32)
        PTm = sb("PTm", [128, 128], BF16)
        gtmp = [sb("gtmp%d" % i, [128, 512], F32) for i in range(4)]
        cct = sb("cct", [128, 4, GT], F32)
        uext = sb("uext", [128, 4, GT + 2 * max(1, NSEQ)], F32)
        cnv = sb("cnv", [128, GT], F32)
        s0f = [sb("s0f%d" % i, [128, 256], F32) for i in range(2)]
        s0b = [sb("s0b%d" % i, [128, 256], BF16) for i in range(2)]
        snew = [sb("snew%d" % i, [128, 256], F32) for i in range(2)]
        ohs = sb("ohs", [128, NSEQ], F32)
        S.op("dve", lambda e: e.tensor_copy(out=ohs.t[:, :], in_=cs.t[:, 642:642 + NSEQ]), r=[cs], w=[ohs])
        scv = sb("scv", [128, 8, NSEQ * 2], F32)
        ctm = sb("ctm", [NSEQ * 2, CH], F32)
        S.dma("sp", lambda e: e.dma_start(out=ctm.t[:], in_=sconv), w=[ctm])
        p = S.ps()
        for blk in range(8):
            S.op("pe", lambda e, blk=blk, p=p: e.transpose(p.t[:, blk * NSEQ * 2:(blk + 1) * NSEQ * 2], ctm.t[0:NSEQ * 2, blk * 128:(blk + 1) * 128],
                                                         ident_f[0:NSEQ * 2, 0:NSEQ * 2]), r=[ctm] + CST, w=[p])
        S.op("dve", lambda e, p=p: e.tensor_copy(out=scv.t[:].rearrange("p a b -> p (a b)"), in_=p.t[:, 0:8 * NSEQ * 2]), r=[p], w=[scv])

        def proj_fm(W, wc0, ntok, t0, M=128):
            p = S.ps()
            for kc in range(16):
                S.op("pe", lambda e, kc=kc, p=p: e.matmul(p.t[0:M, 0:ntok], W.t[:, kc, wc0:wc0 + M], xn.t[:, kc, t0:t0 + ntok],
                                                        start=(kc == 0), stop=(kc == 15)), r=[W, xn], w=[p])
            return p

        def proj_tm(W, tc, n, ncols=512):
            p = S.ps()
            for kc in range(16):
                S.op("pe", lambda e, kc=kc, p=p: e.matmul(p.t[0:n, 0:ncols], xn.t[:, kc, tc * 128:tc * 128 + n], W.t[:, kc, 0:ncols],
                                                        start=(kc == 0), stop=(kc == 15)), r=[W, xn], w=[p])
            return p

        def mixer_group(kind, src, ntok, hoff, last_prefix=False):
            sample = kind == "samp"
            full = kind != "pre"
            ntc = (ntok + 127) // 128
            load_xT(src, ntok, xg, 0)
            rmsnorm_fm(xg, 0, ntok, xn, 0, A1, M_SH1, sample)
            if full:
                W = load_w(w_in, C_Q, 512)
                for h in range(4):
                    p = proj_fm(W, h * 128, ntok, 0)
                    S.op("act", lambda e, p=p, h=h: e.activation(out=qT.t[:, h, 0:ntok], in_=p.t[:, 0:ntok], func=AF.Copy, scale=HK ** -0.5),
                         r=[p], w=[qT])
            W = load_w(w_in, C_K, 512)
            if full:
                for h in range(4):
                    p = proj_fm(W, h * 128, ntok, 0)
                    S.op("dve", lambda e, p=p, h=h: e.tensor_copy(out=kT.t[:, h, 0:ntok], in_=p.t[:, 0:ntok]), r=[p], w=[kT])
            for tc in range(ntc):
                n = min(128, ntok - tc * 128)
                p = proj_tm(W, tc, n)
                S.op("act", lambda e, p=p, tc=tc, n=n: e.activation(out=ktm.t[0:n, tc, :], in_=p.t[0:n, :], func=AF.Copy), r=[p], w=[ktm])
            for vt in range(2):
                W = load_w(w_in, C_V + vt * 512, 512)
                for tc in range(ntc):
                    n = min(128, ntok - tc * 128)
                    p = proj_tm(W, tc, n)
                    S.op("dve" if vt else "act",
                         (lambda e, p=p, tc=tc, n=n, vt=vt: e.tensor_copy(out=vtm.t[0:n, tc, vt * 512:(vt + 1) * 512], in_=p.t[0:n, :])) if vt else
                         (lambda e, p=p, tc=tc, n=n, vt=vt: e.activation(out=vtm.t[0:n, tc, vt * 512:(vt + 1) * 512], in_=p.t[0:n, :], func=AF.Copy)),
                         r=[p], w=[vtm])
            W = load_w(w_in, C_AL, 16)
            p = proj_fm(W, 0, ntok, 0, M=16)
            S.op("dve", lambda e, p=p: e.tensor_copy(out=alT.t[:, 0:ntok], in_=p.t[0:16, 0:ntok]), r=[p], w=[alT])
            for tc in range(ntc):
                n = min(128, ntok - tc * 128)
                p = S.ps()
                S.op("pe", lambda e, p=p, tc=tc, n=n: e.matmul(p.t[0:n, :], alT.t[:, tc * 128:tc * 128 + n], wgk.t[:, :], start=True, stop=False),
                     r=[alT, wgk], w=[p])
                S.op("pe", lambda e, p=p, n=n: e.matmul(p.t[0:n, :], ones_b[0:1, 0:n], bgk.t[:, :], start=False, stop=True), r=[bgk] + CST, w=[p])
                S.op("act", lambda e, p=p, tc=tc, n=n: e.activation(out=Ltm.t[0:n, tc, :], in_=p.t[0:n, :], func=AF.Exp, scale=-1.0), r=[p], w=[Ltm])
                S.op("act", lambda e, tc=tc, n=n: e.activation(out=Ltm.t[0:n, tc, :], in_=Ltm.t[0:n, tc, :], func=AF.Ln, bias=one_c[0:n, :], scale=1.0),
                     r=[Ltm] + CST, w=[Ltm])
            if full:
                for gt in range(2):
                    W = load_w(w_in, C_G + gt * 512, 512)
                    for b4 in range(4):
                        p = proj_fm(W, b4 * 128, ntok, 0)
                        S.op("act", lambda e, p=p, gt=gt, b4=b4: e.activation(out=sgT.t[:, gt * 4 + b4, 0:ntok], in_=p.t[:, 0:ntok], func=AF.Silu),
                             r=[p], w=[sgT])
            for tc in range(ntc):
                n = min(128, ntok - tc * 128)
                mk_f = smask_f[0:n, 0:n] if sample else maskT_f[0:n, 0:n]
                mr_f = smrev_f[0:n, 0:n] if sample else mrev_f[0:n, 0:n]
                p = S.ps()
                S.op("pe", lambda e, p=p, tc=tc, n=n, mr_f=mr_f: e.matmul(p.t[0:n, :], mr_f, Ltm.t[0:n, tc, :], start=True, stop=True),
                     r=[Ltm] + CST, w=[p])
                S.op("act", lambda e, p=p, n=n: e.activation(out=erev.t[0:n, :], in_=p.t[0:n, :], func=AF.Exp, scale=-1.0 / 16), r=[p], w=[erev])
                S.op("dve", lambda e, tc=tc, n=n: e.tensor_tensor(out=kk.t[0:n, :], in0=ktm.t[0:n, tc, :], in1=erev.t[0:n, :], op=ALU.mult),
                     r=[ktm, erev], w=[kk])
                for h in range(4):
                    pb = S.ps()
                    S.op("pe", lambda e, pb=pb, tc=tc, n=n, h=h, mk_f=mk_f: e.matmul(pb.t[:, 0:n], Ltm.t[0:n, tc, h * 128:(h + 1) * 128], mk_f,
                                                                                    start=True, stop=True), r=[Ltm] + CST, w=[pb])
                    S.op("act", lambda e, pb=pb, n=n: e.activation(out=expb.t[:, 0:n], in_=pb.t[:, 0:n], func=AF.Exp, scale=-1.0 / 16), r=[pb], w=[expb])
                    if full:
                        S.op("act", lambda e, pb=pb, n=n: e.activation(out=expnb.t[:, 0:n], in_=pb.t[:, 0:n], func=AF.Exp, scale=1.0 / 16), r=[pb], w=[expnb])
                        S.op("dve", lambda e, h=h, tc=tc, n=n: e.tensor_tensor(out=qp.t[:, 0:n], in0=qT.t[:, h, tc * 128:tc * 128 + n], in1=expb.t[:, 0:n],
                                                                               op=ALU.mult), r=[qT, expb], w=[qp])
                        S.op("dve", lambda e, h=h, tc=tc, n=n: e.tensor_tensor(out=kp.t[:, 0:n], in0=kT.t[:, h, tc * 128:tc * 128 + n], in1=expnb.t[:, 0:n],
                                                                               op=ALU.mult), r=[kT, expnb], w=[kp])
                        pp = S.ps()
                        S.op("pe", lambda e, pp=pp, n=n: e.matmul(pp.t[0:n, 0:n], kp.t[:, 0:n], qp.t[:, 0:n], start=True, stop=True), r=[kp, qp], w=[pp])
                        S.op("dve", lambda e, pp=pp, n=n, mk_f=mk_f: e.tensor_tensor(out=PTm.t[0:n, 0:n], in0=pp.t[0:n, 0:n], in1=mk_f, op=ALU.mult),
                             r=[pp] + CST, w=[PTm])
                        if not sample:
                            po = S.ps()
                            for eb in range(2):
                                S.op("pe", lambda e, po=po, eb=eb, n=n, tc=tc, h=h: e.matmul(po.t[:, eb * 128:eb * 128 + n],
                                                                                            vtm.t[0:n, tc, h * 256 + eb * 128:h * 256 + (eb + 1) * 128],
                                                                                            PTm.t[0:n, 0:n], start=True, stop=False), r=[vtm, PTm], w=[po])
                                S.op("pe", lambda e, po=po, eb=eb, n=n, h=h: e.matmul(po.t[:, eb * 128:eb * 128 + n], Sbf[h].t[:, eb * 128:(eb + 1) * 128],
                                                                                     qp.t[:, 0:n], start=False, stop=True), r=[Sbf[h], qp], w=[po])
                            S.op("act", lambda e, po=po, h=h, tc=tc, n=n: e.activation(out=oT.t[:, 2 * h:2 * h + 2, tc * 128:tc * 128 + n],
                                                                                       in_=po.t[:, 0:256].rearrange("p (a b) -> p a b", a=2)[:, :, 0:n],
                                                                                       func=AF.Copy), r=[po], w=[oT])
                    if not sample:
                        pd = S.ps()
                        S.op("pe", lambda e, pd=pd, n=n, tc=tc, h=h: e.matmul(pd.t[:, 0:256], kk.t[0:n, h * 128:(h + 1) * 128], vtm.t[0:n, tc, h * 256:(h + 1) * 256],
                                                                             start=True, stop=True), r=[kk, vtm], w=[pd])
                        S.op("dve", lambda e, pd=pd, h=h, n=n: e.scalar_tensor_tensor(out=Sst[h].t[:, :], in0=Sst[h].t[:, :], scalar=expb.t[:, n - 1:n], in1=pd.t[:, 0:256],
                                                                                      op0=ALU.mult, op1=ALU.add), r=[Sst[h], expb, pd], w=[Sst[h]])
                        S.op("act", lambda e, h=h: e.activation(out=Sbf[h].t[:, :], in_=Sst[h].t[:, :], func=AF.Copy), r=[Sst[h]], w=[Sbf[h]])
                    else:
                        for s in range(NSEQ):
                            i2 = (s * 4 + h) % 2
                            S.dma("sp", lambda e, s=s, h=h, i2=i2: e.dma_start(out=s0f[i2].t[:, :], in_=sgla[s, h]), w=[s0f[i2]])
                            S.op("act", lambda e, i2=i2: e.activation(out=s0b[i2].t[:, :], in_=s0f[i2].t[:, :], func=AF.Copy), r=[s0f[i2]], w=[s0b[i2]])
                            po = S.ps()
                            for eb in range(2):
                                S.op("pe", lambda e, po=po, eb=eb, n=n, h=h, s=s: e.matmul(po.t[:, eb * 4:eb * 4 + 4],
                                                                                          vtm.t[0:n, 0, h * 256 + eb * 128:h * 256 + (eb + 1) * 128],
                                                                                          PTm.t[0:n, s * 4:s * 4 + 4], start=True, stop=False), r=[vtm, PTm], w=[po])
                                S.op("pe", lambda e, po=po, eb=eb, i2=i2, s=s: e.matmul(po.t[:, eb * 4:eb * 4 + 4], s0b[i2].t[:, eb * 128:(eb + 1) * 128],
                                                                                       qp.t[:, s * 4:s * 4 + 4], start=False, stop=True), r=[s0b[i2], qp], w=[po])
                            S.op("act", lambda e, po=po, h=h, s=s: e.activation(out=oT.t[:, 2 * h:2 * h + 2, s * 4:s * 4 + 4],
                                                                                in_=po.t[:, 0:8].rearrange("p (a b) -> p a b", a=2), func=AF.Copy), r=[po], w=[oT])
                            S.op("dve", lambda e, s=s, h=h, n=n: e.tensor_scalar(out=kp.t[0:n, :], in0=kk.t[0:n, h * 128:(h + 1) * 128], scalar1=ohs.t[0:n, s:s + 1],
                                                                               scalar2=None, op0=ALU.mult), r=[kk, ohs], w=[kp])
                            pd = S.ps()
                            S.op("pe", lambda e, pd=pd, n=n, h=h: e.matmul(pd.t[:, 0:256], kp.t[0:n, :], vtm.t[0:n, 0, h * 256:(h + 1) * 256], start=True, stop=True),
                                 r=[kp, vtm], w=[pd])
                            S.op("dve", lambda e, pd=pd, i2=i2, s=s: e.scalar_tensor_tensor(out=snew[i2].t[:, :], in0=s0f[i2].t[:, :], scalar=expb.t[:, s * 4 + 3:s * 4 + 4],
                                                                                           in1=pd.t[:, 0:256], op0=ALU.mult, op1=ALU.add),
                                 r=[s0f[i2], expb, pd], w=[snew[i2]])
                            S.dma("sp", lambda e, s=s, h=h, i2=i2: e.dma_start(out=gla_s[s, h], in_=snew[i2].t[:, :]), r=[snew[i2]])
            if last_prefix:
                for h in range(4):
                    S.op("dve", lambda e, h=h: e.tensor_scalar(out=Sst[h].t[:, :], in0=Sst[h].t[:, :], scalar1=pfl.t[:, 0:1], scalar2=None, op0=ALU.mult),
                         r=[Sst[h], pfl], w=[Sst[h]])
                    S.op("act", lambda e, h=h: e.activation(out=Sbf[h].t[:, :], in_=Sst[h].t[:, :], func=AF.Copy), r=[Sst[h]], w=[Sbf[h]])
            rstd = rstd_l[0]
            if full:
                for h in range(4):
                    p = S.ps()
                    for eb in range(2):
                        sq = ntmp[eb]
                        S.op("act", lambda e, sq=sq, h=h, eb=eb: e.activation(out=sq.t[:, 0:ntok], in_=oT.t[:, 2 * h + eb, 0:ntok], func=AF.Square), r=[oT], w=[sq])
                        S.op("pe", lambda e, sq=sq, p=p, eb=eb: e.matmul(p.t[:, 0:ntok], ones_f, sq.t[:, 0:ntok], start=(eb == 0), stop=(eb == 1)),
                             r=[sq] + CST, w=[p])
                    S.op("act", lambda e, p=p: e.activation(out=rstd.t[:, 0:ntok], in_=p.t[:, 0:ntok], func=AF.Sqrt, bias=eps_c, scale=1.0 / HV),
                         r=[p] + CST, w=[rstd])
                    S.op("dve", lambda e: e.reciprocal(out=rstd.t[:, 0:ntok], in_=rstd.t[:, 0:ntok]), r=[rstd], w=[rstd])
                    for eb in range(2):
                        tm = gtmp[eb]
                        S.op("dve", lambda e, tm=tm, h=h, eb=eb: e.tensor_tensor(out=tm.t[:, 0:ntok], in0=oT.t[:, 2 * h + eb, 0:ntok], in1=rstd.t[:, 0:ntok], op=ALU.mult),
                             r=[oT, rstd], w=[tm])
                        S.op("dve", lambda e, tm=tm, h=h, eb=eb: e.scalar_tensor_tensor(out=oaT.t[:, 2 * h + eb, 0:ntok], in0=tm.t[:, 0:ntok], scalar=gnT.t[:, eb:eb + 1],
                                                                                       in1=sgT.t[:, 2 * h + eb, 0:ntok], op0=ALU.mult, op1=ALU.mult),
                             r=[tm, gnT, sgT], w=[oaT])
            if full or last_prefix:
                for half in range(2):
                    if sample:
                        ue = uext.t[:, :, 0:NSEQ * 6].rearrange("p b (s t) -> p b s t", t=6)
                        for b4 in range(4):
                            S.op("dve", lambda e, b4=b4, half=half, ue=ue: e.tensor_copy(out=ue[:, b4, :, 0:2],
                                                                                        in_=scv.t[:, half * 4 + b4, :].rearrange("p (s t) -> p s t", t=2)),
                                 r=[scv], w=[uext])
                    elif full:
                        S.op("dve", lambda e, half=half: e.tensor_copy(out=uext.t[:, :, 0:2], in_=halo.t[:, half * 4:half * 4 + 4, :]), r=[halo], w=[uext])
                    t0 = 0 if full else ntok - 2
                    nn = ntok - t0
                    W = load_w(w_in, C_CC + half * 512, 512)
                    for b4 in range(4):
                        p = proj_fm(W, b4 * 128, nn, t0)
                        S.op("act", lambda e, p=p, b4=b4, nn=nn: e.activation(out=cct.t[:, b4, 0:nn], in_=p.t[:, 0:nn], func=AF.Copy), r=[p], w=[cct])
                    W = load_w(w_in, C_CHH + half * 512, 512)
                    for b4 in range(4):
                        p = proj_fm(W, b4 * 128, nn, t0)
                        if sample:
                            ue = uext.t[:, :, 0:NSEQ * 6].rearrange("p b (s t) -> p b s t", t=6)
                            S.op("dve", lambda e, p=p, b4=b4, ue=ue: e.tensor_tensor(out=ue[:, b4, :, 2:6], in0=p.t[:, 0:ntok].rearrange("p (s t) -> p s t", t=4),
                                                                                    in1=cct.t[:, b4, 0:ntok].rearrange("p (s t) -> p s t", t=4), op=ALU.mult),
                                 r=[p, cct], w=[uext])
                        elif full:
                            S.op("dve", lambda e, p=p, b4=b4: e.tensor_tensor(out=uext.t[:, b4, 2:2 + ntok], in0=p.t[:, 0:ntok], in1=cct.t[:, b4, 0:ntok], op=ALU.mult),
                                 r=[p, cct], w=[uext])
                        else:
                            S.op("dve", lambda e, p=p, b4=b4, half=half: e.scalar_tensor_tensor(out=halo.t[:, half * 4 + b4, :], in0=p.t[:, 0:2], scalar=pfl.t[:, 0:1],
                                                                                               in1=cct.t[:, b4, 0:2], op0=ALU.mult, op1=ALU.mult),
                                 r=[p, pfl, cct], w=[halo])
                    if not full:
                        continue
                    W = load_w(w_in, C_CB + half * 512, 512)
                    for b4 in range(4):
                        blk = half * 4 + b4
                        p = proj_fm(W, b4 * 128, ntok, 0)
                        if sample:
                            ue = uext.t[:, :, 0:NSEQ * 6].rearrange("p b (s t) -> p b s t", t=6)
                            cv3 = cnv.t[:, 0:ntok].rearrange("p (s t) -> p s t", t=4)
                            u0, u1, u2 = ue[:, b4, :, 0:4], ue[:, b4, :, 1:5], ue[:, b4, :, 2:6]
                        else:
                            cv3 = cnv.t[:, 0:ntok]
                            u0, u1, u2 = uext.t[:, b4, 0:ntok], uext.t[:, b4, 1:1 + ntok], uext.t[:, b4, 2:2 + ntok]
                        S.op("dve", lambda e, cv3=cv3, u0=u0, blk=blk: e.tensor_scalar(out=cv3, in0=u0, scalar1=wcT.t[:, blk:blk + 1], scalar2=None, op0=ALU.mult),
                             r=[uext, wcT], w=[cnv])
                        S.op("dve", lambda e, cv3=cv3, u1=u1, blk=blk: e.scalar_tensor_tensor(out=cv3, in0=u1, scalar=wcT.t[:, 8 + blk:9 + blk], in1=cv3,
                                                                                             op0=ALU.mult, op1=ALU.add), r=[uext, wcT, cnv], w=[cnv])
                        S.op("dve", lambda e, cv3=cv3, u2=u2, blk=blk: e.scalar_tensor_tensor(out=cv3, in0=u2, scalar=wcT.t[:, 16 + blk:17 + blk], in1=cv3,
                                                                                             op0=ALU.mult, op1=ALU.add), r=[uext, wcT, cnv], w=[cnv])
                        S.op("dve", lambda e, p=p, blk=blk: e.tensor_tensor(out=obT.t[:, blk, 0:ntok], in0=p.t[:, 0:ntok], in1=cnv.t[:, 0:ntok], op=ALU.mult),
                             r=[p, cnv], w=[obT])
                    if sample:
                        ue = uext.t[:, :, 0:NSEQ * 6].rearrange("p b (s t) -> p b s t", t=6)
                        for b4 in range(4):
                            S.op("dve", lambda e, b4=b4, half=half, ue=ue: e.tensor_copy(out=scv.t[:, half * 4 + b4, :].rearrange("p (s t) -> p s t", t=2),
                                                                                        in_=ue[:, b4, :, 4:6]), r=[uext], w=[scv])
                    else:
                        S.op("dve", lambda e, half=half: e.tensor_copy(out=halo.t[:, half * 4:half * 4 + 4, :], in_=uext.t[:, :, ntok:ntok + 2]), r=[uext], w=[halo])
            if not full:
                return
            for t4 in range(4):
                Wo = load_w(w_out, t4 * 512, 512)
                Wa = load_w(w_in, C_GA + t4 * 512, 512)
                Wb = load_w(w_in, C_GB + t4 * 512, 512)
                for sbk in range(4):
                    j = t4 * 4 + sbk
                    pA = S.ps()
                    for kc in range(8):
                        S.op("pe", lambda e, kc=kc, pA=pA, Wo=Wo, sbk=sbk: e.matmul(pA.t[:, 0:ntok], Wo.t[:, kc, sbk * 128:(sbk + 1) * 128], oaT.t[:, kc, 0:ntok],
                                                                                   start=(kc == 0), stop=(kc == 7)), r=[Wo, oaT], w=[pA])
                    pB = S.ps()
                    for kc in range(8):
                        S.op("pe", lambda e, kc=kc, pB=pB, Wo=Wo, sbk=sbk: e.matmul(pB.t[:, 0:ntok], Wo.t[:, 8 + kc, sbk * 128:(sbk + 1) * 128], obT.t[:, kc, 0:ntok],
                                                                                   start=(kc == 0), stop=(kc == 7)), r=[Wo, obT], w=[pB])
                    pGa = proj_fm(Wa, sbk * 128, ntok, 0)
                    pGb = proj_fm(Wb, sbk * 128, ntok, 0)
                    S.op("act", lambda e, pGa=pGa: e.activation(out=gtmp[0].t[:, 0:ntok], in_=pGa.t[:, 0:ntok], func=AF.Sigmoid), r=[pGa], w=[gtmp[0]])
                    S.op("act", lambda e, pGb=pGb: e.activation(out=gtmp[1].t[:, 0:ntok], in_=pGb.t[:, 0:ntok], func=AF.Sigmoid), r=[pGb], w=[gtmp[1]])
                    S.op("dve", lambda e, pA=pA: e.tensor_tensor(out=gtmp[0].t[:, 0:ntok], in0=pA.t[:, 0:ntok], in1=gtmp[0].t[:, 0:ntok], op=ALU.mult),
                         r=[pA, gtmp[0]], w=[gtmp[0]])
                    S.op("dve", lambda e, pB=pB: e.tensor_tensor(out=gtmp[1].t[:, 0:ntok], in0=pB.t[:, 0:ntok], in1=gtmp[1].t[:, 0:ntok], op=ALU.mult),
                         r=[pB, gtmp[1]], w=[gtmp[1]])
                    S.op("dve", lambda e: e.tensor_tensor(out=gtmp[0].t[:, 0:ntok], in0=gtmp[0].t[:, 0:ntok], in1=gtmp[1].t[:, 0:ntok], op=ALU.add),
                         r=[gtmp[0], gtmp[1]], w=[gtmp[0]])
                    if not sample:
                        S.op("dve", lambda e, j=j: e.scalar_tensor_tensor(out=xg.t[:, j, 0:ntok], in0=gtmp[0].t[:, 0:ntok], scalar=modT.t[:, M_G1 + j, NSEQ:NSEQ + 1],
                                                                         in1=xg.t[:, j, 0:ntok], op0=ALU.mult, op1=ALU.add), r=[gtmp[0], modT, xg], w=[xg])
                    else:
                        g3 = gtmp[0].t[:, 0:ntok].rearrange("p (s t) -> p s t", t=4)
                        S.op("dve", lambda e, j=j, g3=g3: e.tensor_tensor(out=g3, in0=g3, in1=modT.t[:, M_G1 + j, 0:NSEQ].unsqueeze(2).to_broadcast([128, NSEQ, 4]),
                                                                         op=ALU.mult), r=[gtmp[0], modT], w=[gtmp[0]])
                        S.op("dve", lambda e, j=j: e.tensor_tensor(out=xg.t[:, j, 0:ntok], in0=gtmp[0].t[:, 0:ntok], in1=xg.t[:, j, 0:ntok], op=ALU.add),
                             r=[gtmp[0], xg], w=[xg])
            S.dma("sp", lambda e: e.dma_start(out=hscr[:, :, hoff:hoff + ntok].rearrange("c p t -> p c t"), in_=xg.t[:, :, 0:ntok]), r=[xg], w=[hbuf], sem=hsem)

        hsem = S.new_dsem("hscr")
        hbuf = Buf("hscr")

        npg = NPRE // GT
        for g in range(npg):
            mixer_group("pre", xpre[g * GT:(g + 1) * GT, :], GT, 0, last_prefix=(g == npg - 1))
        for g in range(NP // GT):
            mixer_group("main", xp[g * GT:(g + 1) * GT, :], GT, g * GT)
        for h in range(4):
            S.dma("sp", lambda e, h=h: e.dma_start(out=gla_p[h], in_=Sst[h].t[:, :]), r=[Sst[h]])
        S.dma("sp", lambda e: [e.dma_start(out=conv_p[:, b * 128:(b + 1) * 128].rearrange("t p -> p t"), in_=halo.t[:, b, :], allow_slow_non_contiguous=True)
                               for b in range(8)], r=[halo], n=8)
        mixer_group("samp", xs, NS, NP)
        cso = sb("cso", [NSEQ * 2, CH], F32)
        for hb in range(2):
            p = S.ps()
            for b4 in range(4):
                S.op("pe", lambda e, p=p, b4=b4, hb=hb: e.transpose(p.t[0:NSEQ * 2, b4 * 128:(b4 + 1) * 128], scv.t[:, hb * 4 + b4, :], ident_f), r=[scv] + CST, w=[p])
            S.op("dve", lambda e, p=p, hb=hb: e.tensor_copy(out=cso.t[:, hb * 512:(hb + 1) * 512], in_=p.t[0:NSEQ * 2, :]), r=[p], w=[cso])
        S.dma("sp", lambda e: e.dma_start(out=conv_s, in_=cso.t[:, :]), r=[cso])

        S.barrier()
        es_mix.close()
        S.cur = es
        NCH = (NT + 127) // 128
        NBLK = (CAP + 127) // 128
        BLKS = [(b * 128, min(128, CAP - b * 128)) for b in range(NBLK)]
        oscr = nc.dram_tensor("oscr", [E, 128, NBLK, D], BF16, kind="Internal").ap()
        obuf = Buf("oscr")
        iota_f = cs.t[:, 1152:1152 + CAP]
        mstrict_f = cs.t[:, 1024:1152]
        osc_sem = S.new_dsem("oscr")
        gwT = sb("gwT", [E, NT], F32)
        rkT = sb("rkT", [E, NT], F32)
        esel = sb("esel", [E, 128], F32)
        es_h = contextlib.ExitStack()
        S.cur = es_h
        alloc_wpool("C", 2)
        hntm = sb("hntm", [128, NCH, D], BF16)
        gwtm = sb("gwtm", [128, NCH, E], F32)
        mktm = sb("mktm", [128, NCH, E], F32)
        rktm = sb("rktm", [128, NCH, E], F32)
        top8 = sb("top8", [128, 8], F32)
        nmx = sb("nmx", [128, 1], F32)
        ssum = sb("ssum", [128, 1], F32)
        wr_f = sb("wr_f", [128, 16, E], F32)
        S.dma("sp", lambda e: e.dma_start(out=wr_f.t[:], in_=w_router.rearrange("(kc p) n -> p kc n", p=128)), w=[wr_f])
        wr = sb("wr", [128, 16, E], BF16)
        S.op("dve", lambda e: e.tensor_copy(out=wr.t[:], in_=wr_f.t[:]), r=[wr_f], w=[wr])
        br_f = sb("br_f", [1, E], F32)
        S.dma("sp", lambda e: e.dma_start(out=br_f.t[:], in_=b_router), w=[br_f])
        br = sb("br", [1, E], BF16)
        S.op("dve", lambda e: e.tensor_copy(out=br.t[:], in_=br_f.t[:]), r=[br_f], w=[br])
        S.op("dve", lambda e: e.memset(mktm.t[:], 0.0), w=[mktm])

        es_m1 = contextlib.ExitStack()
        S.cur = es_m1
        alloc_tmps("M1")
        hTt = sb("hTt", [128, 16, 512], F32)
        hnT = sb("hnT", [128, 16, 512], BF16)
        hnf = sb("hnf", [128, 2, 512], F32)
        lg = sb("lg", [128, E], F32)
        tiles = [(c, min(512, NP - c), False) for c in range(0, NP, 512)] + [(NP, NS, True)]
        for (t0, nn, smp) in tiles:
            S.dma("sp", lambda e, t0=t0, nn=nn: e.dma_start(out=hTt.t[:, :, 0:nn], in_=hscr[:, :, t0:t0 + nn].rearrange("c p t -> p c t")), w=[hTt], r=[hbuf])

            def post(dcf, dc, t0=t0, nn=nn):
                S.op("act", lambda e: e.activation(out=hnT.t[:, dcf, 0:nn], in_=hnf.t[:, dc, 0:nn], func=AF.Copy), r=[hnf], w=[hnT])
                p = S.ps()
                nsub = (nn + 127) // 128
                for c4 in range(nsub):
                    n = min(128, nn - c4 * 128)
                    S.op("pe", lambda e, p=p, c4=c4, n=n: e.transpose(p.t[0:n, c4 * 128:(c4 + 1) * 128], hnf.t[:, dc, c4 * 128:c4 * 128 + n], ident_f),
                         r=[hnf] + CST, w=[p])
                tc0 = t0 // 128
                if nn % 128 == 0:
                    S.op("dve", lambda e, p=p: e.tensor_copy(out=hntm.t[:, tc0:tc0 + nsub, dcf * 128:(dcf + 1) * 128],
                                                             in_=p.t[:, 0:nsub * 128].rearrange("p (c f) -> p c f", f=128)), r=[p], w=[hntm])
                else:
                    assert nsub == 1
                    S.op("dve", lambda e, p=p: e.tensor_copy(out=hntm.t[0:nn, tc0, dcf * 128:(dcf + 1) * 128], in_=p.t[0:nn, 0:128]), r=[p], w=[hntm])
            rmsnorm_fm(hTt, 0, nn, hnf, 0, A2, M_SH2, smp, post=post)
            for c0 in range(0, nn, 128):
                n = min(128, nn - c0)
                ch = (t0 + c0) // 128
                p = S.ps()
                for kc in range(16):
                    S.op("pe", lambda e, p=p, kc=kc, c0=c0, n=n: e.matmul(p.t[0:n, 0:E], hnT.t[:, kc, c0:c0 + n], wr.t[:, kc, :], start=(kc == 0), stop=False),
                         r=[hnT, wr], w=[p])
                S.op("pe", lambda e, p=p, n=n: e.matmul(p.t[0:n, 0:E], ones_b[0:1, 0:n], br.t[:, :], start=False, stop=True), r=[br] + CST, w=[p])
                S.op("dve", lambda e, p=p, n=n: e.tensor_copy(out=lg.t[0:n, :], in_=p.t[0:n, 0:E]), r=[p], w=[lg])
                S.op("dve", lambda e, n=n: e.max(out=top8.t[0:n, :], in_=lg.t[0:n, :]), r=[lg], w=[top8])
                S.op("dve", lambda e, n=n, ch=ch: e.tensor_scalar(out=mktm.t[0:n, ch, :], in0=lg.t[0:n, :], scalar1=top8.t[0:n, TOPK - 1:TOPK], scalar2=None, op0=ALU.is_ge),
                     r=[lg, top8], w=[mktm])
                S.op("dve", lambda e, n=n: e.tensor_scalar(out=nmx.t[0:n, :], in0=top8.t[0:n, 0:1], scalar1=-1.0, scalar2=None, op0=ALU.mult), r=[top8], w=[nmx])
                S.op("act", lambda e, n=n: e.activation(out=lg.t[0:n, :], in_=lg.t[0:n, :], func=AF.Exp, bias=nmx.t[0:n, :], scale=1.0), r=[lg, nmx], w=[lg])
                S.op("dve", lambda e, n=n, ch=ch: e.tensor_tensor(out=lg.t[0:n, :], in0=lg.t[0:n, :], in1=mktm.t[0:n, ch, :], op=ALU.mult), r=[lg, mktm], w=[lg])
                S.op("dve", lambda e, n=n: e.reduce_sum(out=ssum.t[0:n, :], in_=lg.t[0:n, :], axis=mybir.AxisListType.X), r=[lg], w=[ssum])
                S.op("dve", lambda e, n=n: e.reciprocal(out=ssum.t[0:n, :], in_=ssum.t[0:n, :]), r=[ssum], w=[ssum])
                S.op("dve", lambda e, n=n, ch=ch: e.tensor_scalar(out=gwtm.t[0:n, ch, :], in0=lg.t[0:n, :], scalar1=ssum.t[0:n, 0:1], scalar2=None, op0=ALU.mult),
                     r=[lg, ssum], w=[gwtm])
                p2 = S.ps()
                S.op("pe", lambda e, p2=p2, n=n, ch=ch: e.transpose(p2.t[0:E, 0:n], gwtm.t[0:n, ch, :], ident_f[0:n, 0:n]), r=[gwtm] + CST, w=[p2])
                S.op("dve", lambda e, p2=p2, n=n, ch=ch: e.tensor_copy(out=gwT.t[:, ch * 128:ch * 128 + n], in_=p2.t[0:E, 0:n]), r=[p2], w=[gwT])
        SCH = NCH - 1
        for ch in range(NCH):
            n = min(128, NT - ch * 128)
            p = S.ps()
            prev = [] if ch == SCH else [SCH] + list(range(ch))
            S.op("pe", lambda e, p=p, n=n, ch=ch, prev=prev: e.matmul(p.t[0:n, 0:E], mstrict_f[0:n, 0:n], mktm.t[0:n, ch, :], start=True, stop=(len(prev) == 0)),
                 r=[mktm] + CST, w=[p])
            for i2, c2 in enumerate(prev):
                k2 = min(128, NT - c2 * 128)
                S.op("pe", lambda e, p=p, n=n, c2=c2, k2=k2, i2=i2, prev=prev: e.matmul(p.t[0:n, 0:E], ones_f[0:k2, 0:n], mktm.t[0:k2, c2, :], start=False,
                                                                                      stop=(i2 == len(prev) - 1)), r=[mktm] + CST, w=[p])
            S.op("dve", lambda e, p=p, n=n, ch=ch: e.scalar_tensor_tensor(out=rktm.t[0:n, ch, :], in0=p.t[0:n, 0:E], scalar=1.0, in1=mktm.t[0:n, ch, :],
                                                                         op0=ALU.add, op1=ALU.mult), r=[p, mktm], w=[rktm])
            S.op("dve", lambda e, n=n, ch=ch: e.tensor_scalar(out=rktm.t[0:n, ch, :], in0=rktm.t[0:n, ch, :], scalar1=-1.0, scalar2=None, op0=ALU.add),
                 r=[rktm], w=[rktm])
            p2 = S.ps()
            S.op("pe", lambda e, p2=p2, n=n, ch=ch: e.transpose(p2.t[0:E, 0:n], rktm.t[0:n, ch, :], ident_f[0:n, 0:n]), r=[rktm] + CST, w=[p2])
            S.op("dve", lambda e, p2=p2, n=n, ch=ch: e.tensor_copy(out=rkT.t[:, ch * 128:ch * 128 + n], in_=p2.t[0:E, 0:n]), r=[p2], w=[rkT])
        S.barrier()
        es_m1.close()

        es_m2 = contextlib.ExitStack()
        S.cur = es_m2
        sel = sb("sel", [128, NCH, CAP], BF16)
        xbT = sb("xbT", [128, 16, CAP], BF16)
        actT = sb("actT", [128, FC, CAP], BF16)
        oute = [sb("oute%d" % i, [128, NBLK, D], BF16) for i in range(1)]
        bupT = sb("bupT", [128, 2 * FC], F32)
        bstg = sb("bstg", [128, 128], F32)
        mt = [sb("mt%d" % i, [128, CAP], F32) for i in range(3)]
        gs = sb("gs", [128, 4, CAP], F32)
        S.op("dve", lambda e: e.memset(oute[0].t[:], 0.0), w=[oute[0]])
        for ex in range(E):
            S.dma("sp", lambda e, ex=ex: e.dma_start(out=bstg.t[0:2 * FC, :], in_=b_up[ex * 2 * FC:(ex + 1) * 2 * FC, :]), w=[bstg])
            p = S.ps()
            S.op("pe", lambda e, p=p: e.transpose(p.t[:, 0:2 * FC], bstg.t[0:2 * FC, :], ident_f[0:2 * FC, 0:2 * FC]), r=[bstg] + CST, w=[p])
            S.op("dve", lambda e, p=p: e.tensor_copy(out=bupT.t[:, :], in_=p.t[:, 0:2 * FC]), r=[p], w=[bupT])
            for ch in range(NCH):
                n = min(128, NT - ch * 128)
                S.op("dve", lambda e, n=n, ch=ch, ex=ex: e.tensor_scalar(out=sel.t[0:n, ch, :], in0=iota_f[0:n, :], scalar1=rktm.t[0:n, ch, ex:ex + 1], scalar2=None,
                                                                        op0=ALU.is_equal), r=[rktm] + CST, w=[sel])
            for f in range(16):
                p = S.ps()
                for ch in range(NCH):
                    n = min(128, NT - ch * 128)
                    S.op("pe", lambda e, p=p, f=f, ch=ch, n=n: e.matmul(p.t[:, 0:CAP], hntm.t[0:n, ch, f * 128:(f + 1) * 128], sel.t[0:n, ch, :],
                                                                       start=(ch == 0), stop=(ch == NCH - 1)), r=[hntm, sel], w=[p])
                S.op("act" if f % 2 else "dve",
                     (lambda e, p=p, f=f: e.activation(out=xbT.t[:, f, :], in_=p.t[:, 0:CAP], func=AF.Copy)) if f % 2 else
                     (lambda e, p=p, f=f: e.tensor_copy(out=xbT.t[:, f, :], in_=p.t[:, 0:CAP])), r=[p], w=[xbT])
            vsrc = w_up[ex].rearrange("(kc p) n -> p kc n", p=128)
            for f4 in range(0, FC, 4):
                nb = min(4, FC - f4)
                Wg = wtile()
                S.dma("pool", lambda e, W=Wg, f4=f4, nb=nb, vsrc=vsrc: e.dma_start(out=W.t[:, :, 0:nb * 128], in_=vsrc[:, :, f4 * 128:(f4 + nb) * 128]), w=[Wg])
                for fb in range(nb):
                    f = f4 + fb
                    pg = S.ps()
                    for kc in range(16):
                        S.op("pe", lambda e, pg=pg, kc=kc, W=Wg, fb=fb: e.matmul(pg.t[:, 0:CAP], W.t[:, kc, fb * 128:(fb + 1) * 128], xbT.t[:, kc, :],
                                                                                start=(kc == 0), stop=(kc == 15)), r=[Wg, xbT], w=[pg])
                    S.op("dve", lambda e, pg=pg, f=f: e.tensor_scalar(out=mt[0].t[:, :], in0=pg.t[:, 0:CAP], scalar1=bupT.t[:, f:f + 1], scalar2=LIMIT,
                                                                     op0=ALU.add, op1=ALU.min), r=[pg, bupT], w=[mt[0]])
                    S.op("act", lambda e: e.activation(out=mt[1].t[:, :], in_=mt[0].t[:, :], func=AF.Sigmoid, scale=ALPHA), r=[mt[0]], w=[mt[1]])
                    S.op("dve", lambda e, fb=fb: e.tensor_tensor(out=gs.t[:, fb, :], in0=mt[0].t[:, :], in1=mt[1].t[:, :], op=ALU.mult), r=[mt[0], mt[1]], w=[gs])
                Wl = wtile()
                S.dma("pool", lambda e, W=Wl, f4=f4, nb=nb, vsrc=vsrc: e.dma_start(out=W.t[:, :, 0:nb * 128], in_=vsrc[:, :, FF + f4 * 128:FF + (f4 + nb) * 128]), w=[Wl])
                for fb in range(nb):
                    f = f4 + fb
                    pl = S.ps()
                    for kc in range(16):
                        S.op("pe", lambda e, pl=pl, kc=kc, W=Wl, fb=fb: e.matmul(pl.t[:, 0:CAP], W.t[:, kc, fb * 128:(fb + 1) * 128], xbT.t[:, kc, :],
                                                                                start=(kc == 0), stop=(kc == 15)), r=[Wl, xbT], w=[pl])
                    S.op("dve", lambda e, pl=pl, f=f: e.tensor_scalar(out=mt[2].t[:, :], in0=pl.t[:, 0:CAP], scalar1=bupT.t[:, FC + f:FC + f + 1], scalar2=LIMIT,
                                                                     op0=ALU.add, op1=ALU.min), r=[pl, bupT], w=[mt[2]])
                    S.op("dve", lambda e: e.tensor_scalar(out=mt[2].t[:, :], in0=mt[2].t[:, :], scalar1=-LIMIT, scalar2=1.0, op0=ALU.max, op1=ALU.add),
                         r=[mt[2]], w=[mt[2]])
                    S.op("dve", lambda e, f=f, fb=fb: e.tensor_tensor(out=actT.t[:, f, :], in0=gs.t[:, fb, :], in1=mt[2].t[:, :], op=ALU.mult), r=[gs, mt[2]], w=[actT])
            ot = oute[0]
            for t4 in range(4):
                W = wtile()
                vsrc = w_down[ex].rearrange("(kc p) n -> p kc n", p=128)
                S.dma("pool", lambda e, W=W, t4=t4, vsrc=vsrc: e.dma_start(out=W.t[:, 0:FC, :], in_=vsrc[:, :, t4 * 512:(t4 + 1) * 512]), w=[W])
                for blk, (b0, bn) in enumerate(BLKS):
                    py = S.ps()
                    for fc in range(FC):
                        S.op("pe", lambda e, py=py, fc=fc, W=W, b0=b0, bn=bn: e.matmul(py.t[0:bn, :], actT.t[:, fc, b0:b0 + bn], W.t[:, fc, :],
                                                                                     start=(fc == 0), stop=(fc == FC - 1)), r=[W, actT], w=[py])
                    S.op("act" if blk % 2 else "dve",
                         (lambda e, py=py, blk=blk, bn=bn, t4=t4, ot=ot: e.activation(out=ot.t[0:bn, blk, t4 * 512:(t4 + 1) * 512], in_=py.t[0:bn, :], func=AF.Copy)) if blk % 2 else
                         (lambda e, py=py, blk=blk, bn=bn, t4=t4, ot=ot: e.tensor_copy(out=ot.t[0:bn, blk, t4 * 512:(t4 + 1) * 512], in_=py.t[0:bn, :])), r=[py], w=[ot])
            S.dma("sp", lambda e, ex=ex, ot=ot: e.dma_start(out=oscr[ex], in_=ot.t[:, :, :]), r=[ot], w=[obuf], sem=osc_sem)
        S.barrier()
        es_m2.close()
        es_h.close()

        S.cur = es
        hT = sb("hT", [128, 16, NT], F32)
        oin = [sb("oin%d" % i, [128, NBLK, D], BF16) for i in range(2)]
        selT = [sb("selT%d" % i, [128, NBLK, NT], BF16) for i in range(1)]
        gwr = [sb("gwr%d" % i, [128, NT], F32) for i in range(1)]
        ct = [sb("ct%d" % i, [128, NS], F32) for i in range(2)]
        yo = sb("yo", [128, D], F32)
        bdn = sb("bdn", [E, D], F32)
        alloc_tmps("M3")
        S.dma("sp", lambda e: e.dma_start(out=hT.t[:, :, :], in_=hscr.rearrange("c p t -> p c t")), w=[hT], r=[hbuf])
        S.dma("sp", lambda e: e.dma_start(out=bdn.t[:, :], in_=b_down.rearrange("(e a) b -> e (a b)", a=16)), w=[bdn])
        ctiles = [(c, min(512, NP - c)) for c in range(0, NP, 512)] + [(NP, NS)]

        def accum(py, j, c0, nn, k):
            if c0 < NP:
                S.op("dve", lambda e: e.scalar_tensor_tensor(out=hT.t[:, j, c0:c0 + nn], in0=py.t[:, 0:nn], scalar=modT.t[:, M_G2 + j, NSEQ:NSEQ + 1],
                                                             in1=hT.t[:, j, c0:c0 + nn], op0=ALU.mult, op1=ALU.add), r=[py, modT, hT], w=[hT])
            else:
                cc_ = ct[k % 2]
                S.op("dve", lambda e: e.tensor_tensor(out=cc_.t[:, 0:nn].rearrange("p (s t) -> p s t", t=4), in0=py.t[:, 0:nn].rearrange("p (s t) -> p s t", t=4),
                                                      in1=modT.t[:, M_G2 + j, 0:NSEQ].unsqueeze(2).to_broadcast([128, NSEQ, 4]), op=ALU.mult),
                     r=[py, modT], w=[cc_])
                S.op("dve", lambda e: e.tensor_tensor(out=hT.t[:, j, c0:c0 + nn], in0=cc_.t[:, 0:nn], in1=hT.t[:, j, c0:c0 + nn], op=ALU.add),
                     r=[cc_, hT], w=[hT])

        for j in range(16):
            for (c0, nn) in ctiles:
                py = S.ps()
                S.op("pe", lambda e, py=py, j=j, c0=c0, nn=nn: e.matmul(py.t[:, 0:nn], bdn.t[:, j * 128:(j + 1) * 128], gwT.t[:, c0:c0 + nn], start=True, stop=True),
                     r=[bdn, gwT], w=[py])
                accum(py, j, c0, nn, j)
        for ex in range(E):
            oi, sT, gr = oin[ex % 2], selT[0], gwr[0]
            S.dma("sp", lambda e, ex=ex, oi=oi: e.dma_start(out=oi.t[:, :, :], in_=oscr[ex]), w=[oi], r=[obuf])
            S.op("dve", lambda e, ex=ex: e.tensor_copy(out=esel.t[:, :], in_=cs.t[0:E, ex:ex + 1].to_broadcast([E, 128])), r=[cs], w=[esel])
            for (c0, nn) in ctiles:
                p = S.ps()
                S.op("pe", lambda e, p=p, c0=c0, nn=nn: e.matmul(p.t[:, 0:nn], esel.t[:, :], gwT.t[:, c0:c0 + nn], start=True, stop=True), r=[esel, gwT], w=[p])
                S.op("act", lambda e, p=p, c0=c0, nn=nn, gr=gr: e.activation(out=gr.t[:, c0:c0 + nn], in_=p.t[:, 0:nn], func=AF.Copy), r=[p], w=[gr])
                p = S.ps()
                S.op("pe", lambda e, p=p, c0=c0, nn=nn: e.matmul(p.t[:, 0:nn], esel.t[:, :], rkT.t[:, c0:c0 + nn], start=True, stop=True), r=[esel, rkT], w=[p])
                for blk, (b0, bn) in enumerate(BLKS):
                    S.op("dve", lambda e, p=p, c0=c0, nn=nn, blk=blk, bn=bn, sT=sT, gr=gr: e.scalar_tensor_tensor(out=sT.t[0:bn, blk, c0:c0 + nn], in0=p.t[0:bn, 0:nn],
                                                                                                                 scalar=cs.t[0:bn, 960 + blk:961 + blk], in1=gr.t[0:bn, c0:c0 + nn],
                                                                                                                 op0=ALU.is_equal, op1=ALU.mult), r=[p, gr] + CST, w=[sT])
            for j in range(16):
                for (c0, nn) in ctiles:
                    py = S.ps()
                    for blk, (b0, bn) in enumerate(BLKS):
                        S.op("pe", lambda e, py=py, blk=blk, bn=bn, j=j, c0=c0, nn=nn, oi=oi, sT=sT: e.matmul(py.t[:, 0:nn], oi.t[0:bn, blk, j * 128:(j + 1) * 128], sT.t[0:bn, blk, c0:c0 + nn],
                                                                                                             start=(blk == 0), stop=(blk == NBLK - 1)), r=[oi, sT], w=[py])
                    accum(py, j, c0, nn, j)
        osem = S.new_dsem("yout")
        for (c0, nn) in ctiles:
            rmsnorm_fm(hT, c0, nn, hT, c0, nfT, None, False)
        for c0 in range(0, NT, 128):
            n = min(128, NT - c0)
            for q4 in range(4):
                p = S.ps()
                for i in range(4):
                    dc = q4 * 4 + i
                    S.op("pe", lambda e, p=p, i=i, dc=dc, c0=c0, n=n: e.transpose(p.t[0:n, i * 128:(i + 1) * 128], hT.t[:, dc, c0:c0 + n], ident_f), r=[hT] + CST, w=[p])
                S.op("act" if q4 % 2 else "dve",
                     (lambda e, p=p, q4=q4, n=n: e.activation(out=yo.t[0:n, q4 * 512:(q4 + 1) * 512], in_=p.t[0:n, :], func=AF.Copy)) if q4 % 2 else
                     (lambda e, p=p, q4=q4, n=n: e.tensor_copy(out=yo.t[0:n, q4 * 512:(q4 + 1) * 512], in_=p.t[0:n, :])), r=[p], w=[yo])
            if c0 < NP:
                S.dma("sp", lambda e, c0=c0, n=n: e.dma_start(out=y_p[c0:c0 + n, :], in_=yo.t[0:n, :]), r=[yo], sem=osem)
            else:
                S.dma("sp", lambda e, c0=c0, n=n: e.dma_start(out=y_s[c0 - NP:c0 - NP + n, :], in_=yo.t[0:n, :]), r=[yo], sem=osem)

        def fin(e):
            for d in S.dsems:
                if d[1] > 0:
                    e.wait_ge(d[0], d[1])
            return e.nop()
        S.op("sp", fin)

        S.assign()
        with nc.Block() as block:
            @block.tensor
            def _(e):
                S.run("pe", e)

            @block.vector
            def _(e):
                S.run("dve", e)

            @block.scalar
            def _(e):
                S.run("act", e)

            @block.gpsimd
            def _(e):
                S.run("pool", e)

            @block.sync
            def _(e):
                S.run("sp", e)
    return nc


def make_cst(NSEQ):
    c = np.zeros((128, 1664), np.float32)
    i = np.arange(128)
    c[:, 0:128] = np.eye(128, dtype=np.float32)
    c[:, 128:256] = (i[:, None] <= i[None, :]).astype(np.float32)
    c[:, 256:384] = (i[:, None] > i[None, :]).astype(np.float32)
    j = np.arange(64)
    same = (j[:, None] // 4) == (j[None, :] // 4)
    c[0:64, 384:448] = (same & (j[:, None] <= j[None, :])).astype(np.float32)
    c[0:64, 448:512] = (same & (j[:, None] > j[None, :])).astype(np.float32)
    c[:, 512:640] = 1.0
    c[:, 640] = EPS
    c[:, 641] = 1.0
    for s in range(NSEQ):
        c[s * 4:(s + 1) * 4, 642 + s] = 1.0
    c[:, 1152:1664] = np.arange(512, dtype=np.float32)[None, :]
    c[:, 960] = i
    c[:, 961] = i + 128
    c[:, 962] = i + 256
    c[:, 963] = i + 384
    c[:, 1024:1152] = (i[:, None] < i[None, :]).astype(np.float32)
    return c


def make_in_maps(inp, cfg, n_cores, seq_len, n_seq_prompt):
    NP, NPRE, NSEQ, E, FF = cfg["NP"], cfg["NPRE"], cfg["NSEQ"], cfg["E"], cfg["FF"]
    FC = FF // 128
    f = lambda a: np.ascontiguousarray(a, dtype=np.float32)
    shared = dict(
        cst=make_cst(NSEQ),
        w_ada=f(inp["w_ada"][0]), b_ada=f(inp["b_ada"][0].reshape(96, 128)), norm1_w=f(inp["norm1_w"][0].reshape(16, 128)),
        w_in=f(inp["w_in"][0]), w_gk_up=f(inp["w_gk_up"][0]), b_gk=f(inp["b_gk"][0].reshape(1, 512)),
        gla_norm_w=f(inp["gla_norm_w"][0].reshape(2, 128)), w_conv=f(inp["w_conv"][0].reshape(24, 128)), w_out=f(inp["w_out"][0]),
        norm2_w=f(inp["norm2_w"][0].reshape(16, 128)), w_router=f(inp["w_router"][0]), b_router=f(inp["b_router"][0].reshape(1, E)),
        w_up=f(inp["w_up"][0]), b_up=f(inp["b_up"][0].reshape(E * 2 * FC, 128)), w_down=f(inp["w_down"][0]),
        b_down=f(inp["b_down"][0].reshape(E * 16, 128)), final_norm_w=f(inp["final_norm_w"].reshape(16, 128)),
    )
    maps = []
    for c in range(n_cores):
        b, half = c // 2, c % 2
        m = dict(shared)
        m["xp"] = f(inp["x_prompt"][b, half * NP:(half + 1) * NP])
        m["xpre"] = f(inp["x_prompt"][b, 0:NPRE])
        m["pflag"] = np.full((128, 1), float(half), np.float32)
        m["xs"] = f(inp["x_sample"][c * NSEQ:(c + 1) * NSEQ].reshape(NSEQ * 4, D))
        m["cvec"] = f(np.concatenate([inp["c_sample"][c * NSEQ:(c + 1) * NSEQ], inp["c_prompt"][b:b + 1]], axis=0))
        m["sgla"] = f(inp["state_gla"][0, c * NSEQ:(c + 1) * NSEQ])
        m["sconv"] = f(inp["state_conv"][0, c * NSEQ:(c + 1) * NSEQ].reshape(NSEQ * 2, CH))
        maps.append(m)
    return maps


def gather_outputs(res, cfg, n_cores, n_seq_prompt, seq_len):
    NP, NSEQ = cfg["NP"], cfg["NSEQ"]
    y_p = np.zeros((n_seq_prompt, seq_len, D), np.float32)
    y_s = np.zeros((n_cores * NSEQ, 4, D), np.float32)
    gla_p = np.zeros((1, n_seq_prompt, HEADS, HK, HV), np.float32)
    conv_p = np.zeros((1, n_seq_prompt, 2, CH), np.float32)
    gla_s = np.zeros((1, n_cores * NSEQ, HEADS, HK, HV), np.float32)
    conv_s = np.zeros((1, n_cores * NSEQ, 2, CH), np.float32)
    for c in range(n_cores):
        b, half = c // 2, c % 2
        r = res[c]
        y_p[b, half * NP:(half + 1) * NP] = r["y_p"]
        y_s[c * NSEQ:(c + 1) * NSEQ] = r["y_s"].reshape(NSEQ, 4, D)
        if half == 1:
            gla_p[0, b] = r["gla_p"]
            conv_p[0, b] = r["conv_p"]
        gla_s[0, c * NSEQ:(c + 1) * NSEQ] = r["gla_s"]
        conv_s[0, c * NSEQ:(c + 1) * NSEQ] = r["conv_s"].reshape(NSEQ, 2, CH)
    return (y_p, y_s, gla_p, conv_p, gla_s, conv_s)


def kernel(**inputs):
    cfg = FULL
    n_cores = 8
    nc = build(cfg)
    maps = make_in_maps(inputs, cfg, n_cores, 2048, 4)
    res = run_bass_kernel_spmd(nc, maps, core_ids=list(range(n_cores)))
    return gather_outputs(res.results, cfg, n_cores, 4, 2048)
```

```python
import contextlib
import numpy as np
import concourse.bass as bass
import concourse.mybir as mybir
from concourse.bass_utils import run_bass_kernel_spmd

F32 = mybir.dt.float32
BF16 = mybir.dt.bfloat16
AF = mybir.ActivationFunctionType
ALU = mybir.AluOpType

D = 2048
DC = 16
HEADS = 4
HK = 128
HV = 256
DV = 1024
CH = 1024
DIN = 10256
C_Q, C_K, C_V, C_AL, C_G, C_CB, C_CC, C_CHH, C_GA, C_GB = 0, 512, 1024, 2048, 2064, 3088, 4112, 5136, 6160, 8208
EPS = 1e-6
LIMIT = 7.0
ALPHA = 1.702

FULL = dict(NP=1024, NPRE=1024, GT=256, NSEQ=16, E=32, FF=2048, TOPK=4, CAP=448)


class Buf:
    def __init__(self, name):
        self.name = name
        self.w = None
        self.r = []


class Tile:
    def __init__(self, name, t):
        self.name = name
        self.t = t
        self.buf = Buf(name)
        self.dsem = None


class Op:
    __slots__ = ("eng", "fn", "deps", "idx", "signal", "ordinal", "is_dma", "sem", "val", "n")


COMPUTE = ("pe", "act", "dve", "pool")


class Sched:
    def __init__(self, nc, es):
        self.nc = nc
        self.es = es
        self.ops = {k: [] for k in ("pe", "act", "dve", "pool", "sp")}
        self.seen = {k: {} for k in self.ops}
        self.esem = {k: es.enter_context(nc.semaphore("sem_" + k)) for k in COMPUTE}
        self.dsems = []
        self.psum = []
        self.psi = 0
        self.cur = es
        self.pending = {k: [] for k in self.ops}

    def sb(self, name, shape, dt):
        t = self.cur.enter_context(self.nc.sbuf_tensor("s_" + name, list(shape), dt))
        return Tile(name, t)

    def barrier(self):
        deps = []
        for k in COMPUTE:
            if self.ops[k]:
                deps.append(self.ops[k][-1])
        for d in self.dsems:
            if len(d) > 2 and d[2] is not None:
                deps.append(d[2])
        for k in self.ops:
            self.pending[k] = list(deps)

    def init_psum(self, n=8):
        for i in range(n):
            t = self.es.enter_context(self.nc.psum_tensor("ps%d" % i, [128, 512], F32))
            self.psum.append(Tile("ps%d" % i, t))

    def ps(self):
        p = self.psum[self.psi % len(self.psum)]
        self.psi += 1
        return p

    def new_dsem(self, name):
        s = self.es.enter_context(self.nc.semaphore("dsem_" + name))
        d = [s, 0, None]
        self.dsems.append(d)
        return d

    def _bufs(self, lst):
        out = []
        for x in lst:
            out.append(x.buf if isinstance(x, Tile) else x)
        return out

    def _add(self, eng, fn, r, w, is_dma=False, dsem=None, n=1):
        op = Op()
        op.eng, op.fn, op.is_dma, op.n = eng, fn, is_dma, n
        op.signal = False
        op.ordinal = None
        op.idx = len(self.ops[eng])
        rb, wb = self._bufs(r), self._bufs(w)
        deps = []
        for b in rb:
            if b.w is not None:
                deps.append(b.w)
        for b in wb:
            if b.w is not None:
                deps.append(b.w)
            deps.extend(b.r)
        if self.pending[eng]:
            deps.extend(self.pending[eng])
            self.pending[eng] = []
        keep = []
        seen = self.seen[eng]
        for d in deps:
            if d is op:
                continue
            if d.is_dma:
                key = ("d", id(d.sem))
                if seen.get(key, 0) >= d.val:
                    continue
                seen[key] = d.val
                keep.append(d)
            else:
                if d.eng == eng and eng == "pe":
                    continue
                if d.is_dma:
                    continue
                key = ("c", d.eng)
                if seen.get(key, -1) >= d.idx:
                    continue
                seen[key] = d.idx
                d.signal = True
                keep.append(d)
        op.deps = keep
        if is_dma:
            dsem[1] += 16 * n
            op.sem, op.val = dsem, dsem[1]
            dsem[2] = op
        for b in wb:
            b.w = op
            b.r = []
        for b in rb:
            if b not in wb:
                b.r.append(op)
        self.ops[eng].append(op)
        return op

    def op(self, eng, fn, r=(), w=()):
        return self._add(eng, fn, list(r), list(w))

    def dma(self, q, fn, r=(), w=(), sem=None, n=1):
        if sem is None:
            tl = [x for x in list(w) + list(r) if isinstance(x, Tile)][0]
            if tl.dsem is None:
                tl.dsem = self.new_dsem(tl.name)
            sem = tl.dsem
        return self._add(q, fn, list(r), list(w), is_dma=True, dsem=sem, n=n)

    def emit(self, eng, e):
        ordn = 0
        for op in self.ops[eng]:
            if op.signal:
                ordn += 1
                op.ordinal = ordn

    def assign(self):
        for eng in COMPUTE:
            ordn = 0
            for op in self.ops[eng]:
                if op.signal:
                    ordn += 1
                    op.ordinal = ordn

    def run(self, eng, e):
        for op in self.ops[eng]:
            waits = {}
            for d in op.deps:
                if d.is_dma:
                    k = id(d.sem)
                    if k not in waits or waits[k][1] < d.val:
                        waits[k] = (d.sem[0], d.val)
                else:
                    k = d.eng
                    if k not in waits or waits[k][1] < d.ordinal:
                        waits[k] = (self.esem[d.eng], d.ordinal)
            for sem, val in waits.values():
                e.wait_ge(sem, val)
            insts = op.fn(e)
            if not isinstance(insts, (list, tuple)):
                insts = [insts]
            if op.is_dma:
                assert len(insts) == op.n, (len(insts), op.n)
                for i in insts:
                    i.then_inc(op.sem[0], 16)
            elif op.signal:
                insts[-1].then_inc(self.esem[eng], 1)


def build(cfg, debug=()):
    NP, NPRE, GT, NSEQ, E, FF, TOPK, CAP = (cfg[k] for k in ("NP", "NPRE", "GT", "NSEQ", "E", "FF", "TOPK", "CAP"))
    NS = NSEQ * 4
    NT = NP + NS
    FC = FF // 128
    NR = NSEQ + 1
    nc = bass.Bass("TRN2", target_bir_lowering=False)

    def din(name, shape):
        return nc.dram_tensor(name, list(shape), F32, kind="ExternalInput").ap()

    def dout(name, shape):
        return nc.dram_tensor(name, list(shape), F32, kind="ExternalOutput").ap()

    xp = din("xp", [NP, D])
    xpre = din("xpre", [NPRE, D])
    xs = din("xs", [NS, D])
    cvec = din("cvec", [NR, D])
    pflag = din("pflag", [128, 1])
    sgla = din("sgla", [NSEQ, HEADS, HK, HV])
    sconv = din("sconv", [NSEQ * 2, CH])
    cst = din("cst", [128, 1664])
    w_ada = din("w_ada", [D, 6 * D])
    b_ada = din("b_ada", [96, 128])
    norm1_w = din("norm1_w", [16, 128])
    w_in = din("w_in", [D, DIN])
    w_gk_up = din("w_gk_up", [16, 512])
    b_gk = din("b_gk", [1, 512])
    gla_norm_w = din("gla_norm_w", [2, 128])
    w_conv = din("w_conv", [24, 128])
    w_out = din("w_out", [2048, D])
    norm2_w = din("norm2_w", [16, 128])
    w_router = din("w_router", [D, E])
    b_router = din("b_router", [1, E])
    w_up = din("w_up", [E, D, 2 * FF])
    b_up = din("b_up", [E * 2 * FC, 128])
    w_down = din("w_down", [E, FF, D])
    b_down = din("b_down", [E * DC, 128])
    final_norm_w = din("final_norm_w", [16, 128])

    y_p = dout("y_p", [NP, D])
    y_s = dout("y_s", [NS, D])
    gla_p = dout("gla_p", [HEADS, HK, HV])
    conv_p = dout("conv_p", [2, CH])
    gla_s = dout("gla_s", [NSEQ, HEADS, HK, HV])
    conv_s = dout("conv_s", [NSEQ * 2, CH])
    hscr = nc.dram_tensor("hscr", [DC, 128, NT], F32, kind="Internal").ap()
    dbg = {k: dout("dbg_" + k, shp) for k, shp in debug}

    es = contextlib.ExitStack()
    with es:
        S = Sched(nc, es)
        S.init_psum(8)
        sb = S.sb

        cs = sb("cst", [128, 1664], F32)
        S.dma("sp", lambda e: e.dma_start(out=cs.t[:], in_=cst), w=[cs])
        ident_f = cs.t[:, 0:128]
        eps_c = cs.t[:, 640:641]
        one_c = cs.t[:, 641:642]
        cb = sb("cstb", [128, 640], BF16)
        S.op("dve", lambda e: e.tensor_copy(out=cb.t[:], in_=cs.t[:, 0:640]), r=[cs], w=[cb])
        ident_b = cb.t[:, 0:128]
        maskT_b = cb.t[:, 128:256]
        ones_b = cb.t[:, 512:640]
        maskT_f = cs.t[:, 128:256]
        mrev_f = cs.t[:, 256:384]
        smask_f = cs.t[:, 384:448]
        smrev_f = cs.t[:, 448:512]
        ones_f = cs.t[:, 512:640]
        CST = [cs, cb]

        def load_vecT(name, src, n):
            stg = sb(name + "_stg", [128, 128], F32)
            dst = sb(name, [128, n], F32)
            S.dma("sp", lambda e: e.dma_start(out=stg.t[0:n, :], in_=src), w=[stg])
            p = S.ps()
            S.op("pe", lambda e: e.transpose(p.t[:, 0:n], stg.t[0:n, :], ident_f[0:n, 0:n]), r=[stg] + CST, w=[p])
            S.op("dve", lambda e: e.tensor_copy(out=dst.t[:, :], in_=p.t[:, 0:n]), r=[p], w=[dst])
            return dst

        badaT = load_vecT("badaT", b_ada, 96)
        n1T = load_vecT("n1T", norm1_w, 16)
        n2T = load_vecT("n2T", norm2_w, 16)
        nfT = load_vecT("nfT", final_norm_w, 16)
        gnT = load_vecT("gnT", gla_norm_w, 2)
        wcT = load_vecT("wcT", w_conv, 24)
        pfl = sb("pfl", [128, 1], F32)
        S.dma("sp", lambda e: e.dma_start(out=pfl.t[:], in_=pflag), w=[pfl])

        wgk_f = sb("wgk_f", [16, 512], F32)
        S.dma("sp", lambda e: e.dma_start(out=wgk_f.t[:], in_=w_gk_up), w=[wgk_f])
        wgk = sb("wgk", [16, 512], BF16)
        S.op("dve", lambda e: e.tensor_copy(out=wgk.t[:], in_=wgk_f.t[:]), r=[wgk_f], w=[wgk])
        bgk_f = sb("bgk_f", [1, 512], F32)
        S.dma("sp", lambda e: e.dma_start(out=bgk_f.t[:], in_=b_gk), w=[bgk_f])
        bgk = sb("bgk", [1, 512], BF16)
        S.op("dve", lambda e: e.tensor_copy(out=bgk.t[:], in_=bgk_f.t[:]), r=[bgk_f], w=[bgk])

        NW = 3
        wpool = []
        wstate = [0]
        nw_cur = [NW]

        def alloc_wpool(tag, n=NW):
            wpool[:] = [sb("wt%s%d" % (tag, i), [128, 16, 512], BF16) for i in range(n)]
            nw_cur[0] = n

        def wtile():
            t = wpool[wstate[0] % nw_cur[0]]
            wstate[0] += 1
            return t

        def load_w(src2d, c0, ncols, kc=16, t=None, col_off=0):
            if t is None:
                t = wtile()
            v = src2d.rearrange("(kc p) n -> p kc n", p=128)
            S.dma("pool", lambda e: e.dma_start(out=t.t[:, 0:kc, col_off:col_off + ncols], in_=v[:, :, c0:c0 + ncols]), w=[t])
            return t

        siluT = sb("siluT", [128, 16, NR], BF16)
        modT = sb("modT", [128, 96, NR], F32)
        A1 = sb("A1", [128, 16, NR], F32)
        A2 = sb("A2", [128, 16, NR], F32)
        es_ada = contextlib.ExitStack()
        S.cur = es_ada
        alloc_wpool("A", 4)
        cv = sb("cv", [NR, D], F32)
        S.dma("sp", lambda e: e.dma_start(out=cv.t[:], in_=cvec), w=[cv])
        cvs = sb("cvs", [NR, D], F32)
        S.op("act", lambda e: e.activation(out=cvs.t[:], in_=cv.t[:], func=AF.Silu), r=[cv], w=[cvs])
        p = S.ps()
        for kc in range(16):
            S.op("pe", lambda e, kc=kc, p=p: e.transpose(p.t[:, kc * NR:(kc + 1) * NR], cvs.t[0:NR, kc * 128:(kc + 1) * 128],
                                                       ident_f[0:NR, 0:NR]), r=[cvs] + CST, w=[p])
        S.op("dve", lambda e, p=p: e.tensor_copy(out=siluT.t[:].rearrange("p a b -> p (a b)"), in_=p.t[:, 0:16 * NR]), r=[p], w=[siluT])
        for ct in range(24):
            W = load_w(w_ada, ct * 512, 512)
            for sbk in range(4):
                nb = ct * 4 + sbk
                p = S.ps()
                for kc in range(16):
                    S.op("pe", lambda e, kc=kc, p=p, W=W, sbk=sbk: e.matmul(p.t[:, 0:NR], W.t[:, kc, sbk * 128:(sbk + 1) * 128],
                                                                            siluT.t[:, kc, :], start=(kc == 0), stop=(kc == 15)),
                         r=[W, siluT], w=[p])
                S.op("act", lambda e, p=p, nb=nb: e.activation(out=modT.t[:, nb, :], in_=p.t[:, 0:NR], func=AF.Identity,
                                                               bias=badaT.t[:, nb:nb + 1], scale=1.0), r=[p, badaT], w=[modT])
        for dc in range(16):
            S.op("dve", lambda e, dc=dc: e.tensor_scalar(out=A1.t[:, dc, :], in0=modT.t[:, 16 + dc, :], scalar1=1.0,
                                                         scalar2=n1T.t[:, dc:dc + 1], op0=ALU.add, op1=ALU.mult), r=[modT, n1T], w=[A1])
            S.op("dve", lambda e, dc=dc: e.tensor_scalar(out=A2.t[:, dc, :], in0=modT.t[:, 64 + dc, :], scalar1=1.0,
                                                         scalar2=n2T.t[:, dc:dc + 1], op0=ALU.add, op1=ALU.mult), r=[modT, n2T], w=[A2])
        M_SH1, M_G1, M_SH2, M_G2 = 0, 32, 48, 80
        S.barrier()
        es_ada.close()
        S.cur = es

        xstage = []
        xst = [0]

        def load_xT(src, ntok, dst, toff):
            for tc0 in range(0, ntok, 128):
                n = min(128, ntok - tc0)
                stg = xstage[0]
                xst[0] += 1
                S.dma("sp", lambda e, stg=stg, tc0=tc0, n=n: e.dma_start(out=stg.t[0:n, :], in_=src[tc0:tc0 + n, :]), w=[stg])
                for q4 in range(4):
                    p = S.ps()
                    for i in range(4):
                        dc = q4 * 4 + i
                        S.op("pe", lambda e, p=p, i=i, dc=dc, stg=stg, n=n: e.transpose(p.t[:, i * 128:i * 128 + n], stg.t[0:n, dc * 128:(dc + 1) * 128],
                                                                                          ident_f[0:n, 0:n]), r=[stg] + CST, w=[p])
                    S.op("act" if q4 % 2 else "dve",
                         (lambda e, p=p, q4=q4, tc0=tc0, n=n: e.activation(out=dst.t[:, q4 * 4:(q4 + 1) * 4, toff + tc0:toff + tc0 + n],
                                                                           in_=p.t[:, :].rearrange("p (a b) -> p a b", a=4)[:, :, 0:n], func=AF.Copy))
                         if q4 % 2 else
                         (lambda e, p=p, q4=q4, tc0=tc0, n=n: e.tensor_copy(out=dst.t[:, q4 * 4:(q4 + 1) * 4, toff + tc0:toff + tc0 + n],
                                                                            in_=p.t[:, :].rearrange("p (a b) -> p a b", a=4)[:, :, 0:n])),
                         r=[p], w=[dst])

        sqt = []
        rstd_l = []
        ntmp = []

        def alloc_tmps(tag, with_x=False):
            sqt[:] = [sb("sq%s%d" % (tag, i), [128, 512], BF16) for i in range(2)]
            rstd_l[:] = [sb("rstd%s" % tag, [128, 512], F32)]
            ntmp[:] = [sb("ntmp%s%d" % (tag, i), [128, 512], F32) for i in range(2)]
            if with_x:
                xstage[:] = [sb("xstg%s" % tag, [128, D], F32)]

        def rmsnorm_fm(src, soff, ntok, dst, doff, A, B_mod, sample, post=None):
            rstd = rstd_l[0]
            p = S.ps()
            for dc in range(16):
                sq = sqt[dc % 2]
                S.op("act", lambda e, dc=dc, sq=sq: e.activation(out=sq.t[:, 0:ntok], in_=src.t[:, dc, soff:soff + ntok], func=AF.Square),
                     r=[src], w=[sq])
                S.op("pe", lambda e, dc=dc, sq=sq, p=p: e.matmul(p.t[:, 0:ntok], ones_b, sq.t[:, 0:ntok], start=(dc == 0), stop=(dc == 15)),
                     r=[sq] + CST, w=[p])
            S.op("act", lambda e, p=p: e.activation(out=rstd.t[:, 0:ntok], in_=p.t[:, 0:ntok], func=AF.Sqrt, bias=eps_c, scale=1.0 / D),
                 r=[p] + CST, w=[rstd])
            S.op("dve", lambda e: e.reciprocal(out=rstd.t[:, 0:ntok], in_=rstd.t[:, 0:ntok]), r=[rstd], w=[rstd])
            for dc_ in range(16):
                tm = ntmp[dc_ % 2]
                S.op("dve", lambda e, dc=dc_, tm=tm: e.tensor_tensor(out=tm.t[:, 0:ntok], in0=src.t[:, dc, soff:soff + ntok], in1=rstd.t[:, 0:ntok],
                                                                    op=ALU.mult), r=[src, rstd], w=[tm])
                dc = dc_ if post is None else dc_ % 2
                if not sample:
                    if B_mod is None:
                        S.op("act", lambda e, dc=dc, dcf=dc_, tm=tm: e.activation(out=dst.t[:, dc, doff:doff + ntok], in_=tm.t[:, 0:ntok], func=AF.Copy,
                                                                         scale=A.t[:, dcf:dcf + 1]), r=[tm, A], w=[dst])
                    else:
                        S.op("act", lambda e, dc=dc, dcf=dc_, tm=tm: e.activation(out=dst.t[:, dc, doff:doff + ntok], in_=tm.t[:, 0:ntok], func=AF.Identity,
                                                                         scale=A.t[:, dcf, NSEQ:NSEQ + 1], bias=modT.t[:, B_mod + dcf, NSEQ:NSEQ + 1]),
                             r=[tm, A, modT], w=[dst])
                else:
                    tv = tm.t[:, 0:ntok].rearrange("p (s t) -> p s t", t=4)
                    S.op("dve", lambda e, dc=dc, dcf=dc_, tv=tv: e.tensor_tensor(out=tv, in0=tv, in1=A.t[:, dcf, 0:NSEQ].unsqueeze(2).to_broadcast([128, NSEQ, 4]),
                                                                        op=ALU.mult), r=[tm, A], w=[tm])
                    S.op("dve", lambda e, dc=dc, dcf=dc_, tv=tv: e.tensor_tensor(out=dst.t[:, dc, doff:doff + ntok].rearrange("p (s t) -> p s t", t=4), in0=tv,
                                                                        in1=modT.t[:, B_mod + dcf, 0:NSEQ].unsqueeze(2).to_broadcast([128, NSEQ, 4]),
                                                                        op=ALU.add), r=[tm, modT], w=[dst])
                if post is not None:
                    post(dc_, dc)

        es_mix = contextlib.ExitStack()
        S.cur = es_mix
        alloc_wpool("B", 4)
        alloc_tmps("B", with_x=True)
        xg = sb("xg", [128, 16, GT], F32)
        xn = sb("xn", [128, 16, GT], BF16)
        NTC = GT // 128
        qT = sb("qT", [128, 4, GT], F32)
        kT = sb("kT", [128, 4, GT], F32)
        ktm = sb("ktm", [128, NTC, 512], F32)
        vtm = sb("vtm", [128, NTC, 1024], BF16)
        Ltm = sb("Ltm", [128, NTC, 512], F32)
        alT = sb("alT", [16, GT], BF16)
        sgT = sb("sgT", [128, 8, GT], BF16)
        oT = sb("oT", [128, 8, GT], F32)
        oaT = sb("oaT", [128, 8, GT], BF16)
        obT = sb("obT", [128, 8, GT], BF16)
        Sst = [sb("Sst%d" % h, [128, 256], F32) for h in range(4)]
        Sbf = [sb("Sbf%d" % h, [128, 256], BF16) for h in range(4)]
        halo = sb("halo", [128, 8, 2], F32)
        for h in range(4):
            S.op("dve", lambda e, h=h: e.memset(Sst[h].t[:], 0.0), w=[Sst[h]])
            S.op("dve", lambda e, h=h: e.memset(Sbf[h].t[:], 0.0), w=[Sbf[h]])
        S.op("dve", lambda e: e.memset(halo.t[:], 0.0), w=[halo])
        expb = sb("expb", [128, 128], F32)
        expnb = sb("expnb", [128, 128], F32)
        qp = sb("qp", [128, 128], BF16)
        kp = sb("kp", [128, 128], BF16)
        kk = sb("kk", [128, 512], BF16)
        erev = sb("erev", [128, 512], F32)
        PTm = sb("PTm", [128, 128], BF16)
        gtmp = [sb("gtmp%d" % i, [128, GT], F32) for i in range(4)]
        cct = sb("cct", [128, 4, GT], F32)
        uext = sb("uext", [128, 4, GT + 2 * max(1, NSEQ)], F32)
        cnv = sb("cnv", [128, GT], F32)
        s0f = [sb("s0f%d" % i, [128, 256], F32) for i in range(2)]
        s0b = [sb("s0b%d" % i, [128, 256], BF16) for i in range(2)]
        snew = [sb("snew%d" % i, [128, 256], F32) for i in range(2)]
        ohs = sb("ohs", [128, NSEQ], F32)
        S.op("dve", lambda e: e.tensor_copy(out=ohs.t[:, :], in_=cs.t[:, 642:642 + NSEQ]), r=[cs], w=[ohs])
        scv = sb("scv", [128, 8, NSEQ * 2], F32)
        ctm = sb("ctm", [NSEQ * 2, CH], F32)
        S.dma("sp", lambda e: e.dma_start(out=ctm.t[:], in_=sconv), w=[ctm])
        p = S.ps()
        for blk in range(8):
            S.op("pe", lambda e, blk=blk, p=p: e.transpose(p.t[:, blk * NSEQ * 2:(blk + 1) * NSEQ * 2], ctm.t[0:NSEQ * 2, blk * 128:(blk + 1) * 128],
                                                         ident_f[0:NSEQ * 2, 0:NSEQ * 2]), r=[ctm] + CST, w=[p])
        S.op("dve", lambda e, p=p: e.tensor_copy(out=scv.t[:].rearrange("p a b -> p (a b)"), in_=p.t[:, 0:8 * NSEQ * 2]), r=[p], w=[scv])

        def proj_fm(W, wc0, ntok, t0, M=128):
            p = S.ps()
            for kc in range(16):
                S.op("pe", lambda e, kc=kc, p=p: e.matmul(p.t[0:M, 0:ntok], W.t[:, kc, wc0:wc0 + M], xn.t[:, kc, t0:t0 + ntok],
                                                        start=(kc == 0), stop=(kc == 15)), r=[W, xn], w=[p])
            return p

        def proj_tm(W, tc, n, ncols=512):
            p = S.ps()
            for kc in range(16):
                S.op("pe", lambda e, kc=kc, p=p: e.matmul(p.t[0:n, 0:ncols], xn.t[:, kc, tc * 128:tc * 128 + n], W.t[:, kc, 0:ncols],
                                                        start=(kc == 0), stop=(kc == 15)), r=[W, xn], w=[p])
            return p

        def mixer_group(kind, src, ntok, hoff, last_prefix=False):
            sample = kind == "samp"
            full = kind != "pre"
            ntc = (ntok + 127) // 128
            load_xT(src, ntok, xg, 0)
            rmsnorm_fm(xg, 0, ntok, xn, 0, A1, M_SH1, sample)
            if full:
                W = load_w(w_in, C_Q, 512)
                for h in range(4):
                    p = proj_fm(W, h * 128, ntok, 0)
                    S.op("act", lambda e, p=p, h=h: e.activation(out=qT.t[:, h, 0:ntok], in_=p.t[:, 0:ntok], func=AF.Copy, scale=HK ** -0.5),
                         r=[p], w=[qT])
            W = load_w(w_in, C_K, 512)
            if full:
                for h in range(4):
                    p = proj_fm(W, h * 128, ntok, 0)
                    S.op("dve", lambda e, p=p, h=h: e.tensor_copy(out=kT.t[:, h, 0:ntok], in_=p.t[:, 0:ntok]), r=[p], w=[kT])
            for tc in range(ntc):
                n = min(128, ntok - tc * 128)
                p = proj_tm(W, tc, n)
                S.op("act", lambda e, p=p, tc=tc, n=n: e.activation(out=ktm.t[0:n, tc, :], in_=p.t[0:n, :], func=AF.Copy), r=[p], w=[ktm])
            for vt in range(2):
                W = load_w(w_in, C_V + vt * 512, 512)
                for tc in range(ntc):
                    n = min(128, ntok - tc * 128)
                    p = proj_tm(W, tc, n)
                    S.op("dve" if vt else "act",
                         (lambda e, p=p, tc=tc, n=n, vt=vt: e.tensor_copy(out=vtm.t[0:n, tc, vt * 512:(vt + 1) * 512], in_=p.t[0:n, :])) if vt else
                         (lambda e, p=p, tc=tc, n=n, vt=vt: e.activation(out=vtm.t[0:n, tc, vt * 512:(vt + 1) * 512], in_=p.t[0:n, :], func=AF.Copy)),
                         r=[p], w=[vtm])
            W = load_w(w_in, C_AL, 16)
            p = proj_fm(W, 0, ntok, 0, M=16)
            S.op("dve", lambda e, p=p: e.tensor_copy(out=alT.t[:, 0:ntok], in_=p.t[0:16, 0:ntok]), r=[p], w=[alT])
            for tc in range(ntc):
                n = min(128, ntok - tc * 128)
                p = S.ps()
                S.op("pe", lambda e, p=p, tc=tc, n=n: e.matmul(p.t[0:n, :], alT.t[:, tc * 128:tc * 128 + n], wgk.t[:, :], start=True, stop=False),
                     r=[alT, wgk], w=[p])
                S.op("pe", lambda e, p=p, n=n: e.matmul(p.t[0:n, :], ones_b[0:1, 0:n], bgk.t[:, :], start=False, stop=True), r=[bgk] + CST, w=[p])
                S.op("act", lambda e, p=p, tc=tc, n=n: e.activation(out=Ltm.t[0:n, tc, :], in_=p.t[0:n, :], func=AF.Exp, scale=-1.0), r=[p], w=[Ltm])
                S.op("act", lambda e, tc=tc, n=n: e.activation(out=Ltm.t[0:n, tc, :], in_=Ltm.t[0:n, tc, :], func=AF.Ln, bias=one_c[0:n, :], scale=1.0),
                     r=[Ltm] + CST, w=[Ltm])
            if full:
                for gt in range(2):
                    W = load_w(w_in, C_G + gt * 512, 512)
                    for b4 in range(4):
                        p = proj_fm(W, b4 * 128, ntok, 0)
                        S.op("act", lambda e, p=p, gt=gt, b4=b4: e.activation(out=sgT.t[:, gt * 4 + b4, 0:ntok], in_=p.t[:, 0:ntok], func=AF.Silu),
                             r=[p], w=[sgT])
            for tc in range(ntc):
                n = min(128, ntok - tc * 128)
                mk_f = smask_f[0:n, 0:n] if sample else maskT_f[0:n, 0:n]
                mr_f = smrev_f[0:n, 0:n] if sample else mrev_f[0:n, 0:n]
                p = S.ps()
                S.op("pe", lambda e, p=p, tc=tc, n=n, mr_f=mr_f: e.matmul(p.t[0:n, :], mr_f, Ltm.t[0:n, tc, :], start=True, stop=True),
                     r=[Ltm] + CST, w=[p])
                S.op("act", lambda e, p=p, n=n: e.activation(out=erev.t[0:n, :], in_=p.t[0:n, :], func=AF.Exp, scale=-1.0 / 16), r=[p], w=[erev])
                S.op("dve", lambda e, tc=tc, n=n: e.tensor_tensor(out=kk.t[0:n, :], in0=ktm.t[0:n, tc, :], in1=erev.t[0:n, :], op=ALU.mult),
                     r=[ktm, erev], w=[kk])
                for h in range(4):
                    pb = S.ps()
                    S.op("pe", lambda e, pb=pb, tc=tc, n=n, h=h, mk_f=mk_f: e.matmul(pb.t[:, 0:n], Ltm.t[0:n, tc, h * 128:(h + 1) * 128], mk_f,
                                                                                    start=True, stop=True), r=[Ltm] + CST, w=[pb])
                    S.op("act", lambda e, pb=pb, n=n: e.activation(out=expb.t[:, 0:n], in_=pb.t[:, 0:n], func=AF.Exp, scale=-1.0 / 16), r=[pb], w=[expb])
                    if full:
                        S.op("act", lambda e, pb=pb, n=n: e.activation(out=expnb.t[:, 0:n], in_=pb.t[:, 0:n], func=AF.Exp, scale=1.0 / 16), r=[pb], w=[expnb])
                        S.op("dve", lambda e, h=h, tc=tc, n=n: e.tensor_tensor(out=qp.t[:, 0:n], in0=qT.t[:, h, tc * 128:tc * 128 + n], in1=expb.t[:, 0:n],
                                                                               op=ALU.mult), r=[qT, expb], w=[qp])
                        S.op("dve", lambda e, h=h, tc=tc, n=n: e.tensor_tensor(out=kp.t[:, 0:n], in0=kT.t[:, h, tc * 128:tc * 128 + n], in1=expnb.t[:, 0:n],
                                                                               op=ALU.mult), r=[kT, expnb], w=[kp])
                        pp = S.ps()
                        S.op("pe", lambda e, pp=pp, n=n: e.matmul(pp.t[0:n, 0:n], kp.t[:, 0:n], qp.t[:, 0:n], start=True, stop=True), r=[kp, qp], w=[pp])
                        S.op("dve", lambda e, pp=pp, n=n, mk_f=mk_f: e.tensor_tensor(out=PTm.t[0:n, 0:n], in0=pp.t[0:n, 0:n], in1=mk_f, op=ALU.mult),
                             r=[pp] + CST, w=[PTm])
                        if not sample:
                            po = S.ps()
                            for eb in range(2):
                                S.op("pe", lambda e, po=po, eb=eb, n=n, tc=tc, h=h: e.matmul(po.t[:, eb * 128:eb * 128 + n],
                                                                                            vtm.t[0:n, tc, h * 256 + eb * 128:h * 256 + (eb + 1) * 128],
                                                                                            PTm.t[0:n, 0:n], start=True, stop=False), r=[vtm, PTm], w=[po])
                                S.op("pe", lambda e, po=po, eb=eb, n=n, h=h: e.matmul(po.t[:, eb * 128:eb * 128 + n], Sbf[h].t[:, eb * 128:(eb + 1) * 128],
                                                                                     qp.t[:, 0:n], start=False, stop=True), r=[Sbf[h], qp], w=[po])
                            S.op("act", lambda e, po=po, h=h, tc=tc, n=n: e.activation(out=oT.t[:, 2 * h:2 * h + 2, tc * 128:tc * 128 + n],
                                                                                       in_=po.t[:, 0:256].rearrange("p (a b) -> p a b", a=2)[:, :, 0:n],
                                                                                       func=AF.Copy), r=[po], w=[oT])
                    if not sample:
                        pd = S.ps()
                        S.op("pe", lambda e, pd=pd, n=n, tc=tc, h=h: e.matmul(pd.t[:, 0:256], kk.t[0:n, h * 128:(h + 1) * 128], vtm.t[0:n, tc, h * 256:(h + 1) * 256],
                                                                             start=True, stop=True), r=[kk, vtm], w=[pd])
                        S.op("dve", lambda e, pd=pd, h=h, n=n: e.scalar_tensor_tensor(out=Sst[h].t[:, :], in0=Sst[h].t[:, :], scalar=expb.t[:, n - 1:n], in1=pd.t[:, 0:256],
                                                                                      op0=ALU.mult, op1=ALU.add), r=[Sst[h], expb, pd], w=[Sst[h]])
                        S.op("act", lambda e, h=h: e.activation(out=Sbf[h].t[:, :], in_=Sst[h].t[:, :], func=AF.Copy), r=[Sst[h]], w=[Sbf[h]])
                    else:
                        for s in range(NSEQ):
                            i2 = (s * 4 + h) % 2
                            S.dma("sp", lambda e, s=s, h=h, i2=i2: e.dma_start(out=s0f[i2].t[:, :], in_=sgla[s, h]), w=[s0f[i2]])
                            S.op("act", lambda e, i2=i2: e.activation(out=s0b[i2].t[:, :], in_=s0f[i2].t[:, :], func=AF.Copy), r=[s0f[i2]], w=[s0b[i2]])
                            po = S.ps()
                            for eb in range(2):
                                S.op("pe", lambda e, po=po, eb=eb, n=n, h=h, s=s: e.matmul(po.t[:, eb * 4:eb * 4 + 4],
                                                                                          vtm.t[0:n, 0, h * 256 + eb * 128:h * 256 + (eb + 1) * 128],
                                                                                          PTm.t[0:n, s * 4:s * 4 + 4], start=True, stop=False), r=[vtm, PTm], w=[po])
                                S.op("pe", lambda e, po=po, eb=eb, i2=i2, s=s: e.matmul(po.t[:, eb * 4:eb * 4 + 4], s0b[i2].t[:, eb * 128:(eb + 1) * 128],
                                                                                       qp.t[:, s * 4:s * 4 + 4], start=False, stop=True), r=[s0b[i2], qp], w=[po])
                            S.op("act", lambda e, po=po, h=h, s=s: e.activation(out=oT.t[:, 2 * h:2 * h + 2, s * 4:s * 4 + 4],
                                                                                in_=po.t[:, 0:8].rearrange("p (a b) -> p a b", a=2), func=AF.Copy), r=[po], w=[oT])
                            S.op("dve", lambda e, s=s, h=h, n=n: e.tensor_scalar(out=kp.t[0:n, :], in0=kk.t[0:n, h * 128:(h + 1) * 128], scalar1=ohs.t[0:n, s:s + 1],
                                                                               scalar2=None, op0=ALU.mult), r=[kk, ohs], w=[kp])
                            pd = S.ps()
                            S.op("pe", lambda e, pd=pd, n=n, h=h: e.matmul(pd.t[:, 0:256], kp.t[0:n, :], vtm.t[0:n, 0, h * 256:(h + 1) * 256], start=True, stop=True),
                                 r=[kp, vtm], w=[pd])
                            S.op("dve", lambda e, pd=pd, i2=i2, s=s: e.scalar_tensor_tensor(out=snew[i2].t[:, :], in0=s0f[i2].t[:, :], scalar=expb.t[:, s * 4 + 3:s * 4 + 4],
                                                                                           in1=pd.t[:, 0:256], op0=ALU.mult, op1=ALU.add),
                                 r=[s0f[i2], expb, pd], w=[snew[i2]])
                            S.dma("sp", lambda e, s=s, h=h, i2=i2: e.dma_start(out=gla_s[s, h], in_=snew[i2].t[:, :]), r=[snew[i2]])
            if last_prefix:
                for h in range(4):
                    S.op("dve", lambda e, h=h: e.tensor_scalar(out=Sst[h].t[:, :], in0=Sst[h].t[:, :], scalar1=pfl.t[:, 0:1], scalar2=None, op0=ALU.mult),
                         r=[Sst[h], pfl], w=[Sst[h]])
                    S.op("act", lambda e, h=h: e.activation(out=Sbf[h].t[:, :], in_=Sst[h].t[:, :], func=AF.Copy), r=[Sst[h]], w=[Sbf[h]])
            rstd = rstd_l[0]
            if full:
                for h in range(4):
                    p = S.ps()
                    for eb in range(2):
                        sq = ntmp[eb]
                        S.op("act", lambda e, sq=sq, h=h, eb=eb: e.activation(out=sq.t[:, 0:ntok], in_=oT.t[:, 2 * h + eb, 0:ntok], func=AF.Square), r=[oT], w=[sq])
                        S.op("pe", lambda e, sq=sq, p=p, eb=eb: e.matmul(p.t[:, 0:ntok], ones_f, sq.t[:, 0:ntok], start=(eb == 0), stop=(eb == 1)),
                             r=[sq] + CST, w=[p])
                    S.op("act", lambda e, p=p: e.activation(out=rstd.t[:, 0:ntok], in_=p.t[:, 0:ntok], func=AF.Sqrt, bias=eps_c, scale=1.0 / HV),
                         r=[p] + CST, w=[rstd])
                    S.op("dve", lambda e: e.reciprocal(out=rstd.t[:, 0:ntok], in_=rstd.t[:, 0:ntok]), r=[rstd], w=[rstd])
                    for eb in range(2):
                        tm = gtmp[eb]
                        S.op("dve", lambda e, tm=tm, h=h, eb=eb: e.tensor_tensor(out=tm.t[:, 0:ntok], in0=oT.t[:, 2 * h + eb, 0:ntok], in1=rstd.t[:, 0:ntok], op=ALU.mult),
                             r=[oT, rstd], w=[tm])
                        S.op("dve", lambda e, tm=tm, h=h, eb=eb: e.scalar_tensor_tensor(out=oaT.t[:, 2 * h + eb, 0:ntok], in0=tm.t[:, 0:ntok], scalar=gnT.t[:, eb:eb + 1],
                                                                                       in1=sgT.t[:, 2 * h + eb, 0:ntok], op0=ALU.mult, op1=ALU.mult),
                             r=[tm, gnT, sgT], w=[oaT])
            if full or last_prefix:
                for half in range(2):
                    if sample:
                        ue = uext.t[:, :, 0:NSEQ * 6].rearrange("p b (s t) -> p b s t", t=6)
                        for b4 in range(4):
                            S.op("dve", lambda e, b4=b4, half=half, ue=ue: e.tensor_copy(out=ue[:, b4, :, 0:2],
                                                                                        in_=scv.t[:, half * 4 + b4, :].rearrange("p (s t) -> p s t", t=2)),
                                 r=[scv], w=[uext])
                    elif full:
                        S.op("dve", lambda e, half=half: e.tensor_copy(out=uext.t[:, :, 0:2], in_=halo.t[:, half * 4:half * 4 + 4, :]), r=[halo], w=[uext])
                    t0 = 0 if full else ntok - 2
                    nn = ntok - t0
                    W = load_w(w_in, C_CC + half * 512, 512)
                    for b4 in range(4):
                        p = proj_fm(W, b4 * 128, nn, t0)
                        S.op("act", lambda e, p=p, b4=b4, nn=nn: e.activation(out=cct.t[:, b4, 0:nn], in_=p.t[:, 0:nn], func=AF.Copy), r=[p], w=[cct])
                    W = load_w(w_in, C_CHH + half * 512, 512)
                    for b4 in range(4):
                        p = proj_fm(W, b4 * 128, nn, t0)
                        if sample:
                            ue = uext.t[:, :, 0:NSEQ * 6].rearrange("p b (s t) -> p b s t", t=6)
                            S.op("dve", lambda e, p=p, b4=b4, ue=ue: e.tensor_tensor(out=ue[:, b4, :, 2:6], in0=p.t[:, 0:ntok].rearrange("p (s t) -> p s t", t=4),
                                                                                    in1=cct.t[:, b4, 0:ntok].rearrange("p (s t) -> p s t", t=4), op=ALU.mult),
                                 r=[p, cct], w=[uext])
                        elif full:
                            S.op("dve", lambda e, p=p, b4=b4: e.tensor_tensor(out=uext.t[:, b4, 2:2 + ntok], in0=p.t[:, 0:ntok], in1=cct.t[:, b4, 0:ntok], op=ALU.mult),
                                 r=[p, cct], w=[uext])
                        else:
                            S.op("dve", lambda e, p=p, b4=b4, half=half: e.scalar_tensor_tensor(out=halo.t[:, half * 4 + b4, :], in0=p.t[:, 0:2], scalar=pfl.t[:, 0:1],
                                                                                               in1=cct.t[:, b4, 0:2], op0=ALU.mult, op1=ALU.mult),
                                 r=[p, pfl, cct], w=[halo])
                    if not full:
                        continue
                    W = load_w(w_in, C_CB + half * 512, 512)
                    for b4 in range(4):
                        blk = half * 4 + b4
                        p = proj_fm(W, b4 * 128, ntok, 0)
                        if sample:
                            ue = uext.t[:, :, 0:NSEQ * 6].rearrange("p b (s t) -> p b s t", t=6)
                            cv3 = cnv.t[:, 0:ntok].rearrange("p (s t) -> p s t", t=4)
                            u0, u1, u2 = ue[:, b4, :, 0:4], ue[:, b4, :, 1:5], ue[:, b4, :, 2:6]
                        else:
                            cv3 = cnv.t[:, 0:ntok]
                            u0, u1, u2 = uext.t[:, b4, 0:ntok], uext.t[:, b4, 1:1 + ntok], uext.t[:, b4, 2:2 + ntok]
                        S.op("dve", lambda e, cv3=cv3, u0=u0, blk=blk: e.tensor_scalar(out=cv3, in0=u0, scalar1=wcT.t[:, blk:blk + 1], scalar2=None, op0=ALU.mult),
                             r=[uext, wcT], w=[cnv])
                        S.op("dve", lambda e, cv3=cv3, u1=u1, blk=blk: e.scalar_tensor_tensor(out=cv3, in0=u1, scalar=wcT.t[:, 8 + blk:9 + blk], in1=cv3,
                                                                                             op0=ALU.mult, op1=ALU.add), r=[uext, wcT, cnv], w=[cnv])
                        S.op("dve", lambda e, cv3=cv3, u2=u2, blk=blk: e.scalar_tensor_tensor(out=cv3, in0=u2, scalar=wcT.t[:, 16 + blk:17 + blk], in1=cv3,
                                                                                             op0=ALU.mult, op1=ALU.add), r=[uext, wcT, cnv], w=[cnv])
                        S.op("dve", lambda e, p=p, blk=blk: e.tensor_tensor(out=obT.t[:, blk, 0:ntok], in0=p.t[:, 0:ntok], in1=cnv.t[:, 0:ntok], op=ALU.mult),
                             r=[p, cnv], w=[obT])
                    if sample:
                        ue = uext.t[:, :, 0:NSEQ * 6].rearrange("p b (s t) -> p b s t", t=6)
                        for b4 in range(4):
                            S.op("dve", lambda e, b4=b4, half=half, ue=ue: e.tensor_copy(out=scv.t[:, half * 4 + b4, :].rearrange("p (s t) -> p s t", t=2),
                                                                                        in_=ue[:, b4, :, 4:6]), r=[uext], w=[scv])
                    else:
                        S.op("dve", lambda e, half=half: e.tensor_copy(out=halo.t[:, half * 4:half * 4 + 4, :], in_=uext.t[:, :, ntok:ntok + 2]), r=[uext], w=[halo])
            if not full:
                return
            for t4 in range(4):
                Wo = load_w(w_out, t4 * 512, 512)
                Wa = load_w(w_in, C_GA + t4 * 512, 512)
                Wb = load_w(w_in, C_GB + t4 * 512, 512)
                for sbk in range(4):
                    j = t4 * 4 + sbk
                    pA = S.ps()
                    for kc in range(8):
                        S.op("pe", lambda e, kc=kc, pA=pA, Wo=Wo, sbk=sbk: e.matmul(pA.t[:, 0:ntok], Wo.t[:, kc, sbk * 128:(sbk + 1) * 128], oaT.t[:, kc, 0:ntok],
                                                                                   start=(kc == 0), stop=(kc == 7)), r=[Wo, oaT], w=[pA])
                    pB = S.ps()
                    for kc in range(8):
                        S.op("pe", lambda e, kc=kc, pB=pB, Wo=Wo, sbk=sbk: e.matmul(pB.t[:, 0:ntok], Wo.t[:, 8 + kc, sbk * 128:(sbk + 1) * 128], obT.t[:, kc, 0:ntok],
                                                                                   start=(kc == 0), stop=(kc == 7)), r=[Wo, obT], w=[pB])
                    pGa = proj_fm(Wa, sbk * 128, ntok, 0)
                    pGb = proj_fm(Wb, sbk * 128, ntok, 0)
                    S.op("act", lambda e, pGa=pGa: e.activation(out=gtmp[0].t[:, 0:ntok], in_=pGa.t[:, 0:ntok], func=AF.Sigmoid), r=[pGa], w=[gtmp[0]])
                    S.op("act", lambda e, pGb=pGb: e.activation(out=gtmp[1].t[:, 0:ntok], in_=pGb.t[:, 0:ntok], func=AF.Sigmoid), r=[pGb], w=[gtmp[1]])
                    S.op("dve", lambda e, pA=pA: e.tensor_tensor(out=gtmp[0].t[:, 0:ntok], in0=pA.t[:, 0:ntok], in1=gtmp[0].t[:, 0:ntok], op=ALU.mult),
                         r=[pA, gtmp[0]], w=[gtmp[0]])
                    S.op("dve", lambda e, pB=pB: e.tensor_tensor(out=gtmp[1].t[:, 0:ntok], in0=pB.t[:, 0:ntok], in1=gtmp[1].t[:, 0:ntok], op=ALU.mult),
                         r=[pB, gtmp[1]], w=[gtmp[1]])
                    S.op("dve", lambda e: e.tensor_tensor(out=gtmp[0].t[:, 0:ntok], in0=gtmp[0].t[:, 0:ntok], in1=gtmp[1].t[:, 0:ntok], op=ALU.add),
                         r=[gtmp[0], gtmp[1]], w=[gtmp[0]])
                    if not sample:
                        S.op("dve", lambda e, j=j: e.scalar_tensor_tensor(out=xg.t[:, j, 0:ntok], in0=gtmp[0].t[:, 0:ntok], scalar=modT.t[:, M_G1 + j, NSEQ:NSEQ + 1],
                                                                         in1=xg.t[:, j, 0:ntok], op0=ALU.mult, op1=ALU.add), r=[gtmp[0], modT, xg], w=[xg])
                    else:
                        g3 = gtmp[0].t[:, 0:ntok].rearrange("p (s t) -> p s t", t=4)
                        S.op("dve", lambda e, j=j, g3=g3: e.tensor_tensor(out=g3, in0=g3, in1=modT.t[:, M_G1 + j, 0:NSEQ].unsqueeze(2).to_broadcast([128, NSEQ, 4]),
                                                                         op=ALU.mult), r=[gtmp[0], modT], w=[gtmp[0]])
                        S.op("dve", lambda e, j=j: e.tensor_tensor(out=xg.t[:, j, 0:ntok], in0=gtmp[0].t[:, 0:ntok], in1=xg.t[:, j, 0:ntok], op=ALU.add),
                             r=[gtmp[0], xg], w=[xg])
            S.dma("sp", lambda e: e.dma_start(out=hscr[:, :, hoff:hoff + ntok].rearrange("c p t -> p c t"), in_=xg.t[:, :, 0:ntok]), r=[xg], w=[hbuf], sem=hsem)

        hsem = S.new_dsem("hscr")
        hbuf = Buf("hscr")

        npg = NPRE // GT
        for g in range(npg):
            mixer_group("pre", xpre[g * GT:(g + 1) * GT, :], GT, 0, last_prefix=(g == npg - 1))
        for g in range(NP // GT):
            mixer_group("main", xp[g * GT:(g + 1) * GT, :], GT, g * GT)
        for h in range(4):
            S.dma("sp", lambda e, h=h: e.dma_start(out=gla_p[h], in_=Sst[h].t[:, :]), r=[Sst[h]])
        S.dma("sp", lambda e: [e.dma_start(out=conv_p[:, b * 128:(b + 1) * 128].rearrange("t p -> p t"), in_=halo.t[:, b, :], allow_slow_non_contiguous=True)
                               for b in range(8)], r=[halo], n=8)
        mixer_group("samp", xs, NS, NP)
        cso = ctm
        for hb in range(2):
            p = S.ps()
            for b4 in range(4):
                S.op("pe", lambda e, p=p, b4=b4, hb=hb: e.transpose(p.t[0:NSEQ * 2, b4 * 128:(b4 + 1) * 128], scv.t[:, hb * 4 + b4, :], ident_f), r=[scv] + CST, w=[p])
            S.op("dve", lambda e, p=p, hb=hb: e.tensor_copy(out=cso.t[:, hb * 512:(hb + 1) * 512], in_=p.t[0:NSEQ * 2, :]), r=[p], w=[cso])
        S.dma("sp", lambda e: e.dma_start(out=conv_s, in_=cso.t[:, :]), r=[cso])

        S.barrier()
        es_mix.close()
        S.cur = es
        NCH = (NT + 127) // 128
        NBLK = (CAP + 127) // 128
        BLKS = [(b * 128, min(128, CAP - b * 128)) for b in range(NBLK)]
        oscr = nc.dram_tensor("oscr", [E, 128, NBLK, D], BF16, kind="Internal").ap()
        obuf = Buf("oscr")
        iota_f = cs.t[:, 1152:1152 + CAP]
        mstrict_f = cs.t[:, 1024:1152]
        osc_sem = S.new_dsem("oscr")
        gwT = sb("gwT", [E, NT], F32)
        rkT = sb("rkT", [E, NT], F32)
        esel = sb("esel", [E, 128], F32)
        es_h = contextlib.ExitStack()
        S.cur = es_h
        alloc_wpool("C", 4)
        hntm = sb("hntm", [128, NCH, D], BF16)
        gwtm = sb("gwtm", [128, NCH, E], F32)
        mktm = sb("mktm", [128, NCH, E], F32)
        rktm = sb("rktm", [128, NCH, E], F32)
        top8 = sb("top8", [128, 8], F32)
        nmx = sb("nmx", [128, 1], F32)
        ssum = sb("ssum", [128, 1], F32)
        wr_f = sb("wr_f", [128, 16, E], F32)
        S.dma("sp", lambda e: e.dma_start(out=wr_f.t[:], in_=w_router.rearrange("(kc p) n -> p kc n", p=128)), w=[wr_f])
        wr = sb("wr", [128, 16, E], BF16)
        S.op("dve", lambda e: e.tensor_copy(out=wr.t[:], in_=wr_f.t[:]), r=[wr_f], w=[wr])
        br_f = sb("br_f", [1, E], F32)
        S.dma("sp", lambda e: e.dma_start(out=br_f.t[:], in_=b_router), w=[br_f])
        br = sb("br", [1, E], BF16)
        S.op("dve", lambda e: e.tensor_copy(out=br.t[:], in_=br_f.t[:]), r=[br_f], w=[br])
        S.op("dve", lambda e: e.memset(mktm.t[:], 0.0), w=[mktm])

        es_m1 = contextlib.ExitStack()
        S.cur = es_m1
        alloc_tmps("M1")
        hTt = sb("hTt", [128, 16, 512], F32)
        hnT = sb("hnT", [128, 16, 512], BF16)
        hnf = sb("hnf", [128, 2, 512], F32)
        lg = sb("lg", [128, E], F32)
        tiles = [(c, min(512, NP - c), False) for c in range(0, NP, 512)] + [(NP, NS, True)]
        for (t0, nn, smp) in tiles:
            S.dma("sp", lambda e, t0=t0, nn=nn: e.dma_start(out=hTt.t[:, :, 0:nn], in_=hscr[:, :, t0:t0 + nn].rearrange("c p t -> p c t")), w=[hTt], r=[hbuf])

            def post(dcf, dc, t0=t0, nn=nn):
                S.op("act", lambda e: e.activation(out=hnT.t[:, dcf, 0:nn], in_=hnf.t[:, dc, 0:nn], func=AF.Copy), r=[hnf], w=[hnT])
                p = S.ps()
                nsub = (nn + 127) // 128
                for c4 in range(nsub):
                    n = min(128, nn - c4 * 128)
                    S.op("pe", lambda e, p=p, c4=c4, n=n: e.transpose(p.t[0:n, c4 * 128:(c4 + 1) * 128], hnf.t[:, dc, c4 * 128:c4 * 128 + n], ident_f),
                         r=[hnf] + CST, w=[p])
                tc0 = t0 // 128
                if nn % 128 == 0:
                    S.op("dve", lambda e, p=p: e.tensor_copy(out=hntm.t[:, tc0:tc0 + nsub, dcf * 128:(dcf + 1) * 128],
                                                             in_=p.t[:, 0:nsub * 128].rearrange("p (c f) -> p c f", f=128)), r=[p], w=[hntm])
                else:
                    assert nsub == 1
                    S.op("dve", lambda e, p=p: e.tensor_copy(out=hntm.t[0:nn, tc0, dcf * 128:(dcf + 1) * 128], in_=p.t[0:nn, 0:128]), r=[p], w=[hntm])
            rmsnorm_fm(hTt, 0, nn, hnf, 0, A2, M_SH2, smp, post=post)
            for c0 in range(0, nn, 128):
                n = min(128, nn - c0)
                ch = (t0 + c0) // 128
                p = S.ps()
                for kc in range(16):
                    S.op("pe", lambda e, p=p, kc=kc, c0=c0, n=n: e.matmul(p.t[0:n, 0:E], hnT.t[:, kc, c0:c0 + n], wr.t[:, kc, :], start=(kc == 0), stop=False),
                         r=[hnT, wr], w=[p])
                S.op("pe", lambda e, p=p, n=n: e.matmul(p.t[0:n, 0:E], ones_b[0:1, 0:n], br.t[:, :], start=False, stop=True), r=[br] + CST, w=[p])
                S.op("dve", lambda e, p=p, n=n: e.tensor_copy(out=lg.t[0:n, :], in_=p.t[0:n, 0:E]), r=[p], w=[lg])
                S.op("dve", lambda e, n=n: e.max(out=top8.t[0:n, :], in_=lg.t[0:n, :]), r=[lg], w=[top8])
                S.op("dve", lambda e, n=n, ch=ch: e.tensor_scalar(out=mktm.t[0:n, ch, :], in0=lg.t[0:n, :], scalar1=top8.t[0:n, TOPK - 1:TOPK], scalar2=None, op0=ALU.is_ge),
                     r=[lg, top8], w=[mktm])
                S.op("dve", lambda e, n=n: e.tensor_scalar(out=nmx.t[0:n, :], in0=top8.t[0:n, 0:1], scalar1=-1.0, scalar2=None, op0=ALU.mult), r=[top8], w=[nmx])
                S.op("act", lambda e, n=n: e.activation(out=lg.t[0:n, :], in_=lg.t[0:n, :], func=AF.Exp, bias=nmx.t[0:n, :], scale=1.0), r=[lg, nmx], w=[lg])
                S.op("dve", lambda e, n=n, ch=ch: e.tensor_tensor(out=lg.t[0:n, :], in0=lg.t[0:n, :], in1=mktm.t[0:n, ch, :], op=ALU.mult), r=[lg, mktm], w=[lg])
                S.op("dve", lambda e, n=n: e.reduce_sum(out=ssum.t[0:n, :], in_=lg.t[0:n, :], axis=mybir.AxisListType.X), r=[lg], w=[ssum])
                S.op("dve", lambda e, n=n: e.reciprocal(out=ssum.t[0:n, :], in_=ssum.t[0:n, :]), r=[ssum], w=[ssum])
                S.op("dve", lambda e, n=n, ch=ch: e.tensor_scalar(out=gwtm.t[0:n, ch, :], in0=lg.t[0:n, :], scalar1=ssum.t[0:n, 0:1], scalar2=None, op0=ALU.mult),
                     r=[lg, ssum], w=[gwtm])
                p2 = S.ps()
                S.op("pe", lambda e, p2=p2, n=n, ch=ch: e.transpose(p2.t[0:E, 0:n], gwtm.t[0:n, ch, :], ident_f[0:n, 0:n]), r=[gwtm] + CST, w=[p2])
                S.op("dve", lambda e, p2=p2, n=n, ch=ch: e.tensor_copy(out=gwT.t[:, ch * 128:ch * 128 + n], in_=p2.t[0:E, 0:n]), r=[p2], w=[gwT])
        SCH = NCH - 1
        for ch in range(NCH):
            n = min(128, NT - ch * 128)
            p = S.ps()
            prev = [] if ch == SCH else [SCH] + list(range(ch))
            S.op("pe", lambda e, p=p, n=n, ch=ch, prev=prev: e.matmul(p.t[0:n, 0:E], mstrict_f[0:n, 0:n], mktm.t[0:n, ch, :], start=True, stop=(len(prev) == 0)),
                 r=[mktm] + CST, w=[p])
            for i2, c2 in enumerate(prev):
                k2 = min(128, NT - c2 * 128)
                S.op("pe", lambda e, p=p, n=n, c2=c2, k2=k2, i2=i2, prev=prev: e.matmul(p.t[0:n, 0:E], ones_f[0:k2, 0:n], mktm.t[0:k2, c2, :], start=False,
                                                                                      stop=(i2 == len(prev) - 1)), r=[mktm] + CST, w=[p])
            S.op("dve", lambda e, p=p, n=n, ch=ch: e.scalar_tensor_tensor(out=rktm.t[0:n, ch, :], in0=p.t[0:n, 0:E], scalar=1.0, in1=mktm.t[0:n, ch, :],
                                                                         op0=ALU.add, op1=ALU.mult), r=[p, mktm], w=[rktm])
            S.op("dve", lambda e, n=n, ch=ch: e.tensor_scalar(out=rktm.t[0:n, ch, :], in0=rktm.t[0:n, ch, :], scalar1=-1.0, scalar2=None, op0=ALU.add),
                 r=[rktm], w=[rktm])
            p2 = S.ps()
            S.op("pe", lambda e, p2=p2, n=n, ch=ch: e.transpose(p2.t[0:E, 0:n], rktm.t[0:n, ch, :], ident_f[0:n, 0:n]), r=[rktm] + CST, w=[p2])
            S.op("dve", lambda e, p2=p2, n=n, ch=ch: e.tensor_copy(out=rkT.t[:, ch * 128:ch * 128 + n], in_=p2.t[0:E, 0:n]), r=[p2], w=[rkT])
        S.barrier()
        es_m1.close()

        es_m2 = contextlib.ExitStack()
        S.cur = es_m2
        sel = sb("sel", [128, NCH, CAP], BF16)
        xbT = sb("xbT", [128, 16, CAP], BF16)
        actT = sb("actT", [128, FC, CAP], BF16)
        oute = [sb("oute%d" % i, [128, NBLK, D], BF16) for i in range(1)]
        bupT = sb("bupT", [128, 2 * FC], F32)
        bstg = sb("bstg", [128, 128], F32)
        mt = [sb("mt%d" % i, [128, CAP], F32) for i in range(3)]
        gs = sb("gs", [128, 4, CAP], F32)
        S.op("dve", lambda e: e.memset(oute[0].t[:], 0.0), w=[oute[0]])
        for ex in range(E):
            S.dma("sp", lambda e, ex=ex: e.dma_start(out=bstg.t[0:2 * FC, :], in_=b_up[ex * 2 * FC:(ex + 1) * 2 * FC, :]), w=[bstg])
            p = S.ps()
            S.op("pe", lambda e, p=p: e.transpose(p.t[:, 0:2 * FC], bstg.t[0:2 * FC, :], ident_f[0:2 * FC, 0:2 * FC]), r=[bstg] + CST, w=[p])
            S.op("dve", lambda e, p=p: e.tensor_copy(out=bupT.t[:, :], in_=p.t[:, 0:2 * FC]), r=[p], w=[bupT])
            for ch in range(NCH):
                n = min(128, NT - ch * 128)
                S.op("dve", lambda e, n=n, ch=ch, ex=ex: e.tensor_scalar(out=sel.t[0:n, ch, :], in0=iota_f[0:n, :], scalar1=rktm.t[0:n, ch, ex:ex + 1], scalar2=None,
                                                                        op0=ALU.is_equal), r=[rktm] + CST, w=[sel])
            for f in range(16):
                p = S.ps()
                for ch in range(NCH):
                    n = min(128, NT - ch * 128)
                    S.op("pe", lambda e, p=p, f=f, ch=ch, n=n: e.matmul(p.t[:, 0:CAP], hntm.t[0:n, ch, f * 128:(f + 1) * 128], sel.t[0:n, ch, :],
                                                                       start=(ch == 0), stop=(ch == NCH - 1)), r=[hntm, sel], w=[p])
                S.op("act" if f % 2 else "dve",
                     (lambda e, p=p, f=f: e.activation(out=xbT.t[:, f, :], in_=p.t[:, 0:CAP], func=AF.Copy)) if f % 2 else
                     (lambda e, p=p, f=f: e.tensor_copy(out=xbT.t[:, f, :], in_=p.t[:, 0:CAP])), r=[p], w=[xbT])
            vsrc = w_up[ex].rearrange("(kc p) n -> p kc n", p=128)
            for f4 in range(0, FC, 4):
                nb = min(4, FC - f4)
                Wg = wtile()
                S.dma("pool", lambda e, W=Wg, f4=f4, nb=nb, vsrc=vsrc: e.dma_start(out=W.t[:, :, 0:nb * 128], in_=vsrc[:, :, f4 * 128:(f4 + nb) * 128]), w=[Wg])
                for fb in range(nb):
                    f = f4 + fb
                    pg = S.ps()
                    for kc in range(16):
                        S.op("pe", lambda e, pg=pg, kc=kc, W=Wg, fb=fb: e.matmul(pg.t[:, 0:CAP], W.t[:, kc, fb * 128:(fb + 1) * 128], xbT.t[:, kc, :],
                                                                                start=(kc == 0), stop=(kc == 15)), r=[Wg, xbT], w=[pg])
                    S.op("dve", lambda e, pg=pg, f=f: e.tensor_scalar(out=mt[0].t[:, :], in0=pg.t[:, 0:CAP], scalar1=bupT.t[:, f:f + 1], scalar2=LIMIT,
                                                                     op0=ALU.add, op1=ALU.min), r=[pg, bupT], w=[mt[0]])
                    S.op("act", lambda e: e.activation(out=mt[1].t[:, :], in_=mt[0].t[:, :], func=AF.Sigmoid, scale=ALPHA), r=[mt[0]], w=[mt[1]])
                    S.op("dve", lambda e, fb=fb: e.tensor_tensor(out=gs.t[:, fb, :], in0=mt[0].t[:, :], in1=mt[1].t[:, :], op=ALU.mult), r=[mt[0], mt[1]], w=[gs])
                Wl = wtile()
                S.dma("pool", lambda e, W=Wl, f4=f4, nb=nb, vsrc=vsrc: e.dma_start(out=W.t[:, :, 0:nb * 128], in_=vsrc[:, :, FF + f4 * 128:FF + (f4 + nb) * 128]), w=[Wl])
                for fb in range(nb):
                    f = f4 + fb
                    pl = S.ps()
                    for kc in range(16):
                        S.op("pe", lambda e, pl=pl, kc=kc, W=Wl, fb=fb: e.matmul(pl.t[:, 0:CAP], W.t[:, kc, fb * 128:(fb + 1) * 128], xbT.t[:, kc, :],
                                                                                start=(kc == 0), stop=(kc == 15)), r=[Wl, xbT], w=[pl])
                    S.op("dve", lambda e, pl=pl, f=f: e.tensor_scalar(out=mt[2].t[:, :], in0=pl.t[:, 0:CAP], scalar1=bupT.t[:, FC + f:FC + f + 1], scalar2=LIMIT,
                                                                     op0=ALU.add, op1=ALU.min), r=[pl, bupT], w=[mt[2]])
                    S.op("dve", lambda e: e.tensor_scalar(out=mt[2].t[:, :], in0=mt[2].t[:, :], scalar1=-LIMIT, scalar2=1.0, op0=ALU.max, op1=ALU.add),
                         r=[mt[2]], w=[mt[2]])
                    S.op("dve", lambda e, f=f, fb=fb: e.tensor_tensor(out=actT.t[:, f, :], in0=gs.t[:, fb, :], in1=mt[2].t[:, :], op=ALU.mult), r=[gs, mt[2]], w=[actT])
            ot = oute[0]
            for t4 in range(4):
                W = wtile()
                vsrc = w_down[ex].rearrange("(kc p) n -> p kc n", p=128)
                S.dma("pool", lambda e, W=W, t4=t4, vsrc=vsrc: e.dma_start(out=W.t[:, 0:FC, :], in_=vsrc[:, :, t4 * 512:(t4 + 1) * 512]), w=[W])
                for blk, (b0, bn) in enumerate(BLKS):
                    py = S.ps()
                    for fc in range(FC):
                        S.op("pe", lambda e, py=py, fc=fc, W=W, b0=b0, bn=bn: e.matmul(py.t[0:bn, :], actT.t[:, fc, b0:b0 + bn], W.t[:, fc, :],
                                                                                     start=(fc == 0), stop=(fc == FC - 1)), r=[W, actT], w=[py])
                    S.op("act" if blk % 2 else "dve",
                         (lambda e, py=py, blk=blk, bn=bn, t4=t4, ot=ot: e.activation(out=ot.t[0:bn, blk, t4 * 512:(t4 + 1) * 512], in_=py.t[0:bn, :], func=AF.Copy)) if blk % 2 else
                         (lambda e, py=py, blk=blk, bn=bn, t4=t4, ot=ot: e.tensor_copy(out=ot.t[0:bn, blk, t4 * 512:(t4 + 1) * 512], in_=py.t[0:bn, :])), r=[py], w=[ot])
            S.dma("sp", lambda e, ex=ex, ot=ot: e.dma_start(out=oscr[ex], in_=ot.t[:, :, :]), r=[ot], w=[obuf], sem=osc_sem)
        S.barrier()
        es_m2.close()
        es_h.close()

        S.cur = es
        hT = sb("hT", [128, 16, NT], F32)
        oin = [sb("oin%d" % i, [128, NBLK, D], BF16) for i in range(2)]
        selT = [sb("selT%d" % i, [128, NBLK, NT], BF16) for i in range(1)]
        gwr = [sb("gwr%d" % i, [128, NT], F32) for i in range(1)]
        ct = [sb("ct%d" % i, [128, NS], F32) for i in range(2)]
        yo = sb("yo", [128, D], F32)
        bdn = sb("bdn", [E, D], F32)
        alloc_tmps("M3")
        S.dma("sp", lambda e: e.dma_start(out=hT.t[:, :, :], in_=hscr.rearrange("c p t -> p c t")), w=[hT], r=[hbuf])
        S.dma("sp", lambda e: e.dma_start(out=bdn.t[:, :], in_=b_down.rearrange("(e a) b -> e (a b)", a=16)), w=[bdn])
        ctiles = [(c, min(512, NP - c)) for c in range(0, NP, 512)] + [(NP, NS)]

        def accum(py, j, c0, nn, k):
            if c0 < NP:
                S.op("dve", lambda e: e.scalar_tensor_tensor(out=hT.t[:, j, c0:c0 + nn], in0=py.t[:, 0:nn], scalar=modT.t[:, M_G2 + j, NSEQ:NSEQ + 1],
                                                             in1=hT.t[:, j, c0:c0 + nn], op0=ALU.mult, op1=ALU.add), r=[py, modT, hT], w=[hT])
            else:
                cc_ = ct[k % 2]
                S.op("dve", lambda e: e.tensor_tensor(out=cc_.t[:, 0:nn].rearrange("p (s t) -> p s t", t=4), in0=py.t[:, 0:nn].rearrange("p (s t) -> p s t", t=4),
                                                      in1=modT.t[:, M_G2 + j, 0:NSEQ].unsqueeze(2).to_broadcast([128, NSEQ, 4]), op=ALU.mult),
                     r=[py, modT], w=[cc_])
                S.op("dve", lambda e: e.tensor_tensor(out=hT.t[:, j, c0:c0 + nn], in0=cc_.t[:, 0:nn], in1=hT.t[:, j, c0:c0 + nn], op=ALU.add),
                     r=[cc_, hT], w=[hT])

        for j in range(16):
            for (c0, nn) in ctiles:
                py = S.ps()
                S.op("pe", lambda e, py=py, j=j, c0=c0, nn=nn: e.matmul(py.t[:, 0:nn], bdn.t[:, j * 128:(j + 1) * 128], gwT.t[:, c0:c0 + nn], start=True, stop=True),
                     r=[bdn, gwT], w=[py])
                accum(py, j, c0, nn, j)
        for ex in range(E):
            oi, sT, gr = oin[ex % 2], selT[0], gwr[0]
            S.dma("sp", lambda e, ex=ex, oi=oi: e.dma_start(out=oi.t[:, :, :], in_=oscr[ex]), w=[oi], r=[obuf])
            S.op("dve", lambda e, ex=ex: e.tensor_copy(out=esel.t[:, :], in_=cs.t[0:E, ex:ex + 1].to_broadcast([E, 128])), r=[cs], w=[esel])
            for (c0, nn) in ctiles:
                p = S.ps()
                S.op("pe", lambda e, p=p, c0=c0, nn=nn: e.matmul(p.t[:, 0:nn], esel.t[:, :], gwT.t[:, c0:c0 + nn], start=True, stop=True), r=[esel, gwT], w=[p])
                S.op("act", lambda e, p=p, c0=c0, nn=nn, gr=gr: e.activation(out=gr.t[:, c0:c0 + nn], in_=p.t[:, 0:nn], func=AF.Copy), r=[p], w=[gr])
                p = S.ps()
                S.op("pe", lambda e, p=p, c0=c0, nn=nn: e.matmul(p.t[:, 0:nn], esel.t[:, :], rkT.t[:, c0:c0 + nn], start=True, stop=True), r=[esel, rkT], w=[p])
                for blk, (b0, bn) in enumerate(BLKS):
                    S.op("dve", lambda e, p=p, c0=c0, nn=nn, blk=blk, bn=bn, sT=sT, gr=gr: e.scalar_tensor_tensor(out=sT.t[0:bn, blk, c0:c0 + nn], in0=p.t[0:bn, 0:nn],
                                                                                                                 scalar=cs.t[0:bn, 960 + blk:961 + blk], in1=gr.t[0:bn, c0:c0 + nn],
                                                                                                                 op0=ALU.is_equal, op1=ALU.mult), r=[p, gr] + CST, w=[sT])
            for j in range(16):
                for (c0, nn) in ctiles:
                    py = S.ps()
                    for blk, (b0, bn) in enumerate(BLKS):
                        S.op("pe", lambda e, py=py, blk=blk, bn=bn, j=j, c0=c0, nn=nn, oi=oi, sT=sT: e.matmul(py.t[:, 0:nn], oi.t[0:bn, blk, j * 128:(j + 1) * 128], sT.t[0:bn, blk, c0:c0 + nn],
                                                                                                             start=(blk == 0), stop=(blk == NBLK - 1)), r=[oi, sT], w=[py])
                    accum(py, j, c0, nn, j)
        osem = S.new_dsem("yout")
        for (c0, nn) in ctiles:
            rmsnorm_fm(hT, c0, nn, hT, c0, nfT, None, False)
        for c0 in range(0, NT, 128):
            n = min(128, NT - c0)
            for q4 in range(4):
                p = S.ps()
                for i in range(4):
                    dc = q4 * 4 + i
                    S.op("pe", lambda e, p=p, i=i, dc=dc, c0=c0, n=n: e.transpose(p.t[0:n, i * 128:(i + 1) * 128], hT.t[:, dc, c0:c0 + n], ident_f), r=[hT] + CST, w=[p])
                S.op("act" if q4 % 2 else "dve",
                     (lambda e, p=p, q4=q4, n=n: e.activation(out=yo.t[0:n, q4 * 512:(q4 + 1) * 512], in_=p.t[0:n, :], func=AF.Copy)) if q4 % 2 else
                     (lambda e, p=p, q4=q4, n=n: e.tensor_copy(out=yo.t[0:n, q4 * 512:(q4 + 1) * 512], in_=p.t[0:n, :])), r=[p], w=[yo])
            if c0 < NP:
                S.dma("sp", lambda e, c0=c0, n=n: e.dma_start(out=y_p[c0:c0 + n, :], in_=yo.t[0:n, :]), r=[yo], sem=osem)
            else:
                S.dma("sp", lambda e, c0=c0, n=n: e.dma_start(out=y_s[c0 - NP:c0 - NP + n, :], in_=yo.t[0:n, :]), r=[yo], sem=osem)

        def fin(e):
            for d in S.dsems:
                if d[1] > 0:
                    e.wait_ge(d[0], d[1])
            return e.nop()
        S.op("sp", fin)

        S.assign()
        with nc.Block() as block:
            @block.tensor
            def _(e):
                S.run("pe", e)

            @block.vector
            def _(e):
                S.run("dve", e)

            @block.scalar
            def _(e):
                S.run("act", e)

            @block.gpsimd
            def _(e):
                S.run("pool", e)

            @block.sync
            def _(e):
                S.run("sp", e)
    return nc


def make_cst(NSEQ):
    c = np.zeros((128, 1664), np.float32)
    i = np.arange(128)
    c[:, 0:128] = np.eye(128, dtype=np.float32)
    c[:, 128:256] = (i[:, None] <= i[None, :]).astype(np.float32)
    c[:, 256:384] = (i[:, None] > i[None, :]).astype(np.float32)
    j = np.arange(64)
    same = (j[:, None] // 4) == (j[None, :] // 4)
    c[0:64, 384:448] = (same & (j[:, None] <= j[None, :])).astype(np.float32)
    c[0:64, 448:512] = (same & (j[:, None] > j[None, :])).astype(np.float32)
    c[:, 512:640] = 1.0
    c[:, 640] = EPS
    c[:, 641] = 1.0
    for s in range(NSEQ):
        c[s * 4:(s + 1) * 4, 642 + s] = 1.0
    c[:, 1152:1664] = np.arange(512, dtype=np.float32)[None, :]
    c[:, 960] = i
    c[:, 961] = i + 128
    c[:, 962] = i + 256
    c[:, 963] = i + 384
    c[:, 1024:1152] = (i[:, None] < i[None, :]).astype(np.float32)
    return c


def make_in_maps(inp, cfg, n_cores, seq_len, n_seq_prompt):
    NP, NPRE, NSEQ, E, FF = cfg["NP"], cfg["NPRE"], cfg["NSEQ"], cfg["E"], cfg["FF"]
    FC = FF // 128
    f = lambda a: np.ascontiguousarray(a, dtype=np.float32)
    shared = dict(
        cst=make_cst(NSEQ),
        w_ada=f(inp["w_ada"][0]), b_ada=f(inp["b_ada"][0].reshape(96, 128)), norm1_w=f(inp["norm1_w"][0].reshape(16, 128)),
        w_in=f(inp["w_in"][0]), w_gk_up=f(inp["w_gk_up"][0]), b_gk=f(inp["b_gk"][0].reshape(1, 512)),
        gla_norm_w=f(inp["gla_norm_w"][0].reshape(2, 128)), w_conv=f(inp["w_conv"][0].reshape(24, 128)), w_out=f(inp["w_out"][0]),
        norm2_w=f(inp["norm2_w"][0].reshape(16, 128)), w_router=f(inp["w_router"][0]), b_router=f(inp["b_router"][0].reshape(1, E)),
        w_up=f(inp["w_up"][0]), b_up=f(inp["b_up"][0].reshape(E * 2 * FC, 128)), w_down=f(inp["w_down"][0]),
        b_down=f(inp["b_down"][0].reshape(E * 16, 128)), final_norm_w=f(inp["final_norm_w"].reshape(16, 128)),
    )
    maps = []
    for c in range(n_cores):
        b, half = c // 2, c % 2
        m = dict(shared)
        m["xp"] = f(inp["x_prompt"][b, half * NP:(half + 1) * NP])
        m["xpre"] = f(inp["x_prompt"][b, 0:NPRE])
        m["pflag"] = np.full((128, 1), float(half), np.float32)
        m["xs"] = f(inp["x_sample"][c * NSEQ:(c + 1) * NSEQ].reshape(NSEQ * 4, D))
        m["cvec"] = f(np.concatenate([inp["c_sample"][c * NSEQ:(c + 1) * NSEQ], inp["c_prompt"][b:b + 1]], axis=0))
        m["sgla"] = f(inp["state_gla"][0, c * NSEQ:(c + 1) * NSEQ])
        m["sconv"] = f(inp["state_conv"][0, c * NSEQ:(c + 1) * NSEQ].reshape(NSEQ * 2, CH))
        maps.append(m)
    return maps


def gather_outputs(res, cfg, n_cores, n_seq_prompt, seq_len):
    NP, NSEQ = cfg["NP"], cfg["NSEQ"]
    y_p = np.zeros((n_seq_prompt, seq_len, D), np.float32)
    y_s = np.zeros((n_cores * NSEQ, 4, D), np.float32)
    gla_p = np.zeros((1, n_seq_prompt, HEADS, HK, HV), np.float32)
    conv_p = np.zeros((1, n_seq_prompt, 2, CH), np.float32)
    gla_s = np.zeros((1, n_cores * NSEQ, HEADS, HK, HV), np.float32)
    conv_s = np.zeros((1, n_cores * NSEQ, 2, CH), np.float32)
    for c in range(n_cores):
        b, half = c // 2, c % 2
        r = res[c]
        y_p[b, half * NP:(half + 1) * NP] = r["y_p"]
        y_s[c * NSEQ:(c + 1) * NSEQ] = r["y_s"].reshape(NSEQ, 4, D)
        if half == 1:
            gla_p[0, b] = r["gla_p"]
            conv_p[0, b] = r["conv_p"]
        gla_s[0, c * NSEQ:(c + 1) * NSEQ] = r["gla_s"]
        conv_s[0, c * NSEQ:(c + 1) * NSEQ] = r["conv_s"].reshape(NSEQ, 2, CH)
    return (y_p, y_s, gla_p, conv_p, gla_s, conv_s)


def kernel(**inputs):
    cfg = FULL
    n_cores = 8
    nc = build(cfg)
    maps = make_in_maps(inputs, cfg, n_cores, 2048, 4)
    res = run_bass_kernel_spmd(nc, maps, core_ids=list(range(n_cores)))
    return gather_outputs(res.results, cfg, n_cores, 4, 2048)
```

```python
import contextlib
import numpy as np
import concourse.bass as bass
import concourse.mybir as mybir
from concourse.bass_utils import run_bass_kernel_spmd

F32 = mybir.dt.float32
BF16 = mybir.dt.bfloat16
AF = mybir.ActivationFunctionType
ALU = mybir.AluOpType

D = 2048
DC = 16
HEADS = 4
HK = 128
HV = 256
DV = 1024
CH = 1024
DIN = 10256
C_Q, C_K, C_V, C_AL, C_G, C_CB, C_CC, C_CHH, C_GA, C_GB = 0, 512, 1024, 2048, 2064, 3088, 4112, 5136, 6160, 8208
EPS = 1e-6
LIMIT = 7.0
ALPHA = 1.702

FULL = dict(NP=1024, NPRE=1024, GT=256, NSEQ=16, E=32, FF=2048, TOPK=4, CAP=416)


class Buf:
    def __init__(self, name):
        self.name = name
        self.w = None
        self.r = []


class Tile:
    def __init__(self, name, t):
        self.name = name
        self.t = t
        self.buf = Buf(name)
        self.dsem = None


class Op:
    __slots__ = ("eng", "fn", "deps", "idx", "signal", "ordinal", "is_dma", "sem", "val", "n")


COMPUTE = ("pe", "act", "dve", "pool")


class Sched:
    def __init__(self, nc, es):
        self.nc = nc
        self.es = es
        self.ops = {k: [] for k in ("pe", "act", "dve", "pool", "sp")}
        self.seen = {k: {} for k in self.ops}
        self.esem = {k: es.enter_context(nc.semaphore("sem_" + k)) for k in COMPUTE}
        self.dsems = []
        self.psum = []
        self.psi = 0
        self.cur = es
        self.pending = {k: [] for k in self.ops}

    def sb(self, name, shape, dt):
        t = self.cur.enter_context(self.nc.sbuf_tensor("s_" + name, list(shape), dt))
        return Tile(name, t)

    def barrier(self):
        deps = []
        for k in COMPUTE:
            if self.ops[k]:
                deps.append(self.ops[k][-1])
        for d in self.dsems:
            if len(d) > 2 and d[2] is not None:
                deps.append(d[2])
        for k in self.ops:
            self.pending[k] = list(deps)

    def init_psum(self, n=8):
        for i in range(n):
            t = self.es.enter_context(self.nc.psum_tensor("ps%d" % i, [128, 512], F32))
            self.psum.append(Tile("ps%d" % i, t))

    def ps(self):
        p = self.psum[self.psi % len(self.psum)]
        self.psi += 1
        return p

    def sub(self, tile, key):
        d = tile.__dict__.setdefault("subs", {})
        if key not in d:
            d[key] = Buf("%s:%s" % (tile.name, key))
        return d[key]

    def new_dsem(self, name):
        s = self.es.enter_context(self.nc.semaphore("dsem_" + name))
        d = [s, 0, None]
        self.dsems.append(d)
        return d

    def _bufs(self, lst):
        out = []
        for x in lst:
            out.append(x.buf if isinstance(x, Tile) else x)
        return out

    def _add(self, eng, fn, r, w, is_dma=False, dsem=None, n=1):
        op = Op()
        op.eng, op.fn, op.is_dma, op.n = eng, fn, is_dma, n
        op.signal = False
        op.ordinal = None
        op.idx = len(self.ops[eng])
        rb, wb = self._bufs(r), self._bufs(w)
        deps = []
        for b in rb:
            if b.w is not None:
                deps.append(b.w)
        for b in wb:
            if b.w is not None:
                deps.append(b.w)
            deps.extend(b.r)
        if self.pending[eng]:
            deps.extend(self.pending[eng])
            self.pending[eng] = []
        keep = []
        seen = self.seen[eng]
        for d in deps:
            if d is op:
                continue
            if d.is_dma:
                key = ("d", id(d.sem))
                if seen.get(key, 0) >= d.val:
                    continue
                seen[key] = d.val
                keep.append(d)
            else:
                if d.eng == eng and eng == "pe":
                    continue
                if d.is_dma:
                    continue
                key = ("c", d.eng)
                if seen.get(key, -1) >= d.idx:
                    continue
                seen[key] = d.idx
                d.signal = True
                keep.append(d)
        op.deps = keep
        if is_dma:
            dsem[1] += 16 * n
            op.sem, op.val = dsem, dsem[1]
            dsem[2] = op
        for b in wb:
            b.w = op
            b.r = []
        for b in rb:
            if b not in wb:
                b.r.append(op)
        self.ops[eng].append(op)
        return op

    def op(self, eng, fn, r=(), w=()):
        return self._add(eng, fn, list(r), list(w))

    def dma(self, q, fn, r=(), w=(), sem=None, n=1):
        if sem is None:
            tl = [x for x in list(w) + list(r) if isinstance(x, Tile)][0]
            if tl.dsem is None:
                tl.dsem = self.new_dsem(tl.name)
            sem = tl.dsem
        return self._add(q, fn, list(r), list(w), is_dma=True, dsem=sem, n=n)

    def emit(self, eng, e):
        ordn = 0
        for op in self.ops[eng]:
            if op.signal:
                ordn += 1
                op.ordinal = ordn

    def assign(self):
        for eng in COMPUTE:
            ordn = 0
            for op in self.ops[eng]:
                if op.signal:
                    ordn += 1
                    op.ordinal = ordn

    def run(self, eng, e):
        for op in self.ops[eng]:
            waits = {}
            for d in op.deps:
                if d.is_dma:
                    k = id(d.sem)
                    if k not in waits or waits[k][1] < d.val:
                        waits[k] = (d.sem[0], d.val)
                else:
                    k = d.eng
                    if k not in waits or waits[k][1] < d.ordinal:
                        waits[k] = (self.esem[d.eng], d.ordinal)
            for sem, val in waits.values():
                e.wait_ge(sem, val)
            insts = op.fn(e)
            if not isinstance(insts, (list, tuple)):
                insts = [insts]
            if op.is_dma:
                assert len(insts) == op.n, (len(insts), op.n)
                for i in insts:
                    i.then_inc(op.sem[0], 16)
            elif op.signal:
                insts[-1].then_inc(self.esem[eng], 1)


def build(cfg, debug=()):
    NP, NPRE, GT, NSEQ, E, FF, TOPK, CAP = (cfg[k] for k in ("NP", "NPRE", "GT", "NSEQ", "E", "FF", "TOPK", "CAP"))
    NS = NSEQ * 4
    NT = NP + NS
    FC = FF // 128
    NR = NSEQ + 1
    nc = bass.Bass("TRN2", target_bir_lowering=False)

    def din(name, shape):
        return nc.dram_tensor(name, list(shape), F32, kind="ExternalInput").ap()

    def dout(name, shape):
        return nc.dram_tensor(name, list(shape), F32, kind="ExternalOutput").ap()

    xp = din("xp", [NP, D])
    xpre = din("xpre", [NPRE, D])
    xs = din("xs", [NS, D])
    cvec = din("cvec", [NR, D])
    pflag = din("pflag", [128, 1])
    sgla = din("sgla", [NSEQ, HEADS, HK, HV])
    sconv = din("sconv", [NSEQ * 2, CH])
    cst = din("cst", [128, 1664])
    w_ada = din("w_ada", [D, 6 * D])
    b_ada = din("b_ada", [96, 128])
    norm1_w = din("norm1_w", [16, 128])
    w_in = din("w_in", [D, DIN])
    w_gk_up = din("w_gk_up", [16, 512])
    b_gk = din("b_gk", [1, 512])
    gla_norm_w = din("gla_norm_w", [2, 128])
    w_conv = din("w_conv", [24, 128])
    w_out = din("w_out", [2048, D])
    norm2_w = din("norm2_w", [16, 128])
    w_router = din("w_router", [D, E])
    b_router = din("b_router", [1, E])
    w_up = din("w_up", [E, D, 2 * FF])
    b_up = din("b_up", [E * 2 * FC, 128])
    w_down = din("w_down", [E, FF, D])
    b_down = din("b_down", [E * DC, 128])
    final_norm_w = din("final_norm_w", [16, 128])

    y_p = dout("y_p", [NP, D])
    y_s = dout("y_s", [NS, D])
    gla_p = dout("gla_p", [HEADS, HK, HV])
    conv_p = dout("conv_p", [2, CH])
    gla_s = dout("gla_s", [NSEQ, HEADS, HK, HV])
    conv_s = dout("conv_s", [NSEQ * 2, CH])
    hscr = nc.dram_tensor("hscr", [DC, 128, NT], F32, kind="Internal").ap()
    dbg = {k: dout("dbg_" + k, shp) for k, shp in debug}

    es = contextlib.ExitStack()
    with es:
        S = Sched(nc, es)
        S.init_psum(8)
        sb = S.sb

        cs = sb("cst", [128, 1664], F32)
        S.dma("sp", lambda e: e.dma_start(out=cs.t[:], in_=cst), w=[cs])
        ident_f = cs.t[:, 0:128]
        eps_c = cs.t[:, 640:641]
        one_c = cs.t[:, 641:642]
        cb = sb("cstb", [128, 640], BF16)
        S.op("dve", lambda e: e.tensor_copy(out=cb.t[:], in_=cs.t[:, 0:640]), r=[cs], w=[cb])
        ident_b = cb.t[:, 0:128]
        maskT_b = cb.t[:, 128:256]
        ones_b = cb.t[:, 512:640]
        maskT_f = cs.t[:, 128:256]
        mrev_f = cs.t[:, 256:384]
        smask_f = cs.t[:, 384:448]
        smrev_f = cs.t[:, 448:512]
        ones_f = cs.t[:, 512:640]
        CST = [cs, cb]

        def load_vecT(name, src, n):
            stg = sb(name + "_stg", [128, 128], F32)
            dst = sb(name, [128, n], F32)
            S.dma("sp", lambda e: e.dma_start(out=stg.t[0:n, :], in_=src), w=[stg])
            p = S.ps()
            S.op("pe", lambda e: e.transpose(p.t[:, 0:n], stg.t[0:n, :], ident_f[0:n, 0:n]), r=[stg] + CST, w=[p])
            S.op("dve", lambda e: e.tensor_copy(out=dst.t[:, :], in_=p.t[:, 0:n]), r=[p], w=[dst])
            return dst

        badaT = load_vecT("badaT", b_ada, 96)
        n1T = load_vecT("n1T", norm1_w, 16)
        n2T = load_vecT("n2T", norm2_w, 16)
        nfT = load_vecT("nfT", final_norm_w, 16)
        gnT = load_vecT("gnT", gla_norm_w, 2)
        wcT = load_vecT("wcT", w_conv, 24)
        pfl = sb("pfl", [128, 1], F32)
        S.dma("sp", lambda e: e.dma_start(out=pfl.t[:], in_=pflag), w=[pfl])

        wgk_f = sb("wgk_f", [16, 512], F32)
        S.dma("sp", lambda e: e.dma_start(out=wgk_f.t[:], in_=w_gk_up), w=[wgk_f])
        wgk = sb("wgk", [16, 512], BF16)
        S.op("dve", lambda e: e.tensor_copy(out=wgk.t[:], in_=wgk_f.t[:]), r=[wgk_f], w=[wgk])
        bgk_f = sb("bgk_f", [1, 512], F32)
        S.dma("sp", lambda e: e.dma_start(out=bgk_f.t[:], in_=b_gk), w=[bgk_f])
        bgk = sb("bgk", [1, 512], BF16)
        S.op("dve", lambda e: e.tensor_copy(out=bgk.t[:], in_=bgk_f.t[:]), r=[bgk_f], w=[bgk])

        NW = 3
        wpool = []
        wstate = [0]
        nw_cur = [NW]

        def alloc_wpool(tag, n=NW):
            wpool[:] = [sb("wt%s%d" % (tag, i), [128, 16, 512], BF16) for i in range(n)]
            nw_cur[0] = n

        def wtile():
            t = wpool[wstate[0] % nw_cur[0]]
            wstate[0] += 1
            return t

        def load_w(src2d, c0, ncols, kc=16, t=None, col_off=0):
            if t is None:
                t = wtile()
            v = src2d.rearrange("(kc p) n -> p kc n", p=128)
            S.dma("pool", lambda e: e.dma_start(out=t.t[:, 0:kc, col_off:col_off + ncols], in_=v[:, :, c0:c0 + ncols]), w=[t])
            return t

        siluT = sb("siluT", [128, 16, NR], BF16)
        modT = sb("modT", [128, 96, NR], F32)
        A1 = sb("A1", [128, 16, NR], F32)
        A2 = sb("A2", [128, 16, NR], F32)
        es_ada = contextlib.ExitStack()
        S.cur = es_ada
        alloc_wpool("A", 4)
        cv = sb("cv", [NR, D], F32)
        S.dma("sp", lambda e: e.dma_start(out=cv.t[:], in_=cvec), w=[cv])
        cvs = sb("cvs", [NR, D], F32)
        S.op("act", lambda e: e.activation(out=cvs.t[:], in_=cv.t[:], func=AF.Silu), r=[cv], w=[cvs])
        p = S.ps()
        for kc in range(16):
            S.op("pe", lambda e, kc=kc, p=p: e.transpose(p.t[:, kc * NR:(kc + 1) * NR], cvs.t[0:NR, kc * 128:(kc + 1) * 128],
                                                       ident_f[0:NR, 0:NR]), r=[cvs] + CST, w=[p])
        S.op("dve", lambda e, p=p: e.tensor_copy(out=siluT.t[:].rearrange("p a b -> p (a b)"), in_=p.t[:, 0:16 * NR]), r=[p], w=[siluT])
        for ct in range(24):
            W = load_w(w_ada, ct * 512, 512)
            for sbk in range(4):
                nb = ct * 4 + sbk
                p = S.ps()
                for kc in range(16):
                    S.op("pe", lambda e, kc=kc, p=p, W=W, sbk=sbk: e.matmul(p.t[:, 0:NR], W.t[:, kc, sbk * 128:(sbk + 1) * 128],
                                                                            siluT.t[:, kc, :], start=(kc == 0), stop=(kc == 15)),
                         r=[W, siluT], w=[p])
                S.op("act", lambda e, p=p, nb=nb: e.activation(out=modT.t[:, nb, :], in_=p.t[:, 0:NR], func=AF.Identity,
                                                               bias=badaT.t[:, nb:nb + 1], scale=1.0), r=[p, badaT], w=[modT])
        for dc in range(16):
            S.op("dve", lambda e, dc=dc: e.tensor_scalar(out=A1.t[:, dc, :], in0=modT.t[:, 16 + dc, :], scalar1=1.0,
                                                         scalar2=n1T.t[:, dc:dc + 1], op0=ALU.add, op1=ALU.mult), r=[modT, n1T], w=[A1])
            S.op("dve", lambda e, dc=dc: e.tensor_scalar(out=A2.t[:, dc, :], in0=modT.t[:, 64 + dc, :], scalar1=1.0,
                                                         scalar2=n2T.t[:, dc:dc + 1], op0=ALU.add, op1=ALU.mult), r=[modT, n2T], w=[A2])
        M_SH1, M_G1, M_SH2, M_G2 = 0, 32, 48, 80
        S.barrier()
        es_ada.close()
        S.cur = es

        xstage = []
        xst = [0]

        def load_xT(src, ntok, dst, toff):
            for tc0 in range(0, ntok, 128):
                n = min(128, ntok - tc0)
                stg = xstage[0]
                xst[0] += 1
                S.dma("sp", lambda e, stg=stg, tc0=tc0, n=n: e.dma_start(out=stg.t[0:n, :], in_=src[tc0:tc0 + n, :]), w=[stg])
                for q4 in range(4):
                    p = S.ps()
                    for i in range(4):
                        dc = q4 * 4 + i
                        S.op("pe", lambda e, p=p, i=i, dc=dc, stg=stg, n=n: e.transpose(p.t[:, i * 128:i * 128 + n], stg.t[0:n, dc * 128:(dc + 1) * 128],
                                                                                          ident_f[0:n, 0:n]), r=[stg] + CST, w=[p])
                    S.op("act" if q4 % 2 else "dve",
                         (lambda e, p=p, q4=q4, tc0=tc0, n=n: e.activation(out=dst.t[:, q4 * 4:(q4 + 1) * 4, toff + tc0:toff + tc0 + n],
                                                                           in_=p.t[:, :].rearrange("p (a b) -> p a b", a=4)[:, :, 0:n], func=AF.Copy))
                         if q4 % 2 else
                         (lambda e, p=p, q4=q4, tc0=tc0, n=n: e.tensor_copy(out=dst.t[:, q4 * 4:(q4 + 1) * 4, toff + tc0:toff + tc0 + n],
                                                                            in_=p.t[:, :].rearrange("p (a b) -> p a b", a=4)[:, :, 0:n])),
                         r=[p], w=[dst])

        sqt = []
        rstd_l = []
        ntmp = []

        def alloc_tmps(tag, with_x=False):
            sqt[:] = [sb("sq%s%d" % (tag, i), [128, 512], BF16) for i in range(2)]
            rstd_l[:] = [sb("rstd%s" % tag, [128, 512], F32)]
            ntmp[:] = [sb("ntmp%s%d" % (tag, i), [128, 512], F32) for i in range(2)]
            if with_x:
                xstage[:] = [sb("xstg%s" % tag, [128, D], F32)]

        def rmsnorm_fm(src, soff, ntok, dst, doff, A, B_mod, sample, post=None):
            rstd = rstd_l[0]
            p = S.ps()
            for dc in range(16):
                sq = sqt[dc % 2]
                S.op("act", lambda e, dc=dc, sq=sq: e.activation(out=sq.t[:, 0:ntok], in_=src.t[:, dc, soff:soff + ntok], func=AF.Square),
                     r=[src], w=[sq])
                S.op("pe", lambda e, dc=dc, sq=sq, p=p: e.matmul(p.t[:, 0:ntok], ones_b, sq.t[:, 0:ntok], start=(dc == 0), stop=(dc == 15)),
                     r=[sq] + CST, w=[p])
            S.op("act", lambda e, p=p: e.activation(out=rstd.t[:, 0:ntok], in_=p.t[:, 0:ntok], func=AF.Sqrt, bias=eps_c, scale=1.0 / D),
                 r=[p] + CST, w=[rstd])
            S.op("dve", lambda e: e.reciprocal(out=rstd.t[:, 0:ntok], in_=rstd.t[:, 0:ntok]), r=[rstd], w=[rstd])
            for dc_ in range(16):
                tm = ntmp[dc_ % 2]
                S.op("dve", lambda e, dc=dc_, tm=tm: e.tensor_tensor(out=tm.t[:, 0:ntok], in0=src.t[:, dc, soff:soff + ntok], in1=rstd.t[:, 0:ntok],
                                                                    op=ALU.mult), r=[src, rstd], w=[tm])
                dc = dc_ if post is None else dc_ % 2
                if not sample:
                    if B_mod is None:
                        S.op("act", lambda e, dc=dc, dcf=dc_, tm=tm: e.activation(out=dst.t[:, dc, doff:doff + ntok], in_=tm.t[:, 0:ntok], func=AF.Copy,
                                                                         scale=A.t[:, dcf:dcf + 1]), r=[tm, A], w=[dst])
                    else:
                        S.op("act", lambda e, dc=dc, dcf=dc_, tm=tm: e.activation(out=dst.t[:, dc, doff:doff + ntok], in_=tm.t[:, 0:ntok], func=AF.Identity,
                                                                         scale=A.t[:, dcf, NSEQ:NSEQ + 1], bias=modT.t[:, B_mod + dcf, NSEQ:NSEQ + 1]),
                             r=[tm, A, modT], w=[dst])
                else:
                    tv = tm.t[:, 0:ntok].rearrange("p (s t) -> p s t", t=4)
                    S.op("dve", lambda e, dc=dc, dcf=dc_, tv=tv: e.tensor_tensor(out=tv, in0=tv, in1=A.t[:, dcf, 0:NSEQ].unsqueeze(2).to_broadcast([128, NSEQ, 4]),
                                                                        op=ALU.mult), r=[tm, A], w=[tm])
                    S.op("dve", lambda e, dc=dc, dcf=dc_, tv=tv: e.tensor_tensor(out=dst.t[:, dc, doff:doff + ntok].rearrange("p (s t) -> p s t", t=4), in0=tv,
                                                                        in1=modT.t[:, B_mod + dcf, 0:NSEQ].unsqueeze(2).to_broadcast([128, NSEQ, 4]),
                                                                        op=ALU.add), r=[tm, modT], w=[dst])
                if post is not None:
                    post(dc_, dc)

        es_mix = contextlib.ExitStack()
        S.cur = es_mix
        alloc_wpool("B", 4)
        alloc_tmps("B", with_x=True)
        xg = sb("xg", [128, 16, GT], F32)
        xn = sb("xn", [128, 16, GT], BF16)
        NTC = GT // 128
        qT = sb("qT", [128, 4, GT], F32)
        kT = sb("kT", [128, 4, GT], F32)
        ktm = sb("ktm", [128, NTC, 512], F32)
        vtm = sb("vtm", [128, NTC, 1024], BF16)
        Ltm = sb("Ltm", [128, NTC, 512], F32)
        alT = sb("alT", [16, GT], BF16)
        sgT = sb("sgT", [128, 8, GT], BF16)
        oT = sb("oT", [128, 8, GT], F32)
        oaT = sb("oaT", [128, 8, GT], BF16)
        obT = sb("obT", [128, 8, GT], BF16)
        Sst = [sb("Sst%d" % h, [128, 256], F32) for h in range(4)]
        Sbf = [sb("Sbf%d" % h, [128, 256], BF16) for h in range(4)]
        halo = sb("halo", [128, 8, 2], F32)
        for h in range(4):
            S.op("dve", lambda e, h=h: e.memset(Sst[h].t[:], 0.0), w=[Sst[h]])
            S.op("dve", lambda e, h=h: e.memset(Sbf[h].t[:], 0.0), w=[Sbf[h]])
        S.op("dve", lambda e: e.memset(halo.t[:], 0.0), w=[halo])
        expb = sb("expb", [128, 128], F32)
        expnb = sb("expnb", [128, 128], F32)
        qp = sb("qp", [128, 128], BF16)
        kp = sb("kp", [128, 128], BF16)
        kk = sb("kk", [128, 512], BF16)
        erev = sb("erev", [128, 512], F32)
        PTm = sb("PTm", [128, 128], BF16)
        gtmp = [sb("gtmp%d" % i, [128, GT], F32) for i in range(4)]
        cct = sb("cct", [128, 4, GT], F32)
        uext = sb("uext", [128, 4, GT + 2 * max(1, NSEQ)], F32)
        cnv = sb("cnv", [128, GT], F32)
        s0f = [sb("s0f%d" % i, [128, 256], F32) for i in range(2)]
        s0b = [sb("s0b%d" % i, [128, 256], BF16) for i in range(2)]
        snew = [sb("snew%d" % i, [128, 256], F32) for i in range(2)]
        ohs = sb("ohs", [128, NSEQ], F32)
        S.op("dve", lambda e: e.tensor_copy(out=ohs.t[:, :], in_=cs.t[:, 642:642 + NSEQ]), r=[cs], w=[ohs])
        scv = sb("scv", [128, 8, NSEQ * 2], F32)
        ctm = sb("ctm", [NSEQ * 2, CH], F32)
        S.dma("sp", lambda e: e.dma_start(out=ctm.t[:], in_=sconv), w=[ctm])
        p = S.ps()
        for blk in range(8):
            S.op("pe", lambda e, blk=blk, p=p: e.transpose(p.t[:, blk * NSEQ * 2:(blk + 1) * NSEQ * 2], ctm.t[0:NSEQ * 2, blk * 128:(blk + 1) * 128],
                                                         ident_f[0:NSEQ * 2, 0:NSEQ * 2]), r=[ctm] + CST, w=[p])
        S.op("dve", lambda e, p=p: e.tensor_copy(out=scv.t[:].rearrange("p a b -> p (a b)"), in_=p.t[:, 0:8 * NSEQ * 2]), r=[p], w=[scv])

        def proj_fm(W, wc0, ntok, t0, M=128):
            p = S.ps()
            for kc in range(16):
                S.op("pe", lambda e, kc=kc, p=p: e.matmul(p.t[0:M, 0:ntok], W.t[:, kc, wc0:wc0 + M], xn.t[:, kc, t0:t0 + ntok],
                                                        start=(kc == 0), stop=(kc == 15)), r=[W, xn], w=[p])
            return p

        def proj_tm(W, tc, n, ncols=512):
            p = S.ps()
            for kc in range(16):
                S.op("pe", lambda e, kc=kc, p=p: e.matmul(p.t[0:n, 0:ncols], xn.t[:, kc, tc * 128:tc * 128 + n], W.t[:, kc, 0:ncols],
                                                        start=(kc == 0), stop=(kc == 15)), r=[W, xn], w=[p])
            return p

        def mixer_group(kind, src, ntok, hoff, last_prefix=False):
            sample = kind == "samp"
            full = kind != "pre"
            ntc = (ntok + 127) // 128
            load_xT(src, ntok, xg, 0)
            rmsnorm_fm(xg, 0, ntok, xn, 0, A1, M_SH1, sample)
            if full:
                W = load_w(w_in, C_Q, 512)
                for h in range(4):
                    p = proj_fm(W, h * 128, ntok, 0)
                    S.op("act", lambda e, p=p, h=h: e.activation(out=qT.t[:, h, 0:ntok], in_=p.t[:, 0:ntok], func=AF.Copy, scale=HK ** -0.5),
                         r=[p], w=[qT])
            W = load_w(w_in, C_K, 512)
            if full:
                for h in range(4):
                    p = proj_fm(W, h * 128, ntok, 0)
                    S.op("dve", lambda e, p=p, h=h: e.tensor_copy(out=kT.t[:, h, 0:ntok], in_=p.t[:, 0:ntok]), r=[p], w=[kT])
            for tc in range(ntc):
                n = min(128, ntok - tc * 128)
                p = proj_tm(W, tc, n)
                S.op("act", lambda e, p=p, tc=tc, n=n: e.activation(out=ktm.t[0:n, tc, :], in_=p.t[0:n, :], func=AF.Copy), r=[p], w=[ktm])
            for vt in range(2):
                W = load_w(w_in, C_V + vt * 512, 512)
                for tc in range(ntc):
                    n = min(128, ntok - tc * 128)
                    p = proj_tm(W, tc, n)
                    S.op("dve" if vt else "act",
                         (lambda e, p=p, tc=tc, n=n, vt=vt: e.tensor_copy(out=vtm.t[0:n, tc, vt * 512:(vt + 1) * 512], in_=p.t[0:n, :])) if vt else
                         (lambda e, p=p, tc=tc, n=n, vt=vt: e.activation(out=vtm.t[0:n, tc, vt * 512:(vt + 1) * 512], in_=p.t[0:n, :], func=AF.Copy)),
                         r=[p], w=[vtm])
            W = load_w(w_in, C_AL, 16)
            p = proj_fm(W, 0, ntok, 0, M=16)
            S.op("dve", lambda e, p=p: e.tensor_copy(out=alT.t[:, 0:ntok], in_=p.t[0:16, 0:ntok]), r=[p], w=[alT])
            for tc in range(ntc):
                n = min(128, ntok - tc * 128)
                p = S.ps()
                S.op("pe", lambda e, p=p, tc=tc, n=n: e.matmul(p.t[0:n, :], alT.t[:, tc * 128:tc * 128 + n], wgk.t[:, :], start=True, stop=False),
                     r=[alT, wgk], w=[p])
                S.op("pe", lambda e, p=p, n=n: e.matmul(p.t[0:n, :], ones_b[0:1, 0:n], bgk.t[:, :], start=False, stop=True), r=[bgk] + CST, w=[p])
                S.op("act", lambda e, p=p, tc=tc, n=n: e.activation(out=Ltm.t[0:n, tc, :], in_=p.t[0:n, :], func=AF.Exp, scale=-1.0), r=[p], w=[Ltm])
                S.op("act", lambda e, tc=tc, n=n: e.activation(out=Ltm.t[0:n, tc, :], in_=Ltm.t[0:n, tc, :], func=AF.Ln, bias=one_c[0:n, :], scale=1.0),
                     r=[Ltm] + CST, w=[Ltm])
            if full:
                for gt in range(2):
                    W = load_w(w_in, C_G + gt * 512, 512)
                    for b4 in range(4):
                        p = proj_fm(W, b4 * 128, ntok, 0)
                        S.op("act", lambda e, p=p, gt=gt, b4=b4: e.activation(out=sgT.t[:, gt * 4 + b4, 0:ntok], in_=p.t[:, 0:ntok], func=AF.Silu),
                             r=[p], w=[sgT])
            for tc in range(ntc):
                n = min(128, ntok - tc * 128)
                mk_f = smask_f[0:n, 0:n] if sample else maskT_f[0:n, 0:n]
                mr_f = smrev_f[0:n, 0:n] if sample else mrev_f[0:n, 0:n]
                p = S.ps()
                S.op("pe", lambda e, p=p, tc=tc, n=n, mr_f=mr_f: e.matmul(p.t[0:n, :], mr_f, Ltm.t[0:n, tc, :], start=True, stop=True),
                     r=[Ltm] + CST, w=[p])
                S.op("act", lambda e, p=p, n=n: e.activation(out=erev.t[0:n, :], in_=p.t[0:n, :], func=AF.Exp, scale=-1.0 / 16), r=[p], w=[erev])
                S.op("dve", lambda e, tc=tc, n=n: e.tensor_tensor(out=kk.t[0:n, :], in0=ktm.t[0:n, tc, :], in1=erev.t[0:n, :], op=ALU.mult),
                     r=[ktm, erev], w=[kk])
                for h in range(4):
                    pb = S.ps()
                    S.op("pe", lambda e, pb=pb, tc=tc, n=n, h=h, mk_f=mk_f: e.matmul(pb.t[:, 0:n], Ltm.t[0:n, tc, h * 128:(h + 1) * 128], mk_f,
                                                                                    start=True, stop=True), r=[Ltm] + CST, w=[pb])
                    S.op("act", lambda e, pb=pb, n=n: e.activation(out=expb.t[:, 0:n], in_=pb.t[:, 0:n], func=AF.Exp, scale=-1.0 / 16), r=[pb], w=[expb])
                    if full:
                        S.op("act", lambda e, pb=pb, n=n: e.activation(out=expnb.t[:, 0:n], in_=pb.t[:, 0:n], func=AF.Exp, scale=1.0 / 16), r=[pb], w=[expnb])
                        S.op("dve", lambda e, h=h, tc=tc, n=n: e.tensor_tensor(out=qp.t[:, 0:n], in0=qT.t[:, h, tc * 128:tc * 128 + n], in1=expb.t[:, 0:n],
                                                                               op=ALU.mult), r=[qT, expb], w=[qp])
                        S.op("dve", lambda e, h=h, tc=tc, n=n: e.tensor_tensor(out=kp.t[:, 0:n], in0=kT.t[:, h, tc * 128:tc * 128 + n], in1=expnb.t[:, 0:n],
                                                                               op=ALU.mult), r=[kT, expnb], w=[kp])
                        pp = S.ps()
                        S.op("pe", lambda e, pp=pp, n=n: e.matmul(pp.t[0:n, 0:n], kp.t[:, 0:n], qp.t[:, 0:n], start=True, stop=True), r=[kp, qp], w=[pp])
                        S.op("dve", lambda e, pp=pp, n=n, mk_f=mk_f: e.tensor_tensor(out=PTm.t[0:n, 0:n], in0=pp.t[0:n, 0:n], in1=mk_f, op=ALU.mult),
                             r=[pp] + CST, w=[PTm])
                        if not sample:
                            po = S.ps()
                            for eb in range(2):
                                S.op("pe", lambda e, po=po, eb=eb, n=n, tc=tc, h=h: e.matmul(po.t[:, eb * 128:eb * 128 + n],
                                                                                            vtm.t[0:n, tc, h * 256 + eb * 128:h * 256 + (eb + 1) * 128],
                                                                                            PTm.t[0:n, 0:n], start=True, stop=False), r=[vtm, PTm], w=[po])
                                S.op("pe", lambda e, po=po, eb=eb, n=n, h=h: e.matmul(po.t[:, eb * 128:eb * 128 + n], Sbf[h].t[:, eb * 128:(eb + 1) * 128],
                                                                                     qp.t[:, 0:n], start=False, stop=True), r=[Sbf[h], qp], w=[po])
                            S.op("act", lambda e, po=po, h=h, tc=tc, n=n: e.activation(out=oT.t[:, 2 * h:2 * h + 2, tc * 128:tc * 128 + n],
                                                                                       in_=po.t[:, 0:256].rearrange("p (a b) -> p a b", a=2)[:, :, 0:n],
                                                                                       func=AF.Copy), r=[po], w=[oT])
                    if not sample:
                        pd = S.ps()
                        S.op("pe", lambda e, pd=pd, n=n, tc=tc, h=h: e.matmul(pd.t[:, 0:256], kk.t[0:n, h * 128:(h + 1) * 128], vtm.t[0:n, tc, h * 256:(h + 1) * 256],
                                                                             start=True, stop=True), r=[kk, vtm], w=[pd])
                        S.op("dve", lambda e, pd=pd, h=h, n=n: e.scalar_tensor_tensor(out=Sst[h].t[:, :], in0=Sst[h].t[:, :], scalar=expb.t[:, n - 1:n], in1=pd.t[:, 0:256],
                                                                                      op0=ALU.mult, op1=ALU.add), r=[Sst[h], expb, pd], w=[Sst[h]])
                        S.op("act", lambda e, h=h: e.activation(out=Sbf[h].t[:, :], in_=Sst[h].t[:, :], func=AF.Copy), r=[Sst[h]], w=[Sbf[h]])
                    else:
                        for s in range(NSEQ):
                            i2 = (s * 4 + h) % 2
                            S.dma("sp", lambda e, s=s, h=h, i2=i2: e.dma_start(out=s0f[i2].t[:, :], in_=sgla[s, h]), w=[s0f[i2]])
                            S.op("act", lambda e, i2=i2: e.activation(out=s0b[i2].t[:, :], in_=s0f[i2].t[:, :], func=AF.Copy), r=[s0f[i2]], w=[s0b[i2]])
                            po = S.ps()
                            for eb in range(2):
                                S.op("pe", lambda e, po=po, eb=eb, n=n, h=h, s=s: e.matmul(po.t[:, eb * 4:eb * 4 + 4],
                                                                                          vtm.t[0:n, 0, h * 256 + eb * 128:h * 256 + (eb + 1) * 128],
                                                                                          PTm.t[0:n, s * 4:s * 4 + 4], start=True, stop=False), r=[vtm, PTm], w=[po])
                                S.op("pe", lambda e, po=po, eb=eb, i2=i2, s=s: e.matmul(po.t[:, eb * 4:eb * 4 + 4], s0b[i2].t[:, eb * 128:(eb + 1) * 128],
                                                                                       qp.t[:, s * 4:s * 4 + 4], start=False, stop=True), r=[s0b[i2], qp], w=[po])
                            S.op("act", lambda e, po=po, h=h, s=s: e.activation(out=oT.t[:, 2 * h:2 * h + 2, s * 4:s * 4 + 4],
                                                                                in_=po.t[:, 0:8].rearrange("p (a b) -> p a b", a=2), func=AF.Copy), r=[po], w=[oT])
                            S.op("dve", lambda e, s=s, h=h, n=n: e.tensor_scalar(out=kp.t[0:n, :], in0=kk.t[0:n, h * 128:(h + 1) * 128], scalar1=ohs.t[0:n, s:s + 1],
                                                                               scalar2=None, op0=ALU.mult), r=[kk, ohs], w=[kp])
                            pd = S.ps()
                            S.op("pe", lambda e, pd=pd, n=n, h=h: e.matmul(pd.t[:, 0:256], kp.t[0:n, :], vtm.t[0:n, 0, h * 256:(h + 1) * 256], start=True, stop=True),
                                 r=[kp, vtm], w=[pd])
                            S.op("dve", lambda e, pd=pd, i2=i2, s=s: e.scalar_tensor_tensor(out=snew[i2].t[:, :], in0=s0f[i2].t[:, :], scalar=expb.t[:, s * 4 + 3:s * 4 + 4],
                                                                                           in1=pd.t[:, 0:256], op0=ALU.mult, op1=ALU.add),
                                 r=[s0f[i2], expb, pd], w=[snew[i2]])
                            S.dma("sp", lambda e, s=s, h=h, i2=i2: e.dma_start(out=gla_s[s, h], in_=snew[i2].t[:, :]), r=[snew[i2]])
            if last_prefix:
                for h in range(4):
                    S.op("dve", lambda e, h=h: e.tensor_scalar(out=Sst[h].t[:, :], in0=Sst[h].t[:, :], scalar1=pfl.t[:, 0:1], scalar2=None, op0=ALU.mult),
                         r=[Sst[h], pfl], w=[Sst[h]])
                    S.op("act", lambda e, h=h: e.activation(out=Sbf[h].t[:, :], in_=Sst[h].t[:, :], func=AF.Copy), r=[Sst[h]], w=[Sbf[h]])
            rstd = rstd_l[0]
            if full:
                for h in range(4):
                    p = S.ps()
                    for eb in range(2):
                        sq = ntmp[eb]
                        S.op("act", lambda e, sq=sq, h=h, eb=eb: e.activation(out=sq.t[:, 0:ntok], in_=oT.t[:, 2 * h + eb, 0:ntok], func=AF.Square), r=[oT], w=[sq])
                        S.op("pe", lambda e, sq=sq, p=p, eb=eb: e.matmul(p.t[:, 0:ntok], ones_f, sq.t[:, 0:ntok], start=(eb == 0), stop=(eb == 1)),
                             r=[sq] + CST, w=[p])
                    S.op("act", lambda e, p=p: e.activation(out=rstd.t[:, 0:ntok], in_=p.t[:, 0:ntok], func=AF.Sqrt, bias=eps_c, scale=1.0 / HV),
                         r=[p] + CST, w=[rstd])
                    S.op("dve", lambda e: e.reciprocal(out=rstd.t[:, 0:ntok], in_=rstd.t[:, 0:ntok]), r=[rstd], w=[rstd])
                    for eb in range(2):
                        tm = gtmp[eb]
                        S.op("dve", lambda e, tm=tm, h=h, eb=eb: e.tensor_tensor(out=tm.t[:, 0:ntok], in0=oT.t[:, 2 * h + eb, 0:ntok], in1=rstd.t[:, 0:ntok], op=ALU.mult),
                             r=[oT, rstd], w=[tm])
                        S.op("dve", lambda e, tm=tm, h=h, eb=eb: e.scalar_tensor_tensor(out=oaT.t[:, 2 * h + eb, 0:ntok], in0=tm.t[:, 0:ntok], scalar=gnT.t[:, eb:eb + 1],
                                                                                       in1=sgT.t[:, 2 * h + eb, 0:ntok], op0=ALU.mult, op1=ALU.mult),
                             r=[tm, gnT, sgT], w=[oaT])
            if full or last_prefix:
                for half in range(2):
                    if sample:
                        ue = uext.t[:, :, 0:NSEQ * 6].rearrange("p b (s t) -> p b s t", t=6)
                        for b4 in range(4):
                            S.op("dve", lambda e, b4=b4, half=half, ue=ue: e.tensor_copy(out=ue[:, b4, :, 0:2],
                                                                                        in_=scv.t[:, half * 4 + b4, :].rearrange("p (s t) -> p s t", t=2)),
                                 r=[scv], w=[uext])
                    elif full:
                        S.op("dve", lambda e, half=half: e.tensor_copy(out=uext.t[:, :, 0:2], in_=halo.t[:, half * 4:half * 4 + 4, :]), r=[halo], w=[uext])
                    t0 = 0 if full else ntok - 2
                    nn = ntok - t0
                    W = load_w(w_in, C_CC + half * 512, 512)
                    for b4 in range(4):
                        p = proj_fm(W, b4 * 128, nn, t0)
                        S.op("act", lambda e, p=p, b4=b4, nn=nn: e.activation(out=cct.t[:, b4, 0:nn], in_=p.t[:, 0:nn], func=AF.Copy), r=[p], w=[cct])
                    W = load_w(w_in, C_CHH + half * 512, 512)
                    for b4 in range(4):
                        p = proj_fm(W, b4 * 128, nn, t0)
                        if sample:
                            ue = uext.t[:, :, 0:NSEQ * 6].rearrange("p b (s t) -> p b s t", t=6)
                            S.op("dve", lambda e, p=p, b4=b4, ue=ue: e.tensor_tensor(out=ue[:, b4, :, 2:6], in0=p.t[:, 0:ntok].rearrange("p (s t) -> p s t", t=4),
                                                                                    in1=cct.t[:, b4, 0:ntok].rearrange("p (s t) -> p s t", t=4), op=ALU.mult),
                                 r=[p, cct], w=[uext])
                        elif full:
                            S.op("dve", lambda e, p=p, b4=b4: e.tensor_tensor(out=uext.t[:, b4, 2:2 + ntok], in0=p.t[:, 0:ntok], in1=cct.t[:, b4, 0:ntok], op=ALU.mult),
                                 r=[p, cct], w=[uext])
                        else:
                            S.op("dve", lambda e, p=p, b4=b4, half=half: e.scalar_tensor_tensor(out=halo.t[:, half * 4 + b4, :], in0=p.t[:, 0:2], scalar=pfl.t[:, 0:1],
                                                                                               in1=cct.t[:, b4, 0:2], op0=ALU.mult, op1=ALU.mult),
                                 r=[p, pfl, cct], w=[halo])
                    if not full:
                        continue
                    W = load_w(w_in, C_CB + half * 512, 512)
                    for b4 in range(4):
                        blk = half * 4 + b4
                        p = proj_fm(W, b4 * 128, ntok, 0)
                        if sample:
                            ue = uext.t[:, :, 0:NSEQ * 6].rearrange("p b (s t) -> p b s t", t=6)
                            cv3 = cnv.t[:, 0:ntok].rearrange("p (s t) -> p s t", t=4)
                            u0, u1, u2 = ue[:, b4, :, 0:4], ue[:, b4, :, 1:5], ue[:, b4, :, 2:6]
                        else:
                            cv3 = cnv.t[:, 0:ntok]
                            u0, u1, u2 = uext.t[:, b4, 0:ntok], uext.t[:, b4, 1:1 + ntok], uext.t[:, b4, 2:2 + ntok]
                        S.op("dve", lambda e, cv3=cv3, u0=u0, blk=blk: e.tensor_scalar(out=cv3, in0=u0, scalar1=wcT.t[:, blk:blk + 1], scalar2=None, op0=ALU.mult),
                             r=[uext, wcT], w=[cnv])
                        S.op("dve", lambda e, cv3=cv3, u1=u1, blk=blk: e.scalar_tensor_tensor(out=cv3, in0=u1, scalar=wcT.t[:, 8 + blk:9 + blk], in1=cv3,
                                                                                             op0=ALU.mult, op1=ALU.add), r=[uext, wcT, cnv], w=[cnv])
                        S.op("dve", lambda e, cv3=cv3, u2=u2, blk=blk: e.scalar_tensor_tensor(out=cv3, in0=u2, scalar=wcT.t[:, 16 + blk:17 + blk], in1=cv3,
                                                                                             op0=ALU.mult, op1=ALU.add), r=[uext, wcT, cnv], w=[cnv])
                        S.op("dve", lambda e, p=p, blk=blk: e.tensor_tensor(out=obT.t[:, blk, 0:ntok], in0=p.t[:, 0:ntok], in1=cnv.t[:, 0:ntok], op=ALU.mult),
                             r=[p, cnv], w=[obT])
                    if sample:
                        ue = uext.t[:, :, 0:NSEQ * 6].rearrange("p b (s t) -> p b s t", t=6)
                        for b4 in range(4):
                            S.op("dve", lambda e, b4=b4, half=half, ue=ue: e.tensor_copy(out=scv.t[:, half * 4 + b4, :].rearrange("p (s t) -> p s t", t=2),
                                                                                        in_=ue[:, b4, :, 4:6]), r=[uext], w=[scv])
                    else:
                        S.op("dve", lambda e, half=half: e.tensor_copy(out=halo.t[:, half * 4:half * 4 + 4, :], in_=uext.t[:, :, ntok:ntok + 2]), r=[uext], w=[halo])
            if not full:
                return
            for t4 in range(4):
                Wo = load_w(w_out, t4 * 512, 512)
                Wa = load_w(w_in, C_GA + t4 * 512, 512)
                Wb = load_w(w_in, C_GB + t4 * 512, 512)
                for sbk in range(4):
                    j = t4 * 4 + sbk
                    pA = S.ps()
                    for kc in range(8):
                        S.op("pe", lambda e, kc=kc, pA=pA, Wo=Wo, sbk=sbk: e.matmul(pA.t[:, 0:ntok], Wo.t[:, kc, sbk * 128:(sbk + 1) * 128], oaT.t[:, kc, 0:ntok],
                                                                                   start=(kc == 0), stop=(kc == 7)), r=[Wo, oaT], w=[pA])
                    pB = S.ps()
                    for kc in range(8):
                        S.op("pe", lambda e, kc=kc, pB=pB, Wo=Wo, sbk=sbk: e.matmul(pB.t[:, 0:ntok], Wo.t[:, 8 + kc, sbk * 128:(sbk + 1) * 128], obT.t[:, kc, 0:ntok],
                                                                                   start=(kc == 0), stop=(kc == 7)), r=[Wo, obT], w=[pB])
                    pGa = proj_fm(Wa, sbk * 128, ntok, 0)
                    pGb = proj_fm(Wb, sbk * 128, ntok, 0)
                    S.op("act", lambda e, pGa=pGa: e.activation(out=gtmp[0].t[:, 0:ntok], in_=pGa.t[:, 0:ntok], func=AF.Sigmoid), r=[pGa], w=[gtmp[0]])
                    S.op("act", lambda e, pGb=pGb: e.activation(out=gtmp[1].t[:, 0:ntok], in_=pGb.t[:, 0:ntok], func=AF.Sigmoid), r=[pGb], w=[gtmp[1]])
                    S.op("dve", lambda e, pA=pA: e.tensor_tensor(out=gtmp[0].t[:, 0:ntok], in0=pA.t[:, 0:ntok], in1=gtmp[0].t[:, 0:ntok], op=ALU.mult),
                         r=[pA, gtmp[0]], w=[gtmp[0]])
                    S.op("dve", lambda e, pB=pB: e.tensor_tensor(out=gtmp[1].t[:, 0:ntok], in0=pB.t[:, 0:ntok], in1=gtmp[1].t[:, 0:ntok], op=ALU.mult),
                         r=[pB, gtmp[1]], w=[gtmp[1]])
                    S.op("dve", lambda e: e.tensor_tensor(out=gtmp[0].t[:, 0:ntok], in0=gtmp[0].t[:, 0:ntok], in1=gtmp[1].t[:, 0:ntok], op=ALU.add),
                         r=[gtmp[0], gtmp[1]], w=[gtmp[0]])
                    if not sample:
                        S.op("dve", lambda e, j=j: e.scalar_tensor_tensor(out=xg.t[:, j, 0:ntok], in0=gtmp[0].t[:, 0:ntok], scalar=modT.t[:, M_G1 + j, NSEQ:NSEQ + 1],
                                                                         in1=xg.t[:, j, 0:ntok], op0=ALU.mult, op1=ALU.add), r=[gtmp[0], modT, xg], w=[xg])
                    else:
                        g3 = gtmp[0].t[:, 0:ntok].rearrange("p (s t) -> p s t", t=4)
                        S.op("dve", lambda e, j=j, g3=g3: e.tensor_tensor(out=g3, in0=g3, in1=modT.t[:, M_G1 + j, 0:NSEQ].unsqueeze(2).to_broadcast([128, NSEQ, 4]),
                                                                         op=ALU.mult), r=[gtmp[0], modT], w=[gtmp[0]])
                        S.op("dve", lambda e, j=j: e.tensor_tensor(out=xg.t[:, j, 0:ntok], in0=gtmp[0].t[:, 0:ntok], in1=xg.t[:, j, 0:ntok], op=ALU.add),
                             r=[gtmp[0], xg], w=[xg])
            S.dma("sp", lambda e: e.dma_start(out=hscr[:, :, hoff:hoff + ntok].rearrange("c p t -> p c t"), in_=xg.t[:, :, 0:ntok]), r=[xg], w=[hbuf], sem=hsem)

        hsem = S.new_dsem("hscr")
        hbuf = Buf("hscr")

        npg = NPRE // GT
        for g in range(npg):
            mixer_group("pre", xpre[g * GT:(g + 1) * GT, :], GT, 0, last_prefix=(g == npg - 1))
        for g in range(NP // GT):
            mixer_group("main", xp[g * GT:(g + 1) * GT, :], GT, g * GT)
        for h in range(4):
            S.dma("sp", lambda e, h=h: e.dma_start(out=gla_p[h], in_=Sst[h].t[:, :]), r=[Sst[h]])
        S.dma("sp", lambda e: [e.dma_start(out=conv_p[:, b * 128:(b + 1) * 128].rearrange("t p -> p t"), in_=halo.t[:, b, :], allow_slow_non_contiguous=True)
                               for b in range(8)], r=[halo], n=8)
        mixer_group("samp", xs, NS, NP)
        cso = ctm
        for hb in range(2):
            p = S.ps()
            for b4 in range(4):
                S.op("pe", lambda e, p=p, b4=b4, hb=hb: e.transpose(p.t[0:NSEQ * 2, b4 * 128:(b4 + 1) * 128], scv.t[:, hb * 4 + b4, :], ident_f), r=[scv] + CST, w=[p])
            S.op("dve", lambda e, p=p, hb=hb: e.tensor_copy(out=cso.t[:, hb * 512:(hb + 1) * 512], in_=p.t[0:NSEQ * 2, :]), r=[p], w=[cso])
        S.dma("sp", lambda e: e.dma_start(out=conv_s, in_=cso.t[:, :]), r=[cso])

        S.barrier()
        es_mix.close()
        S.cur = es
        NCH = (NT + 127) // 128
        NBLK = (CAP + 127) // 128
        BLKS = [(b * 128, min(128, CAP - b * 128)) for b in range(NBLK)]
        oscr = nc.dram_tensor("oscr", [E, 128, NBLK, D], BF16, kind="Internal").ap()
        obuf = Buf("oscr")
        iota_f = cs.t[:, 1152:1152 + CAP]
        mstrict_f = cs.t[:, 1024:1152]
        osc_sem = S.new_dsem("oscr")
        gwT = sb("gwT", [E, NT], F32)
        rkT = sb("rkT", [E, NT], F32)
        esel = sb("esel", [E, 128], F32)
        es_h = contextlib.ExitStack()
        S.cur = es_h
        alloc_wpool("C", 4)
        hntm = sb("hntm", [128, NCH, D], BF16)
        gwtm = sb("gwtm", [128, NCH, E], F32)
        mktm = sb("mktm", [128, NCH, E], F32)
        rktm = sb("rktm", [128, NCH, E], F32)
        top8 = sb("top8", [128, 8], F32)
        nmx = sb("nmx", [128, 1], F32)
        ssum = sb("ssum", [128, 1], F32)
        wr_f = sb("wr_f", [128, 16, E], F32)
        S.dma("sp", lambda e: e.dma_start(out=wr_f.t[:], in_=w_router.rearrange("(kc p) n -> p kc n", p=128)), w=[wr_f])
        wr = sb("wr", [128, 16, E], BF16)
        S.op("dve", lambda e: e.tensor_copy(out=wr.t[:], in_=wr_f.t[:]), r=[wr_f], w=[wr])
        br_f = sb("br_f", [1, E], F32)
        S.dma("sp", lambda e: e.dma_start(out=br_f.t[:], in_=b_router), w=[br_f])
        br = sb("br", [1, E], BF16)
        S.op("dve", lambda e: e.tensor_copy(out=br.t[:], in_=br_f.t[:]), r=[br_f], w=[br])
        S.op("dve", lambda e: e.memset(mktm.t[:], 0.0), w=[mktm])

        es_m1 = contextlib.ExitStack()
        S.cur = es_m1
        alloc_tmps("M1")
        hTt = sb("hTt", [128, 16, 512], F32)
        hnT = sb("hnT", [128, 16, 512], BF16)
        hnf = sb("hnf", [128, 2, 512], F32)
        lg = sb("lg", [128, E], F32)
        tiles = [(c, min(512, NP - c), False) for c in range(0, NP, 512)] + [(NP, NS, True)]
        for (t0, nn, smp) in tiles:
            S.dma("sp", lambda e, t0=t0, nn=nn: e.dma_start(out=hTt.t[:, :, 0:nn], in_=hscr[:, :, t0:t0 + nn].rearrange("c p t -> p c t")), w=[hTt], r=[hbuf])

            def post(dcf, dc, t0=t0, nn=nn):
                S.op("act", lambda e: e.activation(out=hnT.t[:, dcf, 0:nn], in_=hnf.t[:, dc, 0:nn], func=AF.Copy), r=[hnf], w=[hnT])
                p = S.ps()
                nsub = (nn + 127) // 128
                for c4 in range(nsub):
                    n = min(128, nn - c4 * 128)
                    S.op("pe", lambda e, p=p, c4=c4, n=n: e.transpose(p.t[0:n, c4 * 128:(c4 + 1) * 128], hnf.t[:, dc, c4 * 128:c4 * 128 + n], ident_f),
                         r=[hnf] + CST, w=[p])
                tc0 = t0 // 128
                if nn % 128 == 0:
                    S.op("dve", lambda e, p=p: e.tensor_copy(out=hntm.t[:, tc0:tc0 + nsub, dcf * 128:(dcf + 1) * 128],
                                                             in_=p.t[:, 0:nsub * 128].rearrange("p (c f) -> p c f", f=128)), r=[p], w=[hntm])
                else:
                    assert nsub == 1
                    S.op("dve", lambda e, p=p: e.tensor_copy(out=hntm.t[0:nn, tc0, dcf * 128:(dcf + 1) * 128], in_=p.t[0:nn, 0:128]), r=[p], w=[hntm])
            rmsnorm_fm(hTt, 0, nn, hnf, 0, A2, M_SH2, smp, post=post)
            for c0 in range(0, nn, 128):
                n = min(128, nn - c0)
                ch = (t0 + c0) // 128
                p = S.ps()
                for kc in range(16):
                    S.op("pe", lambda e, p=p, kc=kc, c0=c0, n=n: e.matmul(p.t[0:n, 0:E], hnT.t[:, kc, c0:c0 + n], wr.t[:, kc, :], start=(kc == 0), stop=False),
                         r=[hnT, wr], w=[p])
                S.op("pe", lambda e, p=p, n=n: e.matmul(p.t[0:n, 0:E], ones_b[0:1, 0:n], br.t[:, :], start=False, stop=True), r=[br] + CST, w=[p])
                S.op("dve", lambda e, p=p, n=n: e.tensor_copy(out=lg.t[0:n, :], in_=p.t[0:n, 0:E]), r=[p], w=[lg])
                S.op("dve", lambda e, n=n: e.max(out=top8.t[0:n, :], in_=lg.t[0:n, :]), r=[lg], w=[top8])
                S.op("dve", lambda e, n=n, ch=ch: e.tensor_scalar(out=mktm.t[0:n, ch, :], in0=lg.t[0:n, :], scalar1=top8.t[0:n, TOPK - 1:TOPK], scalar2=None, op0=ALU.is_ge),
                     r=[lg, top8], w=[mktm])
                S.op("dve", lambda e, n=n: e.tensor_scalar(out=nmx.t[0:n, :], in0=top8.t[0:n, 0:1], scalar1=-1.0, scalar2=None, op0=ALU.mult), r=[top8], w=[nmx])
                S.op("act", lambda e, n=n: e.activation(out=lg.t[0:n, :], in_=lg.t[0:n, :], func=AF.Exp, bias=nmx.t[0:n, :], scale=1.0), r=[lg, nmx], w=[lg])
                S.op("dve", lambda e, n=n, ch=ch: e.tensor_tensor(out=lg.t[0:n, :], in0=lg.t[0:n, :], in1=mktm.t[0:n, ch, :], op=ALU.mult), r=[lg, mktm], w=[lg])
                S.op("dve", lambda e, n=n: e.reduce_sum(out=ssum.t[0:n, :], in_=lg.t[0:n, :], axis=mybir.AxisListType.X), r=[lg], w=[ssum])
                S.op("dve", lambda e, n=n: e.reciprocal(out=ssum.t[0:n, :], in_=ssum.t[0:n, :]), r=[ssum], w=[ssum])
                S.op("dve", lambda e, n=n, ch=ch: e.tensor_scalar(out=gwtm.t[0:n, ch, :], in0=lg.t[0:n, :], scalar1=ssum.t[0:n, 0:1], scalar2=None, op0=ALU.mult),
                     r=[lg, ssum], w=[gwtm])
                p2 = S.ps()
                S.op("pe", lambda e, p2=p2, n=n, ch=ch: e.transpose(p2.t[0:E, 0:n], gwtm.t[0:n, ch, :], ident_f[0:n, 0:n]), r=[gwtm] + CST, w=[p2])
                S.op("dve", lambda e, p2=p2, n=n, ch=ch: e.tensor_copy(out=gwT.t[:, ch * 128:ch * 128 + n], in_=p2.t[0:E, 0:n]), r=[p2], w=[gwT])
        SCH = NCH - 1
        for ch in range(NCH):
            n = min(128, NT - ch * 128)
            p = S.ps()
            prev = [] if ch == SCH else [SCH] + list(range(ch))
            S.op("pe", lambda e, p=p, n=n, ch=ch, prev=prev: e.matmul(p.t[0:n, 0:E], mstrict_f[0:n, 0:n], mktm.t[0:n, ch, :], start=True, stop=(len(prev) == 0)),
                 r=[mktm] + CST, w=[p])
            for i2, c2 in enumerate(prev):
                k2 = min(128, NT - c2 * 128)
                S.op("pe", lambda e, p=p, n=n, c2=c2, k2=k2, i2=i2, prev=prev: e.matmul(p.t[0:n, 0:E], ones_f[0:k2, 0:n], mktm.t[0:k2, c2, :], start=False,
                                                                                      stop=(i2 == len(prev) - 1)), r=[mktm] + CST, w=[p])
            S.op("dve", lambda e, p=p, n=n, ch=ch: e.scalar_tensor_tensor(out=rktm.t[0:n, ch, :], in0=p.t[0:n, 0:E], scalar=1.0, in1=mktm.t[0:n, ch, :],
                                                                         op0=ALU.add, op1=ALU.mult), r=[p, mktm], w=[rktm])
            S.op("dve", lambda e, n=n, ch=ch: e.tensor_scalar(out=rktm.t[0:n, ch, :], in0=rktm.t[0:n, ch, :], scalar1=-1.0, scalar2=None, op0=ALU.add),
                 r=[rktm], w=[rktm])
            p2 = S.ps()
            S.op("pe", lambda e, p2=p2, n=n, ch=ch: e.transpose(p2.t[0:E, 0:n], rktm.t[0:n, ch, :], ident_f[0:n, 0:n]), r=[rktm] + CST, w=[p2])
            S.op("dve", lambda e, p2=p2, n=n, ch=ch: e.tensor_copy(out=rkT.t[:, ch * 128:ch * 128 + n], in_=p2.t[0:E, 0:n]), r=[p2], w=[rkT])
        S.barrier()
        es_m1.close()

        es_m2 = contextlib.ExitStack()
        S.cur = es_m2
        sel = sb("sel", [128, NCH, CAP], BF16)
        xbT = sb("xbT", [128, 16, CAP], BF16)
        actT = sb("actT", [128, FC, CAP], BF16)
        oute = [sb("oute%d" % i, [128, NBLK, D], BF16) for i in range(1)]
        bupT = sb("bupT", [128, 2 * FC], F32)
        bstg = sb("bstg", [128, 128], F32)
        mt = [sb("mt%d" % i, [128, CAP], F32) for i in range(3)]
        gs = sb("gs", [128, 4, CAP], F32)
        S.op("dve", lambda e: e.memset(oute[0].t[:], 0.0), w=[S.sub(oute[0], (b_, t_)) for b_ in range(NBLK) for t_ in range(4)])
        for ex in range(E):
            S.dma("sp", lambda e, ex=ex: e.dma_start(out=bstg.t[0:2 * FC, :], in_=b_up[ex * 2 * FC:(ex + 1) * 2 * FC, :]), w=[bstg])
            p = S.ps()
            S.op("pe", lambda e, p=p: e.transpose(p.t[:, 0:2 * FC], bstg.t[0:2 * FC, :], ident_f[0:2 * FC, 0:2 * FC]), r=[bstg] + CST, w=[p])
            S.op("dve", lambda e, p=p: e.tensor_copy(out=bupT.t[:, :], in_=p.t[:, 0:2 * FC]), r=[p], w=[bupT])
            for ch in range(NCH):
                n = min(128, NT - ch * 128)
                S.op("dve", lambda e, n=n, ch=ch, ex=ex: e.tensor_scalar(out=sel.t[0:n, ch, :], in0=iota_f[0:n, :], scalar1=rktm.t[0:n, ch, ex:ex + 1], scalar2=None,
                                                                        op0=ALU.is_equal), r=[rktm] + CST, w=[S.sub(sel, ch)])
            for f in range(16):
                p = S.ps()
                for ch in range(NCH):
                    n = min(128, NT - ch * 128)
                    S.op("pe", lambda e, p=p, f=f, ch=ch, n=n: e.matmul(p.t[:, 0:CAP], hntm.t[0:n, ch, f * 128:(f + 1) * 128], sel.t[0:n, ch, :],
                                                                       start=(ch == 0), stop=(ch == NCH - 1)), r=[hntm, S.sub(sel, ch)], w=[p])
                S.op("act" if f % 2 else "dve",
                     (lambda e, p=p, f=f: e.activation(out=xbT.t[:, f, :], in_=p.t[:, 0:CAP], func=AF.Copy)) if f % 2 else
                     (lambda e, p=p, f=f: e.tensor_copy(out=xbT.t[:, f, :], in_=p.t[:, 0:CAP])), r=[p], w=[S.sub(xbT, f)])
            vsrc = w_up[ex].rearrange("(kc p) n -> p kc n", p=128)
            for f4 in range(0, FC, 4):
                nb = min(4, FC - f4)
                Wg = wtile()
                S.dma("pool", lambda e, W=Wg, f4=f4, nb=nb, vsrc=vsrc: e.dma_start(out=W.t[:, :, 0:nb * 128], in_=vsrc[:, :, f4 * 128:(f4 + nb) * 128]), w=[Wg])
                for fb in range(nb):
                    f = f4 + fb
                    pg = S.ps()
                    for kc in range(16):
                        S.op("pe", lambda e, pg=pg, kc=kc, W=Wg, fb=fb: e.matmul(pg.t[:, 0:CAP], W.t[:, kc, fb * 128:(fb + 1) * 128], xbT.t[:, kc, :],
                                                                                start=(kc == 0), stop=(kc == 15)), r=[Wg, S.sub(xbT, kc)], w=[pg])
                    S.op("dve", lambda e, pg=pg, f=f: e.tensor_scalar(out=mt[0].t[:, :], in0=pg.t[:, 0:CAP], scalar1=bupT.t[:, f:f + 1], scalar2=LIMIT,
                                                                     op0=ALU.add, op1=ALU.min), r=[pg, bupT], w=[mt[0]])
                    S.op("act", lambda e: e.activation(out=mt[1].t[:, :], in_=mt[0].t[:, :], func=AF.Sigmoid, scale=ALPHA), r=[mt[0]], w=[mt[1]])
                    S.op("dve", lambda e, fb=fb: e.tensor_tensor(out=gs.t[:, fb, :], in0=mt[0].t[:, :], in1=mt[1].t[:, :], op=ALU.mult), r=[mt[0], mt[1]], w=[gs])
                Wl = wtile()
                S.dma("pool", lambda e, W=Wl, f4=f4, nb=nb, vsrc=vsrc: e.dma_start(out=W.t[:, :, 0:nb * 128], in_=vsrc[:, :, FF + f4 * 128:FF + (f4 + nb) * 128]), w=[Wl])
                for fb in range(nb):
                    f = f4 + fb
                    pl = S.ps()
                    for kc in range(16):
                        S.op("pe", lambda e, pl=pl, kc=kc, W=Wl, fb=fb: e.matmul(pl.t[:, 0:CAP], W.t[:, kc, fb * 128:(fb + 1) * 128], xbT.t[:, kc, :],
                                                                                start=(kc == 0), stop=(kc == 15)), r=[Wl, S.sub(xbT, kc)], w=[pl])
                    S.op("dve", lambda e, pl=pl, f=f: e.tensor_scalar(out=mt[2].t[:, :], in0=pl.t[:, 0:CAP], scalar1=bupT.t[:, FC + f:FC + f + 1], scalar2=LIMIT,
                                                                     op0=ALU.add, op1=ALU.min), r=[pl, bupT], w=[mt[2]])
                    S.op("dve", lambda e: e.tensor_scalar(out=mt[2].t[:, :], in0=mt[2].t[:, :], scalar1=-LIMIT, scalar2=1.0, op0=ALU.max, op1=ALU.add),
                         r=[mt[2]], w=[mt[2]])
                    S.op("dve", lambda e, f=f, fb=fb: e.tensor_tensor(out=actT.t[:, f, :], in0=gs.t[:, fb, :], in1=mt[2].t[:, :], op=ALU.mult), r=[gs, mt[2]], w=[S.sub(actT, f)])
            ot = oute[0]
            for t4 in range(4):
                W = wtile()
                vsrc = w_down[ex].rearrange("(kc p) n -> p kc n", p=128)
                S.dma("pool", lambda e, W=W, t4=t4, vsrc=vsrc: e.dma_start(out=W.t[:, 0:FC, :], in_=vsrc[:, :, t4 * 512:(t4 + 1) * 512]), w=[W])
                for blk, (b0, bn) in enumerate(BLKS):
                    py = S.ps()
                    for fc in range(FC):
                        S.op("pe", lambda e, py=py, fc=fc, W=W, b0=b0, bn=bn: e.matmul(py.t[0:bn, :], actT.t[:, fc, b0:b0 + bn], W.t[:, fc, :],
                                                                                     start=(fc == 0), stop=(fc == FC - 1)), r=[W, S.sub(actT, fc)], w=[py])
                    S.op("act" if blk % 2 else "dve",
                         (lambda e, py=py, blk=blk, bn=bn, t4=t4, ot=ot: e.activation(out=ot.t[0:bn, blk, t4 * 512:(t4 + 1) * 512], in_=py.t[0:bn, :], func=AF.Copy)) if blk % 2 else
                         (lambda e, py=py, blk=blk, bn=bn, t4=t4, ot=ot: e.tensor_copy(out=ot.t[0:bn, blk, t4 * 512:(t4 + 1) * 512], in_=py.t[0:bn, :])), r=[py], w=[S.sub(ot, (blk, t4))])
            S.dma("sp", lambda e, ex=ex, ot=ot: e.dma_start(out=oscr[ex], in_=ot.t[:, :, :]), r=[S.sub(ot, (b_, t_)) for b_ in range(NBLK) for t_ in range(4)], w=[obuf], sem=osc_sem)
        S.barrier()
        es_m2.close()
        es_h.close()

        S.cur = es
        hT = sb("hT", [128, 16, NT], F32)
        oin = [sb("oin%d" % i, [128, NBLK, D], BF16) for i in range(2)]
        selT = [sb("selT%d" % i, [128, NBLK, NT], BF16) for i in range(1)]
        gwr = [sb("gwr%d" % i, [128, NT], F32) for i in range(1)]
        ct = [sb("ct%d" % i, [128, NS], F32) for i in range(2)]
        yo = sb("yo", [128, D], F32)
        bdn = sb("bdn", [E, D], F32)
        alloc_tmps("M3")
        hsubs = [S.sub(hT, j_) for j_ in range(16)]
        hT.dsem = S.new_dsem("hTload")
        S.dma("sp", lambda e: e.dma_start(out=hT.t[:, :, :], in_=hscr.rearrange("c p t -> p c t")), w=hsubs, r=[hbuf], sem=hT.dsem)
        S.dma("sp", lambda e: e.dma_start(out=bdn.t[:, :], in_=b_down.rearrange("(e a) b -> e (a b)", a=16)), w=[bdn])
        ctiles = [(c, min(512, NP - c)) for c in range(0, NP, 512)] + [(NP, NS)]

        def accum(py, j, c0, nn, k):
            if c0 < NP:
                S.op("dve", lambda e: e.scalar_tensor_tensor(out=hT.t[:, j, c0:c0 + nn], in0=py.t[:, 0:nn], scalar=modT.t[:, M_G2 + j, NSEQ:NSEQ + 1],
                                                             in1=hT.t[:, j, c0:c0 + nn], op0=ALU.mult, op1=ALU.add), r=[py, modT, S.sub(hT, j)], w=[S.sub(hT, j)])
            else:
                cc_ = ct[k % 2]
                S.op("dve", lambda e: e.tensor_tensor(out=cc_.t[:, 0:nn].rearrange("p (s t) -> p s t", t=4), in0=py.t[:, 0:nn].rearrange("p (s t) -> p s t", t=4),
                                                      in1=modT.t[:, M_G2 + j, 0:NSEQ].unsqueeze(2).to_broadcast([128, NSEQ, 4]), op=ALU.mult),
                     r=[py, modT], w=[cc_])
                S.op("dve", lambda e: e.tensor_tensor(out=hT.t[:, j, c0:c0 + nn], in0=cc_.t[:, 0:nn], in1=hT.t[:, j, c0:c0 + nn], op=ALU.add),
                     r=[cc_, S.sub(hT, j)], w=[S.sub(hT, j)])

        for j in range(16):
            for (c0, nn) in ctiles:
                py = S.ps()
                S.op("pe", lambda e, py=py, j=j, c0=c0, nn=nn: e.matmul(py.t[:, 0:nn], bdn.t[:, j * 128:(j + 1) * 128], gwT.t[:, c0:c0 + nn], start=True, stop=True),
                     r=[bdn, gwT], w=[py])
                accum(py, j, c0, nn, j)
        for ex in range(E):
            oi, sT, gr = oin[ex % 2], selT[0], gwr[0]
            S.dma("sp", lambda e, ex=ex, oi=oi: e.dma_start(out=oi.t[:, :, :], in_=oscr[ex]), w=[oi], r=[obuf])
            S.op("dve", lambda e, ex=ex: e.tensor_copy(out=esel.t[:, :], in_=cs.t[0:E, ex:ex + 1].to_broadcast([E, 128])), r=[cs], w=[esel])
            for (c0, nn) in ctiles:
                p = S.ps()
                S.op("pe", lambda e, p=p, c0=c0, nn=nn: e.matmul(p.t[:, 0:nn], esel.t[:, :], gwT.t[:, c0:c0 + nn], start=True, stop=True), r=[esel, gwT], w=[p])
                S.op("act", lambda e, p=p, c0=c0, nn=nn, gr=gr: e.activation(out=gr.t[:, c0:c0 + nn], in_=p.t[:, 0:nn], func=AF.Copy), r=[p], w=[gr])
                p = S.ps()
                S.op("pe", lambda e, p=p, c0=c0, nn=nn: e.matmul(p.t[:, 0:nn], esel.t[:, :], rkT.t[:, c0:c0 + nn], start=True, stop=True), r=[esel, rkT], w=[p])
                for blk, (b0, bn) in enumerate(BLKS):
                    S.op("dve", lambda e, p=p, c0=c0, nn=nn, blk=blk, bn=bn, sT=sT, gr=gr: e.scalar_tensor_tensor(out=sT.t[0:bn, blk, c0:c0 + nn], in0=p.t[0:bn, 0:nn],
                                                                                                                 scalar=cs.t[0:bn, 960 + blk:961 + blk], in1=gr.t[0:bn, c0:c0 + nn],
                                                                                                                 op0=ALU.is_equal, op1=ALU.mult), r=[p, gr] + CST, w=[sT])
            for j in range(16):
                for (c0, nn) in ctiles:
                    py = S.ps()
                    for blk, (b0, bn) in enumerate(BLKS):
                        S.op("pe", lambda e, py=py, blk=blk, bn=bn, j=j, c0=c0, nn=nn, oi=oi, sT=sT: e.matmul(py.t[:, 0:nn], oi.t[0:bn, blk, j * 128:(j + 1) * 128], sT.t[0:bn, blk, c0:c0 + nn],
                                                                                                             start=(blk == 0), stop=(blk == NBLK - 1)), r=[oi, sT], w=[py])
                    accum(py, j, c0, nn, j)
        S.op("dve", lambda e: e.tensor_copy(out=hT.t[:, 0, 0:1], in_=hT.t[:, 0, 0:1]), r=hsubs, w=[hT] + hsubs)
        osem = S.new_dsem("yout")
        for (c0, nn) in ctiles:
            rmsnorm_fm(hT, c0, nn, hT, c0, nfT, None, False)
        for c0 in range(0, NT, 128):
            n = min(128, NT - c0)
            for q4 in range(4):
                p = S.ps()
                for i in range(4):
                    dc = q4 * 4 + i
                    S.op("pe", lambda e, p=p, i=i, dc=dc, c0=c0, n=n: e.transpose(p.t[0:n, i * 128:(i + 1) * 128], hT.t[:, dc, c0:c0 + n], ident_f), r=[hT] + CST, w=[p])
                S.op("act" if q4 % 2 else "dve",
                     (lambda e, p=p, q4=q4, n=n: e.activation(out=yo.t[0:n, q4 * 512:(q4 + 1) * 512], in_=p.t[0:n, :], func=AF.Copy)) if q4 % 2 else
                     (lambda e, p=p, q4=q4, n=n: e.tensor_copy(out=yo.t[0:n, q4 * 512:(q4 + 1) * 512], in_=p.t[0:n, :])), r=[p], w=[yo])
            if c0 < NP:
                S.dma("sp", lambda e, c0=c0, n=n: e.dma_start(out=y_p[c0:c0 + n, :], in_=yo.t[0:n, :]), r=[yo], sem=osem)
            else:
                S.dma("sp", lambda e, c0=c0, n=n: e.dma_start(out=y_s[c0 - NP:c0 - NP + n, :], in_=yo.t[0:n, :]), r=[yo], sem=osem)

        def fin(e):
            for d in S.dsems:
                if d[1] > 0:
                    e.wait_ge(d[0], d[1])
            return e.nop()
        S.op("sp", fin)

        S.assign()
        with nc.Block() as block:
            @block.tensor
            def _(e):
                S.run("pe", e)

            @block.vector
            def _(e):
                S.run("dve", e)

            @block.scalar
            def _(e):
                S.run("act", e)

            @block.gpsimd
            def _(e):
                S.run("pool", e)

            @block.sync
            def _(e):
                S.run("sp", e)
    return nc


def make_cst(NSEQ):
    c = np.zeros((128, 1664), np.float32)
    i = np.arange(128)
    c[:, 0:128] = np.eye(128, dtype=np.float32)
    c[:, 128:256] = (i[:, None] <= i[None, :]).astype(np.float32)
    c[:, 256:384] = (i[:, None] > i[None, :]).astype(np.float32)
    j = np.arange(64)
    same = (j[:, None] // 4) == (j[None, :] // 4)
    c[0:64, 384:448] = (same & (j[:, None] <= j[None, :])).astype(np.float32)
    c[0:64, 448:512] = (same & (j[:, None] > j[None, :])).astype(np.float32)
    c[:, 512:640] = 1.0
    c[:, 640] = EPS
    c[:, 641] = 1.0
    for s in range(NSEQ):
        c[s * 4:(s + 1) * 4, 642 + s] = 1.0
    c[:, 1152:1664] = np.arange(512, dtype=np.float32)[None, :]
    c[:, 960] = i
    c[:, 961] = i + 128
    c[:, 962] = i + 256
    c[:, 963] = i + 384
    c[:, 1024:1152] = (i[:, None] < i[None, :]).astype(np.float32)
    return c


def make_in_maps(inp, cfg, n_cores, seq_len, n_seq_prompt):
    NP, NPRE, NSEQ, E, FF = cfg["NP"], cfg["NPRE"], cfg["NSEQ"], cfg["E"], cfg["FF"]
    FC = FF // 128
    f = lambda a: np.ascontiguousarray(a, dtype=np.float32)
    shared = dict(
        cst=make_cst(NSEQ),
        w_ada=f(inp["w_ada"][0]), b_ada=f(inp["b_ada"][0].reshape(96, 128)), norm1_w=f(inp["norm1_w"][0].reshape(16, 128)),
        w_in=f(inp["w_in"][0]), w_gk_up=f(inp["w_gk_up"][0]), b_gk=f(inp["b_gk"][0].reshape(1, 512)),
        gla_norm_w=f(inp["gla_norm_w"][0].reshape(2, 128)), w_conv=f(inp["w_conv"][0].reshape(24, 128)), w_out=f(inp["w_out"][0]),
        norm2_w=f(inp["norm2_w"][0].reshape(16, 128)), w_router=f(inp["w_router"][0]), b_router=f(inp["b_router"][0].reshape(1, E)),
        w_up=f(inp["w_up"][0]), b_up=f(inp["b_up"][0].reshape(E * 2 * FC, 128)), w_down=f(inp["w_down"][0]),
        b_down=f(inp["b_down"][0].reshape(E * 16, 128)), final_norm_w=f(inp["final_norm_w"].reshape(16, 128)),
    )
    maps = []
    for c in range(n_cores):
        b, half = c // 2, c % 2
        m = dict(shared)
        m["xp"] = f(inp["x_prompt"][b, half * NP:(half + 1) * NP])
        m["xpre"] = f(inp["x_prompt"][b, 0:NPRE])
        m["pflag"] = np.full((128, 1), float(half), np.float32)
        m["xs"] = f(inp["x_sample"][c * NSEQ:(c + 1) * NSEQ].reshape(NSEQ * 4, D))
        m["cvec"] = f(np.concatenate([inp["c_sample"][c * NSEQ:(c + 1) * NSEQ], inp["c_prompt"][b:b + 1]], axis=0))
        m["sgla"] = f(inp["state_gla"][0, c * NSEQ:(c + 1) * NSEQ])
        m["sconv"] = f(inp["state_conv"][0, c * NSEQ:(c + 1) * NSEQ].reshape(NSEQ * 2, CH))
        maps.append(m)
    return maps


def gather_outputs(res, cfg, n_cores, n_seq_prompt, seq_len):
    NP, NSEQ = cfg["NP"], cfg["NSEQ"]
    y_p = np.zeros((n_seq_prompt, seq_len, D), np.float32)
    y_s = np.zeros((n_cores * NSEQ, 4, D), np.float32)
    gla_p = np.zeros((1, n_seq_prompt, HEADS, HK, HV), np.float32)
    conv_p = np.zeros((1, n_seq_prompt, 2, CH), np.float32)
    gla_s = np.zeros((1, n_cores * NSEQ, HEADS, HK, HV), np.float32)
    conv_s = np.zeros((1, n_cores * NSEQ, 2, CH), np.float32)
    for c in range(n_cores):
        b, half = c // 2, c % 2
        r = res[c]
        y_p[b, half * NP:(half + 1) * NP] = r["y_p"]
        y_s[c * NSEQ:(c + 1) * NSEQ] = r["y_s"].reshape(NSEQ, 4, D)
        if half == 1:
            gla_p[0, b] = r["gla_p"]
            conv_p[0, b] = r["conv_p"]
        gla_s[0, c * NSEQ:(c + 1) * NSEQ] = r["gla_s"]
        conv_s[0, c * NSEQ:(c + 1) * NSEQ] = r["conv_s"].reshape(NSEQ, 2, CH)
    return (y_p, y_s, gla_p, conv_p, gla_s, conv_s)


def kernel(**inputs):
    cfg = FULL
    n_cores = 8
    nc = build(cfg)
    maps = make_in_maps(inputs, cfg, n_cores, 2048, 4)
    res = run_bass_kernel_spmd(nc, maps, core_ids=list(range(n_cores)))
    return gather_outputs(res.results, cfg, n_cores, 4, 2048)
```
